# Optimizing a Trainium2 kernel written in Bass

```python
import math
import jax, jax.numpy as jnp
from jax import lax
import numpy as np

D_MODEL = 1024
BATCH = 8
SEQ = 4096
DEPTH = 1
DEC_BATCH = 32
DEC_SEQ = 4
PAST_LEN = 16384
PAGE_SIZE = 128

H_DIFF = 4
DH_DIFF = D_MODEL // (4 * H_DIFF)
DV_DIFF = 2 * DH_DIFF
W_DIFF = H_DIFF * DV_DIFF
H_RET = 4
DK_RET = D_MODEL // (2 * H_RET)
DV_RET = D_MODEL // (2 * H_RET)
W_RET = H_RET * DV_RET
IN_SPLITS = (2 * H_DIFF * DH_DIFF, 2 * H_DIFF * DH_DIFF, H_DIFF * DV_DIFF,
             H_RET * DK_RET, H_RET * DK_RET, H_RET * DV_RET, H_RET * DV_RET)
IN_COLS = 4 * H_DIFF * DH_DIFF + H_DIFF * DV_DIFF + 2 * H_RET * DK_RET + 2 * H_RET * DV_RET
Q_BLOCK = 128
RET_CHUNK = 128
ROPE_THETA = 10000.0
N_GROUPS = 4
EXPERTS_PER_GROUP = 4
N_EXPERTS = N_GROUPS * EXPERTS_PER_GROUP
D_EXPERT = D_MODEL // 4
MOE_TOPK = 2
NORM_EPS = 1e-6
GN_EPS = 1e-5

kernel_name = "hybrid_diffattn_retention_hmoe_decode_step"


def rmsnorm(x, g):
    xf = x.astype(jnp.float32)
    y = xf * lax.rsqrt(jnp.mean(xf * xf, axis=-1, keepdims=True) + NORM_EPS)
    return (y * g.astype(jnp.float32)).astype(x.dtype)


def rope(x, pos):
    d = x.shape[-1]
    half = d // 2
    inv = jnp.power(ROPE_THETA, -(2.0 / d) * jnp.arange(half, dtype=jnp.float32))
    ang = pos[:, None] * inv[None, :]
    cos = jnp.cos(ang)[None, :, None, :]
    sin = jnp.sin(ang)[None, :, None, :]
    xf = x.astype(jnp.float32)
    x1, x2 = xf[..., :half], xf[..., half:]
    return jnp.concatenate([x1 * cos - x2 * sin, x2 * cos + x1 * sin], axis=-1).astype(x.dtype)


def adaln(c, w_ada, b_ada):
    m = (jax.nn.silu(c) @ w_ada + b_ada)[:, None, :]
    return jnp.split(m, 6, axis=-1)


def mixer_inputs(h, pos, w_in):
    B, T, _ = h.shape
    z = h @ w_in
    idx = np.cumsum(IN_SPLITS)[:-1].tolist()
    qd, kd, vd, qr, kr, vr, gr = jnp.split(z, idx, axis=-1)
    qd = rope(qd.reshape(B, T, 2 * H_DIFF, DH_DIFF), pos) * (DH_DIFF ** -0.5)
    kd = rope(kd.reshape(B, T, 2 * H_DIFF, DH_DIFF), pos)
    vd = vd.reshape(B, T, H_DIFF, DV_DIFF)
    qr = rope(qr.reshape(B, T, H_RET, DK_RET), pos)
    kr = rope(kr.reshape(B, T, H_RET, DK_RET), pos) * (DK_RET ** -0.5)
    vr = vr.reshape(B, T, H_RET, DV_RET)
    return qd, kd, vd, qr, kr, vr, gr


def diff_lambda(lq1, lk1, lq2, lk2, lam_init):
    e1 = jnp.exp(jnp.sum(lq1.astype(jnp.float32) * lk1.astype(jnp.float32)))
    e2 = jnp.exp(jnp.sum(lq2.astype(jnp.float32) * lk2.astype(jnp.float32)))
    return e1 - e2 + lam_init


def diff_attn_blocked(q, k, v, lam):
    B, T = q.shape[:2]
    nb = T // Q_BLOCK
    qb = q.reshape(B, nb, Q_BLOCK, 2 * H_DIFF, DH_DIFF).swapaxes(0, 1)
    kpos = jnp.arange(T)

    def one_block(args):
        qblk, start = args
        s = jnp.einsum('bqhd,bkhd->bhqk', qblk, k).astype(jnp.float32)
        qpos = start + jnp.arange(Q_BLOCK)
        s = jnp.where(kpos[None, :] <= qpos[:, None], s, -jnp.inf)
        p = jax.nn.softmax(s, axis=-1).reshape(B, H_DIFF, 2, Q_BLOCK, T)
        a = p[:, :, 0] - lam * p[:, :, 1]
        return jnp.einsum('bhqk,bkhe->bqhe', a.astype(v.dtype), v)

    o = lax.map(one_block, (qb, jnp.arange(nb, dtype=jnp.int32) * Q_BLOCK))
    return o.swapaxes(0, 1).reshape(B, T, H_DIFF, DV_DIFF)


def diff_attn_paged(q, k_new, v_new, k_past, v_past, lam):
    B, T = q.shape[:2]
    P = k_past.shape[1]
    s_past = jnp.einsum('bqhd,bkhd->bhqk', q, k_past).astype(jnp.float32)
    s_new = jnp.einsum('bqhd,bkhd->bhqk', q, k_new).astype(jnp.float32)
    s_new = jnp.where(jnp.tril(jnp.ones((T, T), dtype=bool)), s_new, -jnp.inf)
    p = jax.nn.softmax(jnp.concatenate([s_past, s_new], axis=-1), axis=-1)
    p = p.reshape(B, H_DIFF, 2, T, P + T)
    a = (p[:, :, 0] - lam * p[:, :, 1]).astype(v_new.dtype)
    return (jnp.einsum('bhqk,bkhe->bqhe', a[..., :P], v_past)
            + jnp.einsum('bhqk,bkhe->bqhe', a[..., P:], v_new))


def retention(q, k, v, r0, chunk):
    B, T, H, DK = q.shape
    DV = v.shape[-1]
    n = T // chunk
    log_g = jnp.log1p(-jnp.exp2(-5.0 - jnp.arange(H, dtype=jnp.float32)))
    idx = jnp.arange(chunk, dtype=jnp.float32)
    dif = idx[:, None] - idx[None, :]
    decay_intra = jnp.where(dif >= 0, jnp.exp(jnp.maximum(dif, 0.0)[None] * log_g[:, None, None]), 0.0)
    q_dec = jnp.exp((idx[None, :] + 1.0) * log_g[:, None])
    k_dec = jnp.exp((chunk - 1.0 - idx[None, :]) * log_g[:, None])
    chunk_dec = jnp.exp(chunk * log_g)
    qc = q.astype(jnp.float32).reshape(B, n, chunk, H, DK)
    kc = k.astype(jnp.float32).reshape(B, n, chunk, H, DK)
    vc = v.astype(jnp.float32).reshape(B, n, chunk, H, DV)
    scores = jnp.einsum('bnihd,bnjhd->bnhij', qc, kc) * decay_intra
    o_intra = jnp.einsum('bnhij,bnjhe->bnihe', scores, vc)
    kv = jnp.einsum('bnjhd,bnjhe,hj->nbhde', kc, vc, k_dec)

    def step(r, kv_n):
        return r * chunk_dec[None, :, None, None] + kv_n, r

    r_final, r_prev = lax.scan(step, r0.astype(jnp.float32), kv)
    o_cross = jnp.einsum('bnihd,nbhde,hi->bnihe', qc, r_prev, q_dec)
    return (o_intra + o_cross).reshape(B, T, H, DV), r_final


def mixer_output(od, orr, gr, beta, lam_init, w_out):
    B, T = od.shape[:2]
    of = od.astype(jnp.float32)
    of = of * lax.rsqrt(jnp.mean(of * of, axis=-1, keepdims=True) + NORM_EPS) * (1.0 - lam_init)
    mu = jnp.mean(orr, axis=-1, keepdims=True)
    var = jnp.mean(jnp.square(orr - mu), axis=-1, keepdims=True)
    rf = ((orr - mu) * lax.rsqrt(var + GN_EPS)).reshape(B, T, W_RET) * jax.nn.silu(gr.astype(jnp.float32))
    y = jnp.concatenate([of.reshape(B, T, W_DIFF), rf], axis=-1) * beta.astype(jnp.float32)
    return y.astype(w_out.dtype) @ w_out


def hier_moe(h, w_group, b_group, w_er, b_er, w_g, w_u, w_d):
    B, T, D = h.shape
    x = h.reshape(B * T, D)
    lg = (x @ w_group + b_group).astype(jnp.float32)
    pg = jax.nn.softmax(lg, axis=-1)
    gsel = jnp.argmax(lg, axis=-1)
    pg_sel = jnp.take_along_axis(pg, gsel[:, None], axis=1)
    le_all = (jnp.einsum('nd,gde->nge', x, w_er) + b_er).astype(jnp.float32)
    le = jnp.take_along_axis(le_all, gsel[:, None, None], axis=1)[:, 0]
    top_v, top_i = lax.top_k(le, MOE_TOPK)
    w_top = jax.nn.softmax(top_v, axis=-1) * pg_sel
    e_idx = gsel[:, None] * EXPERTS_PER_GROUP + top_i
    gates = jnp.einsum('nk,nke->ne', w_top, jax.nn.one_hot(e_idx, N_EXPERTS, dtype=jnp.float32))
    a = jnp.einsum('nd,edf->nef', x, w_g)
    u = jnp.einsum('nd,edf->nef', x, w_u)
    hh = jax.nn.silu(a) * u * gates[..., None].astype(x.dtype)
    return jnp.einsum('nef,efd->nd', hh, w_d).reshape(B, T, D)


def run_layer(x, c, pos, attend, r0, ret_chunk, w_ada, b_ada, g_mix, g_ffn, w_in, lam, lam_init,
              beta, w_out, w_group, b_group, w_er, b_er, w_g, w_u, w_d):
    sh1, sc1, gt1, sh2, sc2, gt2 = adaln(c, w_ada, b_ada)
    h = rmsnorm(x, g_mix) * (1.0 + sc1) + sh1
    qd, kd, vd, qr, kr, vr, gr = mixer_inputs(h, pos, w_in)
    od = attend(qd, kd, vd, lam)
    orr, r_new = retention(qr, kr, vr, r0, ret_chunk)
    x = x + gt1 * mixer_output(od, orr, gr, beta, lam_init, w_out)
    h = rmsnorm(x, g_ffn) * (1.0 + sc2) + sh2
    x = x + gt2 * hier_moe(h, w_group, b_group, w_er, b_er, w_g, w_u, w_d)
    return x, kd, vd, r_new


def setup_inputs(seed: int = 0) -> dict:
    key = jax.random.key(seed)
    ks = jax.random.split(key, 27)

    def nrm(k, shape, scale):
        return jax.random.normal(k, shape, jnp.float32) * scale

    n_pages = PAST_LEN // PAGE_SIZE
    n_used = DEC_BATCH * n_pages
    n_phys = n_used + max(1, n_used // 4)
    page_table = jax.random.permutation(ks[5], n_phys)[:n_used].reshape(DEC_BATCH, n_pages).astype(jnp.int32)
    D = D_MODEL
    return {
        "x_prompt": nrm(ks[0], (BATCH, SEQ, D), 1.0),
        "x_sample": nrm(ks[1], (DEC_BATCH, DEC_SEQ, D), 1.0),
        "cache_k": nrm(ks[2], (DEPTH, n_phys, PAGE_SIZE, 2 * H_DIFF, DH_DIFF), 1.0),
        "cache_v": nrm(ks[3], (DEPTH, n_phys, PAGE_SIZE, H_DIFF, DV_DIFF), 1.0),
        "state_ret": nrm(ks[4], (DEPTH, DEC_BATCH, H_RET, DK_RET, DV_RET), 0.5),
        "page_table": page_table,
        "c_prompt": nrm(ks[6], (BATCH, D), 1.0),
        "c_sample": nrm(ks[7], (DEC_BATCH, D), 1.0),
        "w_ada": nrm(ks[8], (DEPTH, D, 6 * D), 0.5 * D ** -0.5),
        "b_ada": nrm(ks[9], (DEPTH, 6 * D), 0.01),
        "norm_mix_g": 1.0 + nrm(ks[10], (DEPTH, D), 0.02),
        "norm_ffn_g": 1.0 + nrm(ks[11], (DEPTH, D), 0.02),
        "w_in": nrm(ks[12], (DEPTH, D, IN_COLS), D ** -0.5),
        "lambda_q1": nrm(ks[13], (DEPTH, DH_DIFF), 0.1),
        "lambda_k1": nrm(ks[14], (DEPTH, DH_DIFF), 0.1),
        "lambda_q2": nrm(ks[15], (DEPTH, DH_DIFF), 0.1),
        "lambda_k2": nrm(ks[16], (DEPTH, DH_DIFF), 0.1),
        "beta_mix": 1.0 + nrm(ks[17], (DEPTH, D), 0.02),
        "w_out": nrm(ks[18], (DEPTH, D, D), D ** -0.5),
        "w_group": nrm(ks[19], (DEPTH, D, N_GROUPS), D ** -0.5),
        "b_group": nrm(ks[20], (DEPTH, N_GROUPS), 0.01),
        "w_expert_router": nrm(ks[21], (DEPTH, N_GROUPS, D, EXPERTS_PER_GROUP), D ** -0.5),
        "b_expert_router": nrm(ks[22], (DEPTH, N_GROUPS, EXPERTS_PER_GROUP), 0.01),
        "w_gate_e": nrm(ks[23], (DEPTH, N_EXPERTS, D, D_EXPERT), D ** -0.5),
        "w_up_e": nrm(ks[24], (DEPTH, N_EXPERTS, D, D_EXPERT), D ** -0.5),
        "w_down_e": nrm(ks[25], (DEPTH, N_EXPERTS, D_EXPERT, D), D_EXPERT ** -0.5),
        "final_g": 1.0 + nrm(ks[26], (D,), 0.02),
    }


def reference(x_prompt, x_sample, cache_k, cache_v, state_ret, page_table, c_prompt, c_sample,
              w_ada, b_ada, norm_mix_g, norm_ffn_g, w_in, lambda_q1, lambda_k1, lambda_q2, lambda_k2,
              beta_mix, w_out, w_group, b_group, w_expert_router, b_expert_router,
              w_gate_e, w_up_e, w_down_e, final_g):
    Bp, Tp = x_prompt.shape[:2]
    Bs, Ts = x_sample.shape[:2]
    n_pages = page_table.shape[1]
    past_len = n_pages * PAGE_SIZE
    pos_p = jnp.arange(Tp, dtype=jnp.float32)
    pos_s = past_len + jnp.arange(Ts, dtype=jnp.float32)
    xp, xs = x_prompt, x_sample
    kps, vps, rps, kss, vss, rss = [], [], [], [], [], []
    for l in range(DEPTH):
        lam_init = 0.8 - 0.6 * math.exp(-0.3 * l)
        lam = diff_lambda(lambda_q1[l], lambda_k1[l], lambda_q2[l], lambda_k2[l], lam_init)
        shared = (w_ada[l], b_ada[l], norm_mix_g[l], norm_ffn_g[l], w_in[l], lam, lam_init,
                  beta_mix[l], w_out[l], w_group[l], b_group[l], w_expert_router[l], b_expert_router[l],
                  w_gate_e[l], w_up_e[l], w_down_e[l])
        r0p = jnp.zeros((Bp, H_RET, DK_RET, DV_RET), jnp.float32)
        xp, kp, vp, rp = run_layer(xp, c_prompt, pos_p, diff_attn_blocked, r0p, RET_CHUNK, *shared)
        k_past = cache_k[l, page_table].reshape(Bs, past_len, 2 * H_DIFF, DH_DIFF)
        v_past = cache_v[l, page_table].reshape(Bs, past_len, H_DIFF, DV_DIFF)
        attend_s = lambda q, k, v, lm: diff_attn_paged(q, k, v, k_past, v_past, lm)
        xs, ks_, vs_, rs = run_layer(xs, c_sample, pos_s, attend_s, state_ret[l], Ts, *shared)
        kps.append(kp); vps.append(vp); rps.append(rp.astype(state_ret.dtype))
        kss.append(ks_); vss.append(vs_); rss.append(rs.astype(state_ret.dtype))
    y_prompt = rmsnorm(xp, final_g)
    y_sample = rmsnorm(xs, final_g)
    return (y_prompt, y_sample, jnp.stack(kps), jnp.stack(vps), jnp.stack(rps),
            jnp.stack(kss), jnp.stack(vss), jnp.stack(rss))
```

```python
import math
from contextlib import ExitStack
import numpy as np
import concourse.bass as bass
import concourse.mybir as mybir
from concourse.bass_utils import run_bass_kernel_spmd

F32 = mybir.dt.float32
BF16 = mybir.dt.bfloat16
I32 = mybir.dt.int32
AF = mybir.ActivationFunctionType
ALU = mybir.AluOpType
AX = mybir.AxisListType

D = 1024
NCOL = 3584
NEXP = 16
DEXP = 256
EPS = 1e-6
GN_EPS = 1e-5
LAM_INIT = 0.2
CAP = 1152
SCH = 384
TRASH = NEXP * CAP


class R:
    __slots__ = ("name", "w", "rd", "excl")

    ALL = []

    def __init__(self, name):
        self.name = name
        self.w = None
        self.rd = {}
        self.excl = False
        R.ALL.append(self)


class Sched:
    def __init__(self, nc, es):
        self.nc, self.es = nc, es
        self.engs = {"pe": nc.tensor, "act": nc.scalar, "dve": nc.vector, "pool": nc.gpsimd, "sp": nc.sync}
        self.sems, self.cnt = {}, {}
        for e in ("pe", "act", "dve", "pool"):
            self.sems[e] = es.enter_context(nc.semaphore("s_" + e))
            self.cnt[e] = 0
        self.seen = {e: {} for e in self.engs}
        self.nops = {e: 0 for e in self.engs}

    def _deps(self, r, w):
        d = {}

        def add(k, v):
            if d.get(k, 0) < v:
                d[k] = v
        for x in r:
            if x.w is not None:
                add(*x.w)
            if x.excl:
                for k, v in x.rd.items():
                    add(k, v)
        for x in w:
            if x.w is not None:
                add(*x.w)
            for k, v in x.rd.items():
                add(k, v)
        return d

    def _wait(self, E, deps):
        for k, v in deps.items():
            if k == E and E == "pe":
                continue
            if self.seen[E].get(k, 0) >= v:
                continue
            self.engs[E].wait_ge(self.sems[k], v)
            self.seen[E][k] = v

    def _mark(self, tok, r, w):
        k, v = tok
        for x in r:
            if x.excl:
                x.w = tok
                x.rd = {}
            elif x.rd.get(k, 0) < v:
                x.rd[k] = v
        for x in w:
            x.w = tok
            x.rd = {}

    def op(self, E, fn, r=(), w=(), sig=True):
        self._wait(E, self._deps(r, w))
        ins = fn()
        self.nops[E] += 1
        if sig:
            self.cnt[E] += 1
            ins.then_inc(self.sems[E], 1)
            tok = (E, self.cnt[E])
        else:
            tok = (E, self.cnt[E] + 1)
        self._mark(tok, r, w)
        return ins

    def dma(self, Q, fn, r=(), w=(), key=None):
        self._wait(Q, self._deps(r, w))
        if key is None:
            key = ("L_" + w[0].name) if w else ("S_" + r[0].name)
        if key not in self.sems:
            self.sems[key] = self.es.enter_context(self.nc.semaphore(key))
            self.cnt[key] = 0
        ins = fn()
        self.nops[Q] += 1
        self.cnt[key] += 16
        ins.then_inc(self.sems[key], 16)
        tok = (key, self.cnt[key])
        self._mark(tok, r, w)
        return tok

    def finish(self, resources):
        d = {}
        for x in resources:
            if x.w is not None and d.get(x.w[0], 0) < x.w[1]:
                d[x.w[0]] = x.w[1]
            for k, v in x.rd.items():
                if d.get(k, 0) < v:
                    d[k] = v
        self._wait("sp", d)


def build(T, NPG, NPHYS, debug=False):
    import os
    STAGE = int(os.environ.get("KSTAGE", "9"))
    KCUT = int(os.environ.get("KCUT", "99"))
    DEFER = (STAGE >= 5) and not os.environ.get("KNODEFER")
    R.ALL = []
    NT = T // 128
    nc = bass.Bass("TRN2", target_bir_lowering=False)
    es = ExitStack()
    S = Sched(nc, es)

    def din(name, shape, dt=F32):
        return nc.dram_tensor(name, list(shape), dt, kind="ExternalInput").ap()

    def dout(name, shape, dt=F32):
        return nc.dram_tensor(name, list(shape), dt, kind="ExternalOutput").ap()

    xp = din("xp", [T, D])
    xs = din("xs", [16, D])
    cp_rep = din("cp_rep", [128, D])
    cs_rep = din("cs_rep", [16, D])
    w_ada = din("w_ada", [D, 6 * D])
    b_ada = din("b_ada", [1, 6 * D])
    g_mix = din("g_mix", [1, D])
    g_ffn = din("g_ffn", [1, D])
    w_in = din("w_in", [D, NCOL])
    lam_p = din("lam_p", [1, 256])
    beta = din("beta", [1, D])
    w_out = din("w_out", [D, D])
    w_rt = din("w_rt", [D, 20])
    b_rt = din("b_rt", [1, 20])
    w_ge = din("w_ge", [NEXP, D, DEXP])
    w_ue = din("w_ue", [NEXP, D, DEXP])
    w_de = din("w_de", [NEXP, DEXP, D])
    fin_g = din("fin_g", [1, D])
    c_ident = din("c_ident", [128, 128])
    c_mask = din("c_mask", [128, 128])
    c_ut = din("c_ut", [128, 128])
    c_rope_p = din("c_rope_p", [T, 384])
    c_rope_s = din("c_rope_s", [16, 384])
    c_dect = din("c_dect", [128, 512])
    c_qdec = din("c_qdec", [128, 512])
    c_kdec = din("c_kdec", [128, 4])
    c_ec = din("c_ec", [128, 18])
    c_dect_s = din("c_dect_s", [128, 512])
    c_qdec_s = din("c_qdec_s", [128, 2048])
    c_kdec_s = din("c_kdec_s", [128, 16])
    c_masknew = din("c_masknew", [128, 128])
    c_sel = din("c_sel", [32, 256])
    c_smask = din("c_smask", [32, 2])
    c_pcol = din("c_pcol", [128, 1])
    ckv = din("ckv", [NPHYS * 128, 1024])
    ptab = din("ptab", [1, 4 * NPG], I32)
    st_ret = din("st_ret", [4, 4, 128, 128])

    y_p = dout("y_p", [T, D])
    y_s = dout("y_s", [16, D])
    k_p = dout("k_p", [T, 512])
    v_p = dout("v_p", [T, 512])
    ret_p = dout("ret_p", [4, 128, 128])
    k_s = dout("k_s", [16, 512])
    v_s = dout("v_s", [16, 512])
    ret_s = dout("ret_s", [4, 4, 128, 128])

    NTT = NT + 1
    x2_scr = nc.dram_tensor("x2_scr", [NTT * 128, D], F32, kind="Internal").ap()
    X_scr = nc.dram_tensor("X_scr", [TRASH + 256, D], BF16, kind="Internal").ap()
    Y_scr = nc.dram_tensor("Y_scr", [TRASH + 256, D], F32, kind="Internal").ap()
    mods_scr = nc.dram_tensor("mods_scr", [16, 6 * D], F32, kind="Internal").ap()
    gt2_scr = nc.dram_tensor("gt2_scr", [1, D], F32, kind="Internal").ap()

    def sb(name, shape, dt=F32):
        t = es.enter_context(nc.sbuf_tensor(name, list(shape), dt))
        return t, R(name)

    def ps(name, shape, dt=F32):
        t = es.enter_context(nc.psum_tensor(name, list(shape), dt))
        r = R(name)
        r.excl = True
        return t, r

    V, A, P, PE, SP = "dve", "act", "pool", "pe", "sp"

    def barrier(resources, tag):
        d = {}
        for x in resources:
            if x.w is not None and d.get(x.w[0], 0) < x.w[1]:
                d[x.w[0]] = x.w[1]
            for k, v in x.rd.items():
                if d.get(k, 0) < v:
                    d[k] = v
        for E in ("pe", "act", "dve", "pool", "sp"):
            S._wait(E, d)

    pG = [ps(f"pG{i}", [128, 512]) for i in range(2)]
    pT = [ps(f"pT{i}", [128, 1024], BF16) for i in range(1)]
    pS = [ps(f"pS{i}", [128, 512]) for i in range(2)]
    pO = ps("pO", [128, 512])
    pX = [ps(f"pX{i}", [128, 512]) for i in range(2)]
    pO_reg = [R(f"pO_r{i}") for i in range(3)]
    gctr = [0]

    def nextG():
        gctr[0] += 1
        return pG[gctr[0] % 2]

    ident_b, r_ident_b = sb("ident_b", [128, 128], BF16)
    ident_f, r_ident_f = sb("ident_f", [128, 128], F32)
    mask_b, r_mask = sb("mask_b", [128, 128], BF16)
    ut_b, r_ut = sb("ut_b", [128, 128], BF16)
    ones_b, r_ones = sb("ones_b", [128, 128], BF16)
    dect, r_dect = sb("dect", [128, 512], F32)
    qdec, r_qdec = sb("qdec", [128, 512], BF16)
    kdec, r_kdec = sb("kdec", [128, 4], F32)
    ecr, r_ecr = sb("ecr", [128, 18], F32)
    beta_fm, r_betafm = sb("beta_fm", [128, 8])
    brt_bc, r_brt = sb("brt_bc", [128, 20])
    wrt, r_wrt = sb("wrt", [128, 8, 20], F32)
    a2_bc, r_a2 = sb("a2_bc", [128, D])
    sh2_bc, r_sh2 = sb("sh2_bc", [128, D])
    a1_fm, r_a1fm = sb("a1_fm", [128, 8])
    sh1_fm, r_sh1fm = sb("sh1_fm", [128, 8])
    neglam, r_neglam = sb("neglam", [128, 1])
    idx_all, r_idx = sb("idx_all", [128, NTT, 2], I32)
    wts_all, r_wts = sb("wts_all", [128, NTT, 2], F32)
    base_bc, r_base = sb("base_bc", [128, 16], F32)
    win, r_win = sb("win", [128, 8, NCOL], BF16)
    wout, r_wout = sb("wout", [128, 8, D], BF16)

    def load_const(dst, rdst, src, eng=SP):
        if eng == SP:
            S.dma(SP, lambda: nc.sync.dma_start(out=dst, in_=src), w=[rdst])
        else:
            S.dma(P, lambda: nc.gpsimd.dma_start(out=dst, in_=src), w=[rdst])

    load_const(ident_f[:], r_ident_f, c_ident)
    load_const(ident_b[:], r_ident_b, c_ident, P)
    load_const(mask_b[:], r_mask, c_mask, P)
    load_const(ut_b[:], r_ut, c_ut, P)
    load_const(dect[:], r_dect, c_dect)
    load_const(qdec[:], r_qdec, c_qdec, P)
    load_const(kdec[:], r_kdec, c_kdec)
    load_const(ecr[:], r_ecr, c_ec)
    load_const(brt_bc[:], r_brt, b_rt.partition_broadcast(128))
    load_const(wrt[:], r_wrt, w_rt.rearrange("(k p) n -> p k n", p=128))
    S.op(V, lambda: nc.vector.memset(ones_b[:], 1.0), w=[r_ones])
    S.op(V, lambda: nc.vector.memset(base_bc[:], 0.0), w=[r_base])
    w_in_v = w_in.rearrange("(k p) n -> p k n", p=128)
    for k in range(8):
        S.dma(P, lambda k=k: nc.gpsimd.dma_start(out=win[:, k, :], in_=w_in_v[:, k, :]), w=[r_win], key="L_win")
    S.dma(P, lambda: nc.gpsimd.dma_start(out=wout[:], in_=w_out.rearrange("(k p) n -> p k n", p=128)), w=[r_wout])

    es_setup = ExitStack()

    def sbt(name, shape, dt=F32):
        t = es_setup.enter_context(nc.sbuf_tensor(name, list(shape), dt))
        return t, R(name)

    lam_t, r_lamt = sbt("lam_t", [128, 256])
    lam_j, r_lamj = sbt("lam_j", [128, 64])
    lam_s, r_lams = sbt("lam_s", [128, 2])
    S.dma(SP, lambda: nc.sync.dma_start(out=lam_t[:], in_=lam_p.partition_broadcast(128)), w=[r_lamt])
    for i in range(2):
        S.op(V, lambda i=i: nc.vector.scalar_tensor_tensor(
            out=lam_j[:], in0=lam_t[:, 128 * i:128 * i + 64], scalar=1.0, in1=lam_t[:, 128 * i + 64:128 * i + 128],
            op0=ALU.mult, op1=ALU.mult, accum_out=lam_s[:, i:i + 1]),
            r=[r_lamt], w=[r_lamj, r_lams])
    S.op(A, lambda: nc.scalar.activation(out=lam_s[:], in_=lam_s[:], func=AF.Exp), r=[r_lams], w=[r_lams])
    S.op(V, lambda: nc.vector.scalar_tensor_tensor(out=neglam[:], in0=lam_s[:, 1:2], scalar=-LAM_INIT,
                                                   in1=lam_s[:, 0:1], op0=ALU.add, op1=ALU.subtract),
         r=[r_lams], w=[r_neglam])

    if KCUT == 1:
        barrier(R.ALL, 'end')
        return nc, es
    modp, r_modp = sbt("modp", [128, 6 * D])
    mods, r_mods = sbt("mods", [16, 6 * D])
    beta_bc, r_beta = sbt("beta_bc", [128, D])
    S.dma(SP, lambda: nc.sync.dma_start(out=beta_bc[:], in_=beta.partition_broadcast(128)), w=[r_beta])
    c_f, r_cf = sbt("c_f", [128, D])
    c_b, r_cb = sbt("c_b", [128, D], BF16)
    cs_f, r_csf = sbt("cs_f", [16, D])
    cs_b, r_csb = sbt("cs_b", [16, D], BF16)
    cT, r_cT = sbt("cT", [128, 8, 128], BF16)
    csT, r_csT = sbt("csT", [128, 8, 16], BF16)
    bada, r_bada = sbt("bada", [128, 6 * D])
    S.dma(SP, lambda: nc.sync.dma_start(out=c_f[:], in_=cp_rep), w=[r_cf])
    S.dma(SP, lambda: nc.sync.dma_start(out=cs_f[:], in_=cs_rep), w=[r_csf])
    S.dma(SP, lambda: nc.sync.dma_start(out=bada[:], in_=b_ada.partition_broadcast(128)), w=[r_bada])
    S.op(A, lambda: nc.scalar.activation(out=c_b[:], in_=c_f[:], func=AF.Silu), r=[r_cf], w=[r_cb])
    S.op(A, lambda: nc.scalar.activation(out=cs_b[:], in_=cs_f[:], func=AF.Silu), r=[r_csf], w=[r_csb])
    pt, r_pt = pT[0]
    for k in range(8):
        S.op(PE, lambda k=k: nc.tensor.transpose(pt[:, k * 128:(k + 1) * 128], c_b[:, k * 128:(k + 1) * 128], ident_b[:]),
             r=[r_cb, r_ident_b], w=[r_pt], sig=(k == 7))
    S.op(A, lambda: nc.scalar.copy(out=cT[:].rearrange("p k t -> p (k t)"), in_=pt[:]), r=[r_pt], w=[r_cT])
    for k in range(8):
        S.op(PE, lambda k=k: nc.tensor.transpose(pt[:, k * 16:(k + 1) * 16], cs_b[:, k * 128:(k + 1) * 128], ident_b[0:16, 0:16]),
             r=[r_csb, r_ident_b], w=[r_pt], sig=(k == 7))
    S.op(A, lambda: nc.scalar.copy(out=csT[:].rearrange("p k t -> p (k t)"), in_=pt[:, 0:128]), r=[r_pt], w=[r_csT])
    wa = [sbt(f"wa{i}", [128, 8, 512], BF16) for i in range(2)]
    w_ada_v = w_ada.rearrange("(k p) n -> p k n", p=128)
    for g in range(12):
        wt, r_w = wa[g % 2]
        S.dma(P, lambda g=g, wt=wt: nc.gpsimd.dma_start(out=wt[:], in_=w_ada_v[:, :, g * 512:(g + 1) * 512]), w=[r_w])
        for (lT, r_l, M, dst, r_d) in ((cT, r_cT, 128, modp, r_modp), (csT, r_csT, 16, mods, r_mods)):
            pg, r_pg = nextG()
            for k in range(8):
                S.op(PE, lambda k=k, pg=pg, lT=lT, M=M, wt=wt: nc.tensor.matmul(
                    pg[0:M, :], lT[:, k, :], wt[:, k, :], start=(k == 0), stop=(k == 7)),
                    r=[r_l, r_w], w=[r_pg], sig=(k == 7))
            S.op(V, lambda pg=pg, M=M, dst=dst, g=g: nc.vector.tensor_tensor(
                out=dst[0:M, g * 512:(g + 1) * 512], in0=pg[0:M, :], in1=bada[0:M, g * 512:(g + 1) * 512], op=ALU.add),
                r=[r_pg, r_bada], w=[r_d])
    gm_bc, r_gm = sbt("gm_bc", [128, D])
    gf_bc, r_gf = sbt("gf_bc", [128, D])
    a1_tok, r_a1t = sbt("a1_tok", [128, D])
    S.dma(SP, lambda: nc.sync.dma_start(out=gm_bc[:], in_=g_mix.partition_broadcast(128)), w=[r_gm])
    S.dma(SP, lambda: nc.sync.dma_start(out=gf_bc[:], in_=g_ffn.partition_broadcast(128)), w=[r_gf])
    S.op(V, lambda: nc.vector.scalar_tensor_tensor(out=a1_tok[:], in0=modp[:, D:2 * D], scalar=1.0, in1=gm_bc[:],
                                                   op0=ALU.add, op1=ALU.mult), r=[r_modp, r_gm], w=[r_a1t])
    S.op(V, lambda: nc.vector.scalar_tensor_tensor(out=a2_bc[:], in0=modp[:, 4 * D:5 * D], scalar=1.0, in1=gf_bc[:],
                                                   op0=ALU.add, op1=ALU.mult), r=[r_modp, r_gf], w=[r_a2])
    S.op(V, lambda: nc.vector.tensor_copy(out=sh2_bc[:], in_=modp[:, 3 * D:4 * D]), r=[r_modp], w=[r_sh2])
    r_gt2scr = R("gt2scr")
    S.dma(SP, lambda: nc.sync.dma_start(out=gt2_scr, in_=modp[0:1, 5 * D:6 * D]), r=[r_modp], w=[r_gt2scr], key="S_modp")
    S.op(V, lambda: nc.vector.scalar_tensor_tensor(out=mods[:, D:2 * D], in0=mods[:, D:2 * D], scalar=1.0, in1=gm_bc[0:16, :],
                                                   op0=ALU.add, op1=ALU.mult), r=[r_mods, r_gm], w=[r_mods])
    S.op(V, lambda: nc.vector.scalar_tensor_tensor(out=mods[:, 4 * D:5 * D], in0=mods[:, 4 * D:5 * D], scalar=1.0, in1=gf_bc[0:16, :],
                                                   op0=ALU.add, op1=ALU.mult), r=[r_mods, r_gf], w=[r_mods])
    for (src, r_src, dstt, r_dst) in ((a1_tok, r_a1t, a1_fm, r_a1fm), (modp, r_modp, sh1_fm, r_sh1fm), (beta_bc, r_beta, beta_fm, r_betafm)):
        for half in range(2):
            pg, r_pg = nextG()
            for kk in range(4):
                k = half * 4 + kk
                S.op(PE, lambda k=k, kk=kk, pg=pg, src=src: nc.tensor.transpose(
                    pg[:, kk * 128:(kk + 1) * 128], src[:, k * 128:(k + 1) * 128], ident_f[:]),
                    r=[r_src, r_ident_f], w=[r_pg], sig=(kk == 3))
            S.op(V, lambda pg=pg, dstt=dstt, half=half: nc.vector.tensor_copy(
                out=dstt[:, half * 4:half * 4 + 4], in_=pg[:].rearrange("p (k t) -> p k t", k=4)[:, :, 0]),
                r=[r_pg], w=[r_dst])

    r_modsscr = R("modsscr")
    S.dma(SP, lambda: nc.sync.dma_start(out=mods_scr, in_=mods[:]), r=[r_mods], w=[r_modsscr], key="S_mods")
    for k in range(8):
        S.op(V, lambda k=k: nc.vector.scalar_tensor_tensor(out=wout[:, k, :], in0=wout[:, k, :], scalar=beta_fm[:, k:k + 1],
                                                           in1=modp[:, 2 * D:3 * D], op0=ALU.mult, op1=ALU.mult),
             r=[r_wout, r_betafm, r_modp], w=[r_wout])
    setup_res = [r_lamt, r_lamj, r_lams, r_modp, r_cf, r_cb, r_csf, r_csb, r_cT, r_csT, r_bada,
                 wa[0][1], wa[1][1], r_gm, r_gf, r_a1t, r_mods, r_beta]
    setup_guard = R("setup_guard")

    def barrier(resources, tag):
        d = {}
        for x in resources:
            if x.w is not None and d.get(x.w[0], 0) < x.w[1]:
                d[x.w[0]] = x.w[1]
            for k, v in x.rd.items():
                if d.get(k, 0) < v:
                    d[k] = v
        for E in ("pe", "act", "dve", "pool", "sp"):
            S._wait(E, d)

    if KCUT == 2:
        barrier(R.ALL, 'end')
        return nc, es
    barrier(setup_res, "setup")
    es_setup.close()

    es1 = ExitStack()

    def sb1(name, shape, dt=F32):
        t = es1.enter_context(nc.sbuf_tensor(name, list(shape), dt))
        return t, R(name)

    p1_res = []

    def sb1r(name, shape, dt=F32):
        t, r = sb1(name, shape, dt)
        p1_res.append(r)
        return t, r

    rst, r_rst = sb1r("rst", [128, 4, 128], F32)
    rbf, r_rbf = sb1r("rbf", [128, 4, 128], BF16)
    S.op(V, lambda: nc.vector.memset(rst[:].rearrange("p a b -> p (a b)"), 0.0), w=[r_rst])
    S.op(V, lambda: nc.vector.memset(rbf[:].rearrange("p a b -> p (a b)"), 0.0), w=[r_rbf])

    def ring(name, n, shape, dt=F32):
        out = []
        for i in range(n):
            out.append(sb1r(f"{name}{i}", shape, dt))
        return out

    xt = ring("xt", 2, [128, D])
    rt = ring("rt", 2, [128, 384])
    xn, r_xn = sb1r("xn", [128, D], BF16)
    junk, r_junk = xn, r_xn
    ymix, r_ymix = xn, r_xn
    hT, r_hT = sb1r("hT", [128, 8, 128], BF16)
    yT, r_yT = hT, r_hT
    st4, r_st4 = sb1r("st4", [128, 8])
    tt, r_tt = sb1r("tt", [128, D])
    t1, r_t1 = tt[:, 0:512], r_tt
    t2, r_t2 = tt[:, 512:1024], r_tt
    h2f, r_h2f = tt, r_tt
    rb_, r_rb = sb1r("rb_", [128, 512], BF16)
    qb, r_qb = rb_, r_rb
    kb, r_kb = rb_, r_rb
    qrb, r_qrb = rb_, r_rb
    QT = ring("QT", 2, [128, 4, 128], BF16)
    kf = ring("kf", 1, [128, 512])
    vf = ring("vf", 1, [128, 512])
    qrT_r = ring("qrT", 2, [128, 4, 128], BF16)
    qrTd_r = ring("qrTd", 2, [128, 4, 128], BF16)
    krb_r = ring("krb", 2, [128, 4, 128], BF16)
    krd_r = ring("krd", 2, [128, 4, 128], BF16)
    krT_r = ring("krT", 2, [128, 4, 128], BF16)
    vrb_r = ring("vrb", 2, [128, 4, 128], BF16)
    sg_r = ring("sg", 2, [128, 512], BF16)
    PTr = ring("PT", 2, [128, 512], BF16)
    odr = ring("od", 2, [128, 4, 128])
    rl, r_rl = sb1r("rl", [128, 2])
    sTm, r_sTm = sb1r("sTm", [128, 4, 128], BF16)
    h2b = ring("h2b", 2, [128, D], BF16)
    h2T, r_h2T = sb1r("h2T", [128, 4, 128], F32)
    rlg, r_rlg = sb1r("rlg", [128, 20])
    rs_, r_rs = sb1r("rs_", [128, 100])
    oh16, r_oh16 = sb1r("oh16", [128, 2, 16])
    mb, r_mb = sb1r("mb", [128, 16], BF16)
    slotf, r_slotf = sb1r("slotf", [128, 4])

    def rope(pg, r_pg, H, tab, r_tab, coff, out_ap, r_out):
        hf = 256 // H
        zv = pg[:].rearrange("p (h two f) -> p h two f", h=H, two=2)
        cosv = tab[:, coff:coff + 2 * hf].rearrange("p (two f) -> p two f", two=2).unsqueeze(1).broadcast_to([128, H, 2, hf])
        sinv = tab[:, coff + 2 * hf:coff + 4 * hf].rearrange("p (two f) -> p two f", two=2)
        t1v = t1.rearrange("p (h two f) -> p h two f", h=H, two=2)
        t2v = t2.rearrange("p (h two f) -> p h two f", h=H, two=2)
        S.op(V, lambda: nc.vector.tensor_tensor(out=t1v, in0=zv, in1=cosv, op=ALU.mult), r=[r_pg, r_tab], w=[r_t1])
        for half in range(2):
            S.op(V, lambda half=half: nc.vector.tensor_tensor(
                out=t2v[:, :, half, :], in0=zv[:, :, 1 - half, :],
                in1=sinv[:, half, :].unsqueeze(1).broadcast_to([128, H, hf]), op=ALU.mult),
                r=[r_pg, r_tab], w=[r_t2])
        S.op(V, lambda: nc.vector.tensor_tensor(out=out_ap, in0=t1, in1=t2, op=ALU.add), r=[r_t1], w=[r_out])

    def transpose4(src, r_src, dst, r_dst, copy_eng=A):
        pt, r_pt = pT[0]
        for c in range(4):
            S.op(PE, lambda c=c: nc.tensor.transpose(pt[:, c * 128:(c + 1) * 128], src[:, c * 128:(c + 1) * 128], ident_b[:]),
                 r=[r_src, r_ident_b], w=[r_pt], sig=(c == 3))
        if copy_eng == A:
            S.op(A, lambda: nc.scalar.copy(out=dst, in_=pt[:, 0:512]), r=[r_pt], w=[r_dst])
        else:
            S.op(V, lambda: nc.vector.tensor_copy(out=dst, in_=pt[:, 0:512]), r=[r_pt], w=[r_dst])

    def rms_rstd(src, r_src, dst_col, r_dst, n, eps, M=128):
        S.op(A, lambda: nc.scalar.activation(out=junk[0:M, 0:n], in_=src, func=AF.Square, accum_out=dst_col),
             r=[r_src], w=[r_junk, r_dst])
        S.op(A, lambda: nc.scalar.activation(out=dst_col, in_=dst_col, func=AF.Ln, scale=1.0 / n, bias=eps_t[0:M, 0:1]),
             r=[r_dst, r_eps], w=[r_dst])
        S.op(A, lambda: nc.scalar.activation(out=dst_col, in_=dst_col, func=AF.Exp, scale=-0.5), r=[r_dst], w=[r_dst])

    eps_t, r_eps = sb1r("eps_t", [128, 2])
    S.op(V, lambda: nc.vector.memset(eps_t[:, 0:1], EPS), w=[r_eps])
    S.op(V, lambda: nc.vector.memset(eps_t[:, 1:2], GN_EPS), w=[r_eps])

    esKV = ExitStack()
    KT = esKV.enter_context(nc.sbuf_tensor("KT", [128, 4, T], BF16))
    r_KT = [R(f"KT{i}") for i in range(NT)]
    Vt = esKV.enter_context(nc.sbuf_tensor("Vt", [128, NT, 4, 129], BF16))
    r_V = [R(f"V{i}") for i in range(NT)]
    S.op(P, lambda: nc.gpsimd.memset(Vt[:].rearrange("p a b c -> p (a b c)"), 1.0), w=r_V)
    out_res = []
    if KCUT == 3:
        barrier(R.ALL, 'end')
        return nc, es


    ztile, r_ztile = xt[1]
    S.op(V, lambda: nc.vector.memset(ztile[:], 0.0), w=[r_ztile])
    r_ytrash = R("ytrash")
    S.dma(SP, lambda: nc.sync.dma_start(out=Y_scr[TRASH:TRASH + 128, :], in_=ztile[:]), r=[r_ztile], w=[r_ytrash], key="S_ztile")
    S.dma(SP, lambda: nc.sync.dma_start(out=Y_scr[TRASH + 128:TRASH + 256, :], in_=ztile[:]), r=[r_ztile], w=[r_ytrash], key="S_ztile")
    zb, r_zb = h2b[0]
    S.op(V, lambda: nc.vector.memset(zb[:], 0.0), w=[r_zb])
    NXR = (TRASH + 256) // 128
    r_xscr = R("xscr")
    for a in range(0, NXR, 16):
        b = min(NXR, a + 16)
        S.dma(SP, lambda a=a, b=b: nc.sync.dma_start(
            out=X_scr[a * 128:b * 128, :].rearrange("(t p) d -> p t d", p=128),
            in_=zb[:].unsqueeze(1).broadcast_to([128, b - a, D])), r=[r_zb], w=[r_xscr], key="S_zero")
    S.op(V, lambda: nc.vector.tensor_copy(out=idx_all[:, NT, :], in_=ecr[:, 16:18]), r=[r_ecr], w=[r_idx])
    S.op(V, lambda: nc.vector.memset(wts_all[:, NT, :], 0.0), w=[r_wts])

    def route_tile(ti, x2_t, r_x2, a2ap, r_a2_, sh2ap, r_sh2_, M):
        h2b_t, r_h2b = h2b[ti % 2]
        rms_rstd(x2_t[0:M, :], r_x2, st4[0:M, 1:2], r_st4, D, EPS, M)
        S.op(V, lambda: nc.vector.scalar_tensor_tensor(out=h2f[0:M, :], in0=x2_t[0:M, :], scalar=st4[0:M, 1:2], in1=a2ap,
                                                       op0=ALU.mult, op1=ALU.mult), r=[r_x2, r_st4, r_a2_], w=[r_h2f])
        S.op(P, lambda: nc.gpsimd.tensor_tensor(out=h2f[0:M, :], in0=h2f[0:M, :], in1=sh2ap, op=ALU.add),
             r=[r_h2f, r_sh2_], w=[r_h2f])
        S.op(A, lambda: nc.scalar.copy(out=h2b_t[0:M, :], in_=h2f[0:M, :]), r=[r_h2f], w=[r_h2b])
        yield 1
        pgr, r_pgr = pX[0]
        for half in range(2):
            pg, r_pg = nextG()
            for kk in range(4):
                k = half * 4 + kk
                S.op(PE, lambda k=k, kk=kk, pg=pg: nc.tensor.transpose(pg[:, kk * 128:kk * 128 + M], h2f[0:M, k * 128:(k + 1) * 128],
                                                                       ident_f[0:M, 0:M]),
                     r=[r_h2f, r_ident_f], w=[r_pg], sig=(kk == 3))
            yield 1
            S.op(V, lambda pg=pg: nc.vector.tensor_copy(
                out=h2T[:, :, 0:M], in_=pg[:].rearrange("p (k t) -> p k t", k=4)[:, :, 0:M]),
                r=[r_pg], w=[r_h2T])
            yield 1
            for kk in range(4):
                k = half * 4 + kk
                S.op(PE, lambda k=k, kk=kk: nc.tensor.matmul(pgr[0:M, 0:20], h2T[:, kk, 0:M], wrt[:, k, :], start=(k == 0), stop=(k == 7)),
                     r=[r_h2T, r_wrt], w=[r_pgr], sig=(kk == 3))
        pg, r_pg = pgr, r_pgr
        yield 1
        q = rs_
        rr = [r_rs]

        def v(fn, r=(), w=()):
            S.op(V, fn, r=list(r) + rr, w=list(w) + rr)
        S.op(V, lambda pg=pg: nc.vector.tensor_tensor(out=rlg[0:M, :], in0=pg[0:M, 0:20], in1=brt_bc[0:M, :], op=ALU.add),
             r=[r_pg, r_brt], w=[r_rlg])
        lg = rlg[0:M, 0:4]
        v(lambda: nc.vector.tensor_reduce(out=q[0:M, 0:1], in_=lg, axis=AX.X, op=ALU.max), r=[r_rlg])
        v(lambda: nc.vector.tensor_scalar(out=q[0:M, 4:8], in0=lg, scalar1=q[0:M, 0:1], scalar2=None, op0=ALU.is_ge), r=[r_rlg])
        v(lambda: nc.vector.tensor_scalar(out=q[0:M, 1:2], in0=q[0:M, 0:1], scalar1=-1.0, scalar2=None, op0=ALU.mult))
        S.op(A, lambda: nc.scalar.activation(out=q[0:M, 8:12], in_=lg, func=AF.Exp, bias=q[0:M, 1:2], accum_out=q[0:M, 2:3]),
             r=[r_rlg, r_rs], w=[r_rs])
        v(lambda: nc.vector.reciprocal(out=q[0:M, 2:3], in_=q[0:M, 2:3]))
        v(lambda: nc.vector.tensor_tensor(out=q[0:M, 16:32].rearrange("p (g e) -> p g e", g=4),
                                          in0=rlg[0:M, 4:20].rearrange("p (g e) -> p g e", g=4),
                                          in1=q[0:M, 4:8].unsqueeze(2).broadcast_to([M, 4, 4]), op=ALU.mult), r=[r_rlg])
        v(lambda: nc.vector.tensor_reduce(out=q[0:M, 12:16], in_=q[0:M, 16:32].rearrange("p (g e) -> p e g", g=4), axis=AX.X, op=ALU.add))
        le = q[0:M, 12:16]
        v(lambda: nc.vector.tensor_reduce(out=q[0:M, 3:4], in_=le, axis=AX.X, op=ALU.max))
        v(lambda: nc.vector.tensor_scalar(out=q[0:M, 32:36], in0=le, scalar1=q[0:M, 3:4], scalar2=None, op0=ALU.is_ge))
        v(lambda: nc.vector.scalar_tensor_tensor(out=q[0:M, 36:40], in0=q[0:M, 32:36], scalar=-1e30, in1=le, op0=ALU.mult, op1=ALU.add))
        v(lambda: nc.vector.tensor_reduce(out=q[0:M, 40:41], in_=q[0:M, 36:40], axis=AX.X, op=ALU.max))
        v(lambda: nc.vector.tensor_scalar(out=q[0:M, 44:48], in0=q[0:M, 36:40], scalar1=q[0:M, 40:41], scalar2=None, op0=ALU.is_ge))
        v(lambda: nc.vector.tensor_scalar(out=q[0:M, 42:43], in0=q[0:M, 3:4], scalar1=-1.0, scalar2=None, op0=ALU.mult))
        S.op(A, lambda: nc.scalar.activation(out=q[0:M, 41:42], in_=q[0:M, 40:41], func=AF.Exp, bias=q[0:M, 42:43]),
             r=[r_rs], w=[r_rs])
        v(lambda: nc.vector.tensor_scalar(out=q[0:M, 48:49], in0=q[0:M, 41:42], scalar1=1.0, scalar2=None, op0=ALU.add))
        v(lambda: nc.vector.reciprocal(out=q[0:M, 48:49], in_=q[0:M, 48:49]))
        v(lambda: nc.vector.tensor_tensor(out=q[0:M, 49:50], in0=q[0:M, 41:42], in1=q[0:M, 48:49], op=ALU.mult))
        v(lambda: nc.vector.tensor_scalar(out=q[0:M, 48:50], in0=q[0:M, 48:50], scalar1=q[0:M, 2:3], scalar2=None, op0=ALU.mult))
        for c, off in ((0, 32), (1, 44)):
            S.op(V, lambda c=c, off=off: nc.vector.tensor_tensor(
                out=oh16[0:M, c, :].rearrange("p (g e) -> p g e", g=4),
                in0=q[0:M, 4:8].unsqueeze(2).broadcast_to([M, 4, 4]),
                in1=q[0:M, off:off + 4].unsqueeze(1).broadcast_to([M, 4, 4]), op=ALU.mult), r=[r_rs], w=[r_oh16])
        S.op(V, lambda: nc.vector.tensor_tensor(out=mb[0:M, :], in0=oh16[0:M, 0, :], in1=oh16[0:M, 1, :], op=ALU.add),
             r=[r_oh16], w=[r_mb])
        yield 1
        pc, r_pc = nextG()
        S.op(PE, lambda: nc.tensor.matmul(pc[0:M, 0:16], ut_b[0:M, 0:M], mb[0:M, :], start=True, stop=True),
             r=[r_ut, r_mb], w=[r_pc], sig=False)
        S.op(PE, lambda: nc.tensor.matmul(pc[:, 16:32], ones_b[0:M, :], mb[0:M, :], start=True, stop=True),
             r=[r_ones, r_mb], w=[r_pc])
        yield 1
        S.op(V, lambda: nc.vector.tensor_tensor(out=q[0:M, 50:66], in0=pc[0:M, 0:16], in1=base_bc[0:M, :], op=ALU.add),
             r=[r_pc, r_base, r_rs], w=[r_rs])
        v(lambda: nc.vector.tensor_tensor(out=q[0:M, 66:82], in0=q[0:M, 50:66], in1=ecr[0:M, 0:16], op=ALU.add), r=[r_ecr])
        S.op(V, lambda: nc.vector.tensor_tensor(out=base_bc[:], in0=base_bc[:], in1=pc[:, 16:32], op=ALU.add),
             r=[r_pc, r_base], w=[r_base])
        for c in range(2):
            S.op(V, lambda c=c: nc.vector.scalar_tensor_tensor(out=q[0:M, 82:98], in0=oh16[0:M, c, :], scalar=1.0, in1=q[0:M, 66:82],
                                                               op0=ALU.mult, op1=ALU.mult, accum_out=slotf[0:M, c:c + 1]),
                 r=[r_oh16, r_rs], w=[r_rs, r_slotf])
            S.op(V, lambda c=c: nc.vector.scalar_tensor_tensor(out=q[0:M, 82:98], in0=oh16[0:M, c, :], scalar=1.0, in1=q[0:M, 50:66],
                                                               op0=ALU.mult, op1=ALU.mult, accum_out=slotf[0:M, 2 + c:3 + c]),
                 r=[r_oh16, r_rs], w=[r_rs, r_slotf])
        sf = [r_slotf]
        S.op(V, lambda: nc.vector.tensor_scalar(out=slotf[0:M, 2:4], in0=slotf[0:M, 2:4], scalar1=float(CAP), scalar2=None, op0=ALU.is_lt),
             r=sf, w=sf)
        S.op(V, lambda: nc.vector.tensor_tensor(out=slotf[0:M, 0:2], in0=slotf[0:M, 0:2], in1=ecr[0:M, 16:18],
                                                op=ALU.subtract), r=sf + [r_ecr], w=sf)
        S.op(V, lambda: nc.vector.tensor_tensor(out=slotf[0:M, 0:2], in0=slotf[0:M, 0:2], in1=slotf[0:M, 2:4], op=ALU.mult), r=sf, w=sf)
        S.op(V, lambda: nc.vector.tensor_tensor(out=slotf[0:M, 0:2], in0=slotf[0:M, 0:2], in1=ecr[0:M, 16:18],
                                                op=ALU.add), r=sf + [r_ecr], w=sf)
        S.op(V, lambda: nc.vector.tensor_copy(out=idx_all[0:M, ti, :], in_=slotf[0:M, 0:2]), r=sf, w=[r_idx])
        S.op(V, lambda: nc.vector.tensor_tensor(out=wts_all[0:M, ti, :], in0=q[0:M, 48:50], in1=slotf[0:M, 2:4], op=ALU.mult),
             r=sf + [r_rs], w=[r_wts])
        for c in range(2):
            S.dma(P, lambda c=c: nc.gpsimd.indirect_dma_start(
                out=X_scr, out_offset=bass.IndirectOffsetOnAxis(ap=idx_all[:, ti, c:c + 1], axis=0),
                in_=h2b_t[:], in_offset=None), r=[r_h2b, r_idx], key=f"S_h2b{ti % 2}")

    smp = {}

    def tile_body(i, SMP=False):
        x_t, r_x = xt[i % 2]
        od, r_od = odr[i % 2]
        smp['od'], smp['r_od'] = od, r_od
        qrT, r_qrT = qrT_r[i % 2]
        qrTd, r_qrTd = qrTd_r[i % 2]
        krb, r_krb = krb_r[i % 2]
        krd, r_krd = krd_r[i % 2]
        krT, r_krT = krT_r[i % 2]
        vrb, r_vrb = vrb_r[i % 2]
        sg, r_sg = sg_r[i % 2]
        smp.update(qrT=qrT, r_qrT=r_qrT, krb=krb, r_krb=r_krb, vrb=vrb, r_vrb=r_vrb)
        r_t, r_r = rt[i % 2]
        tok = slice(i * 128, (i + 1) * 128)
        if SMP:
            S.op(V, lambda: nc.vector.memset(x_t[:], 0.0), w=[r_x])
            S.op(V, lambda: nc.vector.memset(r_t[:], 0.0), w=[r_r])
            S.dma(SP, lambda: nc.sync.dma_start(out=x_t[0:16, :], in_=xs), w=[r_x])
            S.dma(SP, lambda: nc.sync.dma_start(out=r_t[0:16, :], in_=c_rope_s), w=[r_r])
        else:
            S.dma(SP, lambda: nc.sync.dma_start(out=x_t[:], in_=xp[tok, :]), w=[r_x])
            S.dma(SP, lambda: nc.sync.dma_start(out=r_t[:], in_=c_rope_p[tok, :]), w=[r_r])
        rms_rstd(x_t[:], r_x, st4[:, 0:1], r_st4, D, EPS)
        if SMP:
            S.op(V, lambda: nc.vector.scalar_tensor_tensor(out=tt[:], in0=x_t[:], scalar=st4[:, 0:1], in1=smp["mA"][:, 0, :],
                                                           op0=ALU.mult, op1=ALU.mult), r=[r_x, r_st4, smp["r_mA"]], w=[r_tt])
            S.op(V, lambda: nc.vector.tensor_tensor(out=xn[:], in0=tt[:], in1=smp["mA"][:, 1, :], op=ALU.add),
                 r=[r_tt, smp["r_mA"]], w=[r_xn])
        else:
            S.op(V, lambda: nc.vector.tensor_scalar(out=xn[:], in0=x_t[:], scalar1=st4[:, 0:1], scalar2=None, op0=ALU.mult),
                 r=[r_x, r_st4], w=[r_xn])
        pt, r_pt = pT[0]
        for k in range(8):
            S.op(PE, lambda k=k: nc.tensor.transpose(pt[:, k * 128:(k + 1) * 128], xn[:, k * 128:(k + 1) * 128], ident_b[:]),
                 r=[r_xn, r_ident_b], w=[r_pt], sig=(k == 7))
        if SMP:
            S.op(A, lambda: nc.scalar.copy(out=hT[:].rearrange("p k t -> p (k t)"), in_=pt[:]), r=[r_pt], w=[r_hT])
        else:
            for k in range(8):
                S.op(A, lambda k=k: nc.scalar.activation(out=hT[:, k, :], in_=pt[:, k * 128:(k + 1) * 128], func=AF.Identity,
                                                         scale=a1_fm[:, k:k + 1], bias=sh1_fm[:, k:k + 1]),
                     r=[r_pt, r_a1fm, r_sh1fm], w=[r_hT])
        if KCUT == 4:
            return
        yield 1
        q_t, r_q = QT[i % 2]
        k_f, r_kf = kf[0]
        v_f, r_vf = vf[0]
        for g in range(int(os.environ.get('KG', '7'))):
            pg, r_pg = nextG()
            for k in range(8):
                S.op(PE, lambda k=k, g=g, pg=pg: nc.tensor.matmul(pg[:], hT[:, k, :], win[:, k, g * 512:(g + 1) * 512],
                                                                  start=(k == 0), stop=(k == 7)),
                     r=[r_hT, r_win], w=[r_pg], sig=(k == 7))
            yield 1
            if g == 0:
                rope(pg, r_pg, 8, r_t, r_r, 0, qb[:], r_qb)
                yield 1
                transpose4(qb, r_qb, q_t[:].rearrange("p c t -> p (c t)"), r_q)
            elif g == 1:
                rope(pg, r_pg, 8, r_t, r_r, 0, k_f[:], r_kf)
                if SMP:
                    S.dma(SP, lambda: nc.sync.dma_start(out=k_s, in_=k_f[0:16, :]), r=[r_kf])
                else:
                    S.dma(SP, lambda: nc.sync.dma_start(out=k_p[tok, :], in_=k_f[:]), r=[r_kf])
                S.op(A, lambda: nc.scalar.copy(out=kb[:], in_=k_f[:]), r=[r_kf], w=[r_kb])
                yield 1
                if SMP:
                    transpose4(kb, r_kb, smp["KTs"][:].rearrange("p c t -> p (c t)"), smp["r_KTs"])
                else:
                    transpose4(kb, r_kb, KT[:, :, tok], r_KT[i])
            elif g == 2:
                S.op(A, lambda pg=pg: nc.scalar.copy(out=v_f[:], in_=pg[:]), r=[r_pg], w=[r_vf])
                if SMP:
                    S.op(V, lambda pg=pg: nc.vector.tensor_copy(out=smp["Vs"][:], in_=pg[:]), r=[r_pg], w=[smp["r_Vs"]])
                    S.dma(SP, lambda: nc.sync.dma_start(out=v_s, in_=v_f[0:16, :]), r=[r_vf])
                else:
                    S.op(V, lambda pg=pg: nc.vector.tensor_copy(out=Vt[:, i, :, 0:128], in_=pg[:].rearrange("p (h e) -> p h e", h=4)),
                         r=[r_pg], w=[r_V[i]])
                    S.dma(SP, lambda: nc.sync.dma_start(out=v_p[tok, :], in_=v_f[:]), r=[r_vf])
            elif g == 3:
                rope(pg, r_pg, 4, r_t, r_r, 128, qrb[:], r_qrb)
                yield 1
                transpose4(qrb, r_qrb, qrT[:].rearrange("p c t -> p (c t)"), r_qrT)
                S.op(P, lambda: nc.gpsimd.tensor_tensor(out=qrTd[:].rearrange("p c t -> p (c t)"),
                                                        in0=qrT[:].rearrange("p c t -> p (c t)"), in1=qdec[:], op=ALU.mult),
                     r=[r_qrT, r_qdec], w=[r_qrTd])
            elif g == 4:
                rope(pg, r_pg, 4, r_t, r_r, 128, krb[:].rearrange("p h d -> p (h d)"), r_krb)
                yield 1
                transpose4(krb[:].rearrange("p h d -> p (h d)"), r_krb, krT[:].rearrange("p c t -> p (c t)"), r_krT)
                S.op(P, lambda: nc.gpsimd.tensor_tensor(out=krd[:], in0=krb[:],
                                                        in1=kdec[:].unsqueeze(2).broadcast_to([128, 4, 128]), op=ALU.mult),
                     r=[r_krb, r_kdec], w=[r_krd])
            elif g == 5:
                S.op(A, lambda pg=pg: nc.scalar.copy(out=vrb[:].rearrange("p h d -> p (h d)"), in_=pg[:]), r=[r_pg], w=[r_vrb])
            else:
                S.op(A, lambda pg=pg: nc.scalar.activation(out=t1, in_=pg[:], func=AF.Exp, scale=-1.0), r=[r_pg], w=[r_t1])
                S.op(V, lambda: nc.vector.tensor_scalar(out=t1, in0=t1, scalar1=1.0, scalar2=None, op0=ALU.add), r=[r_t1], w=[r_t1])
                S.op(V, lambda: nc.vector.reciprocal(out=t1, in_=t1), r=[r_t1], w=[r_t1])
                S.op(V, lambda pg=pg: nc.vector.tensor_tensor(out=sg[:], in0=pg[:], in1=t1, op=ALU.mult), r=[r_pg, r_t1], w=[r_sg])
        if KCUT == 5:
            return
            yield 1
        yield 'ATTN'
        units = []
        for j in range(8):
            blocks = list(range(i + 1))
            grp = [blocks[a:a + 4] for a in range(0, len(blocks), 4)]
            for gi, gb in enumerate(grp):
                units.append((j, gi, gb, gi == 0, gi == len(grp) - 1))

        def emit_qk(u, ui):
            j, gi, gb, first, last = u
            h, s = j // 2, j % 2
            psl = slice(64 * s, 64 * s + 64)
            pS_t, r_pS = pS[ui % 2]
            for bi, kbk in enumerate(gb):
                S.op(PE, lambda bi=bi, kbk=kbk: nc.tensor.matmul(
                    pS_t[:, bi * 128:(bi + 1) * 128], KT[psl, h, kbk * 128:(kbk + 1) * 128], q_t[psl, h, :],
                    start=True, stop=True),
                    r=[r_KT[kbk], r_q], w=[r_pS], sig=(bi == len(gb) - 1))

        def emit_av(u, ui):
            j, gi, gb, first, last = u
            h, s = j // 2, j % 2
            pS_t, r_pS = pS[ui % 2]
            P_t, r_P = PTr[ui % 2]
            n = len(gb) * 128
            S.op(A, lambda: nc.scalar.activation(out=P_t[:, 0:n], in_=pS_t[:, 0:n], func=AF.Exp, scale=0.125),
                 r=[r_pS], w=[r_P])
            if gb[-1] == i:
                S.op(V, lambda: nc.vector.tensor_tensor(out=P_t[:, n - 128:n], in0=P_t[:, n - 128:n], in1=mask_b[:], op=ALU.mult),
                     r=[r_P, r_mask], w=[r_P])
            reg = 0
            po, r_po = (pO if j % 2 == 0 else pX[1])
            for bi, kbk in enumerate(gb):
                S.op(PE, lambda bi=bi, kbk=kbk: nc.tensor.matmul(
                    po[:, reg * 129:reg * 129 + 129], P_t[:, bi * 128:(bi + 1) * 128], Vt[:, kbk, h, :],
                    start=(first and bi == 0), stop=(last and bi == len(gb) - 1)),
                    r=[r_P, r_V[kbk]], w=[r_po], sig=(bi == len(gb) - 1))
            if last:
                c0 = reg * 129
                S.op(V, lambda: nc.vector.reciprocal(out=rl[:, s:s + 1], in_=po[:, c0 + 128:c0 + 129]), r=[r_po], w=[r_rl])
                if s == 0:
                    S.op(V, lambda: nc.vector.tensor_scalar(out=od[:, h, :], in0=po[:, c0:c0 + 128], scalar1=rl[:, 0:1],
                                                            scalar2=None, op0=ALU.mult), r=[r_po, r_rl], w=[r_od])
                else:
                    S.op(V, lambda: nc.vector.tensor_scalar(out=rl[:, 1:2], in0=rl[:, 1:2], scalar1=neglam[:, 0:1],
                                                            scalar2=None, op0=ALU.mult), r=[r_rl, r_neglam], w=[r_rl])
                    S.op(V, lambda: nc.vector.scalar_tensor_tensor(out=od[:, h, :], in0=po[:, c0:c0 + 128], scalar=rl[:, 1:2],
                                                                   in1=od[:, h, :], op0=ALU.mult, op1=ALU.add),
                         r=[r_po, r_rl, r_od], w=[r_od])

        if SMP:
            units = []
            yield 'SATT'
            yield from sample_attention(q_t, r_q)
            yield 'SATT_END'
        elif STAGE < 1:
            units = []
            S.op(V, lambda: nc.vector.memset(od[:].rearrange('p a b -> p (a b)'), 0.5), w=[r_od])
        else:
            emit_qk(units[0], 0)
        for ui, u in enumerate(units):
            if ui + 1 < len(units):
                emit_qk(units[ui + 1], ui + 1)
            emit_av(u, ui)
            yield 1

        yield 'TAIL'
        for h in range(4):
            S.op(A, lambda h=h: nc.scalar.activation(out=junk[:, 0:128], in_=od[:, h, :], func=AF.Square,
                                                     accum_out=st4[:, 4 + h:5 + h]), r=[r_od], w=[r_junk, r_st4])
        c08 = (1.0 - LAM_INIT) ** 2
        S.op(A, lambda: nc.scalar.activation(out=st4[:, 4:8], in_=st4[:, 4:8], func=AF.Ln, scale=1.0 / (128 * c08),
                                             bias=eps_t[:, 0:1]), r=[r_st4, r_eps], w=[r_st4])
        S.op(A, lambda: nc.scalar.activation(out=st4[:, 4:8], in_=st4[:, 4:8], func=AF.Exp, scale=-0.5), r=[r_st4], w=[r_st4])
        for h in range(4):
            S.op(V, lambda h=h: nc.vector.tensor_scalar(out=ymix[:, h * 128:(h + 1) * 128], in0=od[:, h, :],
                                                        scalar1=st4[:, 4 + h:5 + h], scalar2=None, op0=ALU.mult),
                 r=[r_od, r_st4], w=[r_ymix])

        if KCUT == 6:
            return
        yield 1
        pg, r_pg = nextG()
        for h in range(4):
            S.op(PE, lambda h=h, pg=pg: nc.tensor.matmul(pg[:, h * 128:(h + 1) * 128], krT[:, h, :], qrT[:, h, :],
                                                         start=True, stop=True),
                 r=[r_krT, r_qrT], w=[r_pg], sig=(h == 3))
        yield 1
        dect_u, r_dect_u = (smp["dect_s"], smp["r_dect_s"]) if SMP else (dect, r_dect)
        S.op(V, lambda pg=pg: nc.vector.tensor_tensor(out=sTm[:].rearrange("p h t -> p (h t)"), in0=pg[:], in1=dect_u[:], op=ALU.mult),
             r=[r_pg, r_dect_u], w=[r_sTm])
        yield 1
        po_, r_po_ = nextG()
        if SMP:
            sample_ret_cross(po_, r_po_)
        else:
            for h in range(4):
                S.op(PE, lambda h=h, po_=po_: nc.tensor.matmul(po_[:, h * 128:(h + 1) * 128], sTm[:, h, :], vrb[:, h, :],
                                                               start=True, stop=False),
                     r=[r_sTm, r_vrb], w=[r_po_], sig=False)
                S.op(PE, lambda h=h, po_=po_: nc.tensor.matmul(po_[:, h * 128:(h + 1) * 128], qrTd[:, h, :], rbf[:, h, :],
                                                               start=False, stop=True),
                     r=[r_qrTd, r_rbf], w=[r_po_], sig=(h == 3))
            pk, r_pk = pX[0]
            for h in range(4):
                S.op(PE, lambda h=h: nc.tensor.matmul(pk[:, h * 128:(h + 1) * 128], krd[:, h, :], vrb[:, h, :],
                                                      start=True, stop=True),
                     r=[r_krd, r_vrb], w=[r_pk], sig=(h == 3))
            for h in range(4):
                cd = (1.0 - 2.0 ** (-5.0 - h)) ** 128
                S.op(V, lambda h=h, cd=cd: nc.vector.scalar_tensor_tensor(out=rst[:, h, :], in0=rst[:, h, :], scalar=float(cd),
                                                                          in1=pk[:, h * 128:(h + 1) * 128], op0=ALU.mult, op1=ALU.add),
                     r=[r_rst, r_pk], w=[r_rst])
            S.op(A, lambda: nc.scalar.copy(out=rbf[:], in_=rst[:]), r=[r_rst], w=[r_rbf])
        yield 1
        S.op(V, lambda po_=po_: nc.vector.tensor_reduce(out=rs_[:, 0:4], in_=po_[:].rearrange("p (h e) -> p h e", h=4),
                                                        axis=AX.X, op=ALU.add), r=[r_po_], w=[r_rs])
        S.op(A, lambda po_=po_: nc.scalar.activation(out=t1, in_=po_[:], func=AF.Square), r=[r_po_], w=[r_t1])
        S.op(V, lambda: nc.vector.tensor_reduce(out=rs_[:, 4:8], in_=t1.rearrange("p (h e) -> p h e", h=4),
                                                axis=AX.X, op=ALU.add), r=[r_t1], w=[r_rs])
        S.op(V, lambda: nc.vector.tensor_scalar(out=rs_[:, 0:4], in0=rs_[:, 0:4], scalar1=1.0 / 128, scalar2=None, op0=ALU.mult),
             r=[r_rs], w=[r_rs])
        S.op(V, lambda: nc.vector.tensor_tensor(out=rs_[:, 8:12], in0=rs_[:, 0:4], in1=rs_[:, 0:4], op=ALU.mult), r=[r_rs], w=[r_rs])
        S.op(V, lambda: nc.vector.scalar_tensor_tensor(out=rs_[:, 4:8], in0=rs_[:, 4:8], scalar=1.0 / 128, in1=rs_[:, 8:12],
                                                       op0=ALU.mult, op1=ALU.subtract), r=[r_rs], w=[r_rs])
        S.op(A, lambda: nc.scalar.activation(out=rs_[:, 4:8], in_=rs_[:, 4:8], func=AF.Ln, bias=eps_t[:, 1:2]),
             r=[r_rs, r_eps], w=[r_rs])
        S.op(A, lambda: nc.scalar.activation(out=rs_[:, 4:8], in_=rs_[:, 4:8], func=AF.Exp, scale=-0.5), r=[r_rs], w=[r_rs])
        for h in range(4):
            S.op(V, lambda h=h, po_=po_: nc.vector.tensor_scalar(out=t2[:, h * 128:(h + 1) * 128], in0=po_[:, h * 128:(h + 1) * 128],
                                                                 scalar1=rs_[:, h:h + 1], scalar2=rs_[:, 4 + h:5 + h],
                                                                 op0=ALU.subtract, op1=ALU.mult),
                 r=[r_po_, r_rs], w=[r_t2])
        S.op(V, lambda: nc.vector.tensor_tensor(out=ymix[:, 512:1024], in0=t2, in1=sg[:], op=ALU.mult),
             r=[r_t2, r_sg], w=[r_ymix])

        if KCUT == 7:
            return
        yield 1
        pt, r_pt = pT[0]
        for k in range(8):
            S.op(PE, lambda k=k: nc.tensor.transpose(pt[:, k * 128:(k + 1) * 128], ymix[:, k * 128:(k + 1) * 128], ident_b[:]),
                 r=[r_ymix, r_ident_b], w=[r_pt], sig=(k == 7))
        S.op(A, lambda: nc.scalar.copy(out=yT[:].rearrange("p k t -> p (k t)"), in_=pt[:]), r=[r_pt], w=[r_yT])
        yield 1
        x2_t, r_x2 = x_t, r_x
        for n in range(2):
            pg, r_pg = nextG()
            for k in range(8):
                S.op(PE, lambda k=k, n=n, pg=pg: nc.tensor.matmul(pg[:], yT[:, k, :], wout[:, k, n * 512:(n + 1) * 512],
                                                                  start=(k == 0), stop=(k == 7)),
                     r=[r_yT, r_wout], w=[r_pg], sig=(k == 7))
            yield 1
            cs_ = slice(n * 512, (n + 1) * 512)
            if SMP:
                S.op(V, lambda pg=pg, cs_=cs_: nc.vector.tensor_tensor(out=tt[:, cs_], in0=pg[:], in1=smp["gt1s"][:, cs_], op=ALU.mult),
                     r=[r_pg, smp["r_gt1s"]], w=[r_tt])
                S.op(V, lambda cs_=cs_: nc.vector.tensor_tensor(out=x2_t[:, cs_], in0=tt[:, cs_], in1=x_t[:, cs_], op=ALU.add),
                     r=[r_tt, r_x], w=[r_x2])
            else:
                S.op(V, lambda pg=pg, cs_=cs_: nc.vector.tensor_tensor(out=x2_t[:, cs_], in0=pg[:], in1=x_t[:, cs_], op=ALU.add),
                     r=[r_pg, r_x], w=[r_x2])
        if SMP:
            S.dma(SP, lambda: nc.sync.dma_start(out=x2_scr[i * 128:i * 128 + 16, :], in_=x2_t[0:16, :]), r=[r_x2], key=f"S_xt{i % 2}")
        else:
            S.dma(SP, lambda: nc.sync.dma_start(out=x2_scr[tok, :], in_=x2_t[:]), r=[r_x2], key=f"S_xt{i % 2}")

        if STAGE >= 2:
            if SMP:
                smp["load_mA"]((4, 3))
                yield from route_tile(i, x2_t, r_x2, smp["mA"][0:16, 0, :], smp["r_mA"], smp["mA"][0:16, 1, :], smp["r_mA"], 16)
            elif not DEFER:
                yield from route_tile(i, x2_t, r_x2, a2_bc[:], r_a2, sh2_bc[:], r_sh2, 128)

    def run_until(gen, marker):
        for v in gen:
            if v == marker:
                return True
        return False

    def step_bg(bg):
        while bg:
            gen, marker = bg[0]
            try:
                v = next(gen)
            except StopIteration:
                bg.pop(0)
                continue
            if marker is not None and v == marker:
                bg.pop(0)
                continue
            return

    if KCUT < 99 or os.environ.get("KSEQ"):
        for i_ in range(NT):
            run_until(tile_body(i_), None)
    else:
        cur = tile_body(0)
        run_until(cur, "ATTN")
        prev_tail = None
        for i_ in range(NT):
            nxt = tile_body(i_ + 1) if i_ + 1 < NT else None
            bg = []
            if prev_tail is not None:
                bg.append((prev_tail, None))
            if nxt is not None:
                bg.append((nxt, "ATTN"))
            n_units = 8 * ((i_ + 4) // 4)
            kk = max(1, -(-44 // n_units))
            while True:
                v = next(cur)
                if v == "TAIL":
                    break
                for _ in range(kk):
                    step_bg(bg)
            while bg:
                step_bg(bg)
            prev_tail = cur
            cur = nxt
        run_until(prev_tail, None)

    if KCUT < 99:
        barrier(R.ALL, 'end')
        return nc, es
    S.dma(SP, lambda: nc.sync.dma_start(out=ret_p.rearrange("h d e -> d h e"), in_=rst[:]), r=[r_rst])

    barrier(r_KT + r_V, "kv")
    esKV.close()
    esS = ExitStack()
    s_res = []

    def sbs(name, shape, dt=F32):
        t = esS.enter_context(nc.sbuf_tensor(name, list(shape), dt))
        r = R(name)
        s_res.append(r)
        return t, r

    mA, r_mA = sbs("mA", [128, 2, D])
    gt1s, r_gt1s = sbs("gt1s", [128, D])
    KTs, r_KTs = sbs("KTs", [128, 4, 128], BF16)
    Vs, r_Vs = sbs("Vs", [128, 512], BF16)
    Qb, r_Qb = sbs("Qb", [128, 4, 4, 8], BF16)
    pti, r_pti = sbs("pti", [128, 4 * NPG], I32)
    idxp, r_idxp = sbs("idxp", [128, 4 * NPG], I32)
    pcol, r_pcol = sbs("pcol", [128, 1])
    KVpg = [sbs(f"KVpg{i}", [128, 1024]) for i in range(3)]
    Kb = [sbs(f"Kb{i}", [128, 512], BF16) for i in range(2)]
    KTp = [sbs(f"KTp{i}", [128, 512], BF16) for i in range(2)]
    Vb = [sbs(f"Vb{i}", [128, 512], BF16) for i in range(2)]
    PTd = [sbs(f"PTd{i}", [128, 32], BF16) for i in range(2)]
    Osc, r_Osc = sbs("Osc", [32, 4, 512], BF16)
    sel, r_sel = sbs("sel", [32, 256], BF16)
    smk, r_smk = sbs("smk", [32, 2])
    rl32, r_rl32 = sbs("rl32", [32, 2])
    rstS, r_rstS = sbs("rstS", [128, 4, 4, 128])
    rbS, r_rbS = sbs("rbS", [128, 4, 4, 128], BF16)
    dect_s, r_dect_s = sbs("dect_s", [128, 512])
    qdec_s, r_qdec_s = sbs("qdec_s", [128, 4, 4, 128], BF16)
    kdec_s, r_kdec_s = sbs("kdec_s", [128, 16])
    qrTdm, r_qrTdm = sbs("qrTdm", [128, 4, 4, 128], BF16)
    krdS, r_krdS = sbs("krdS", [128, 4, 128], BF16)
    mnew, r_mnew = sbs("mnew", [128, 4, 32])
    smp.update(mA=mA, r_mA=r_mA, gt1s=gt1s, r_gt1s=r_gt1s, KTs=KTs, r_KTs=r_KTs, Vs=Vs, r_Vs=r_Vs,
               dect_s=dect_s, r_dect_s=r_dect_s)
    S.op(V, lambda: nc.vector.memset(mA[:].rearrange("p a d -> p (a d)"), 0.0), w=[r_mA])
    S.op(V, lambda: nc.vector.memset(gt1s[:], 0.0), w=[r_gt1s])
    def load_mA(chs):
        for j, ch in enumerate(chs):
            S.dma(SP, lambda j=j, ch=ch: nc.sync.dma_start(out=mA[0:16, j, :], in_=mods_scr[:, ch * D:(ch + 1) * D]),
                  r=[r_modsscr], w=[r_mA])
    load_mA((1, 0))
    smp["load_mA"] = load_mA
    S.dma(SP, lambda: nc.sync.dma_start(out=gt1s[0:16, :], in_=mods_scr[:, 2 * D:3 * D]), r=[r_modsscr], w=[r_gt1s])
    S.dma(SP, lambda: nc.sync.dma_start(out=dect_s[:], in_=c_dect_s), w=[r_dect_s])
    S.dma(P, lambda: nc.gpsimd.dma_start(out=qdec_s[:].rearrange("p a b c -> p (a b c)"), in_=c_qdec_s), w=[r_qdec_s])
    S.dma(SP, lambda: nc.sync.dma_start(out=kdec_s[:], in_=c_kdec_s), w=[r_kdec_s])
    S.dma(SP, lambda: nc.sync.dma_start(out=mnew[:].rearrange("p a b -> p (a b)"), in_=c_masknew), w=[r_mnew])
    S.dma(P, lambda: nc.gpsimd.dma_start(out=sel[:], in_=c_sel), w=[r_sel])
    S.dma(SP, lambda: nc.sync.dma_start(out=smk[:], in_=c_smask), w=[r_smk])
    S.dma(SP, lambda: nc.sync.dma_start(out=pcol[:], in_=c_pcol), w=[r_pcol])
    S.dma(SP, lambda: nc.sync.dma_start(out=pti[:], in_=ptab.partition_broadcast(128)), w=[r_pti])
    S.dma(SP, lambda: nc.sync.dma_start(out=rstS[:], in_=st_ret.rearrange("b h d e -> d b h e")), w=[r_rstS])
    S.op(A, lambda: nc.scalar.copy(out=rbS[:].rearrange("p a b c -> p (a b c)"), in_=rstS[:].rearrange("p a b c -> p (a b c)")),
         r=[r_rstS], w=[r_rbS])
    S.op(V, lambda: nc.vector.tensor_scalar(out=idxp[:], in0=pti[:], scalar1=128.0, scalar2=pcol[:, 0:1], op0=ALU.mult, op1=ALU.add),
         r=[r_pti, r_pcol], w=[r_idxp])
    S.dma(P, lambda: nc.gpsimd.dma_start(out=wout[:], in_=w_out.rearrange("(k p) n -> p k n", p=128)), w=[r_wout])
    for k in range(8):
        S.op(V, lambda k=k: nc.vector.tensor_scalar(out=wout[:, k, :], in0=wout[:, k, :], scalar1=beta_fm[:, k:k + 1], scalar2=None,
                                                    op0=ALU.mult), r=[r_wout, r_betafm], w=[r_wout])
    S.op(V, lambda: nc.vector.scalar_tensor_tensor(out=rl32[:, 1:2], in0=smk[:, 1:2], scalar=neglam[0:32, 0:1], in1=smk[:, 0:1],
                                                   op0=ALU.mult, op1=ALU.add), r=[r_smk, r_neglam], w=[r_rl32])

    def sample_attention(q_t, r_q):
        S.op(V, lambda: nc.vector.memset(Qb[:].rearrange("p a b c -> p (a b c)"), 0.0), w=[r_Qb])
        for b in range(4):
            for s_ in range(2):
                psl = slice(64 * s_, 64 * s_ + 64)
                S.op(V, lambda b=b, s_=s_, psl=psl: nc.vector.tensor_copy(out=Qb[psl, b, :, 4 * s_:4 * s_ + 4],
                                                                          in_=q_t[psl, :, 4 * b:4 * b + 4]),
                     r=[r_q], w=[r_Qb])
        po, r_po = pO
        psm, r_psm = pX[1]
        cc_ = 0
        for b in range(4):
            for pg_ in range(NPG):
                KVg, r_Kg = KVpg[cc_ % 3]
                r_Vg = r_Kg
                Kb_t, r_Kb = Kb[cc_ % 2]
                KT_t, r_KTp = KTp[cc_ % 2]
                Vb_t, r_Vb = Vb[cc_ % 2]
                P_t, r_P = PTd[cc_ % 2]
                pS_t, r_pS = pS[cc_ % 2]
                cc_ += 1
                col = b * NPG + pg_
                S.dma(P, lambda KVg=KVg, col=col: nc.gpsimd.indirect_dma_start(
                    out=KVg[:], out_offset=None, in_=ckv, in_offset=bass.IndirectOffsetOnAxis(ap=idxp[:, col:col + 1], axis=0)),
                    r=[r_idxp], w=[r_Kg])
                S.op(A, lambda KVg=KVg, Kb_t=Kb_t: nc.scalar.copy(out=Kb_t[:], in_=KVg[:, 0:512]), r=[r_Kg], w=[r_Kb])
                S.op(V, lambda KVg=KVg, Vb_t=Vb_t: nc.vector.tensor_copy(out=Vb_t[:], in_=KVg[:, 512:1024]), r=[r_Vg], w=[r_Vb])
                pt, r_pt = pT[0]
                for c in range(4):
                    S.op(PE, lambda c=c, Kb_t=Kb_t: nc.tensor.transpose(pt[:, c * 128:(c + 1) * 128], Kb_t[:, c * 128:(c + 1) * 128], ident_b[:]),
                         r=[r_Kb, r_ident_b], w=[r_pt], sig=(c == 3))
                S.op(V, lambda KT_t=KT_t: nc.vector.tensor_copy(out=KT_t[:], in_=pt[:, 0:512]), r=[r_pt], w=[r_KTp])
                for h in range(4):
                    S.op(PE, lambda h=h, KT_t=KT_t, pS_t=pS_t, b=b: nc.tensor.matmul(
                        pS_t[:, h * 8:(h + 1) * 8], KT_t[:, h * 128:(h + 1) * 128], Qb[:, b, h, :], start=True, stop=True),
                        r=[r_KTp, r_Qb], w=[r_pS], sig=(h == 3))
                S.op(A, lambda P_t=P_t, pS_t=pS_t: nc.scalar.activation(out=P_t[:], in_=pS_t[:, 0:32], func=AF.Exp, scale=0.125),
                     r=[r_pS], w=[r_P])
                S.op(PE, lambda P_t=P_t, Vb_t=Vb_t, pg_=pg_: nc.tensor.matmul(po[0:32, :], P_t[:], Vb_t[:], start=(pg_ == 0), stop=False),
                     r=[r_P, r_Vb], w=[r_po], sig=False)
                S.op(PE, lambda P_t=P_t, pg_=pg_: nc.tensor.matmul(psm[0:32, 0:1], P_t[:], ones_b[:, 0:1], start=(pg_ == 0), stop=False),
                     r=[r_P, r_ones], w=[r_psm])
                yield 1
            P_t, r_P = PTd[cc_ % 2]
            pS_t, r_pS = pS[cc_ % 2]
            cc_ += 1
            for h in range(4):
                S.op(PE, lambda h=h, pS_t=pS_t, b=b: nc.tensor.matmul(
                    pS_t[0:16, h * 8:(h + 1) * 8], KTs[:, h, 0:16], Qb[:, b, h, :], start=True, stop=True),
                    r=[r_KTs, r_Qb], w=[r_pS], sig=(h == 3))
            S.op(A, lambda P_t=P_t, pS_t=pS_t: nc.scalar.activation(out=P_t[0:16, :], in_=pS_t[0:16, 0:32], func=AF.Exp, scale=0.125),
                 r=[r_pS], w=[r_P])
            S.op(V, lambda P_t=P_t, b=b: nc.vector.tensor_tensor(out=P_t[0:16, :], in0=P_t[0:16, :], in1=mnew[0:16, b, :], op=ALU.mult),
                 r=[r_P, r_mnew], w=[r_P])
            S.op(PE, lambda P_t=P_t: nc.tensor.matmul(po[0:32, :], P_t[0:16, :], Vs[0:16, :], start=False, stop=True),
                 r=[r_P, r_Vs], w=[r_po], sig=False)
            S.op(PE, lambda P_t=P_t: nc.tensor.matmul(psm[0:32, 0:1], P_t[0:16, :], ones_b[0:16, 0:1], start=False, stop=True),
                 r=[r_P, r_ones], w=[r_psm])
            S.op(V, lambda: nc.vector.reciprocal(out=rl32[:, 0:1], in_=psm[0:32, 0:1]), r=[r_psm], w=[r_rl32])
            S.op(V, lambda: nc.vector.tensor_tensor(out=rl32[:, 0:1], in0=rl32[:, 0:1], in1=rl32[:, 1:2], op=ALU.mult),
                 r=[r_rl32], w=[r_rl32])
            S.op(V, lambda b=b: nc.vector.tensor_scalar(out=Osc[:, b, :], in0=po[0:32, :], scalar1=rl32[:, 0:1], scalar2=None, op0=ALU.mult),
                 r=[r_po, r_rl32], w=[r_Osc])
        pg2, r_pg2 = nextG()
        for h in range(4):
            for b in range(4):
                S.op(PE, lambda h=h, b=b: nc.tensor.matmul(
                    pg2[0:16, h * 128:(h + 1) * 128], sel[:, (b * 4 + h) * 16:(b * 4 + h + 1) * 16], Osc[:, b, h * 128:(h + 1) * 128],
                    start=(b == 0), stop=(b == 3)),
                    r=[r_sel, r_Osc], w=[r_pg2], sig=(b == 3 and h == 3))
        S.op(V, lambda: nc.vector.tensor_copy(out=smp["od"][0:16, :, :].rearrange("p h e -> p (h e)"), in_=pg2[0:16, :]), r=[r_pg2], w=[smp["r_od"]])

    def sample_ret_cross(po_, r_po_):
        qrT, r_qrT, krb, r_krb, vrb, r_vrb = smp['qrT'], smp['r_qrT'], smp['krb'], smp['r_krb'], smp['vrb'], smp['r_vrb']
        for b in range(4):
            S.op(V, lambda b=b: nc.vector.tensor_tensor(out=qrTdm[:, b, :, :], in0=qrT[:], in1=qdec_s[:, b, :, :], op=ALU.mult),
                 r=[r_qrT, r_qdec_s], w=[r_qrTdm])
        for h in range(4):
            S.op(PE, lambda h=h: nc.tensor.matmul(po_[:, h * 128:(h + 1) * 128], sTm[:, h, :], vrb[:, h, :], start=True, stop=False),
                 r=[r_sTm, r_vrb], w=[r_po_], sig=False)
            for b in range(4):
                S.op(PE, lambda h=h, b=b: nc.tensor.matmul(po_[:, h * 128:(h + 1) * 128], qrTdm[:, b, h, :], rbS[:, b, h, :],
                                                           start=False, stop=(b == 3)),
                     r=[r_qrTdm, r_rbS], w=[r_po_], sig=(h == 3 and b == 3))
        g4 = [(1.0 - 2.0 ** (-5.0 - h)) ** 4 for h in range(4)]
        for b in range(4):
            pk, r_pk = pX[0]
            S.op(V, lambda b=b: nc.vector.tensor_tensor(out=krdS[:], in0=krb[:],
                                                        in1=kdec_s[:, b * 4:(b + 1) * 4].unsqueeze(2).broadcast_to([128, 4, 128]), op=ALU.mult),
                 r=[r_krb, r_kdec_s], w=[r_krdS])
            for h in range(4):
                S.op(PE, lambda h=h, b=b: nc.tensor.matmul(pk[:, h * 128:(h + 1) * 128], krdS[:, h, :], vrb[:, h, :],
                                                           start=True, stop=True),
                     r=[r_krdS, r_vrb], w=[r_pk], sig=(h == 3))
            for h in range(4):
                S.op(V, lambda h=h, b=b: nc.vector.scalar_tensor_tensor(out=rstS[:, b, h, :], in0=rstS[:, b, h, :], scalar=float(g4[h]),
                                                                        in1=pk[:, h * 128:(h + 1) * 128], op0=ALU.mult, op1=ALU.add),
                     r=[r_rstS, r_pk, r_rbS], w=[r_rstS])
        S.dma(SP, lambda: nc.sync.dma_start(out=ret_s.rearrange("b h d e -> d b h e"), in_=rstS[:]), r=[r_rstS])

    def route_gen(ti):
        x_t, r_x = xt[(NT + 1) % 2]
        S.dma(SP, lambda: nc.sync.dma_start(out=x_t[:], in_=x2_scr[ti * 128:(ti + 1) * 128, :]), w=[r_x])
        yield 1
        yield from route_tile(ti, x_t, r_x, a2_bc[:], r_a2, sh2_bc[:], r_sh2, 128)

    if STAGE >= 5:
        cur = tile_body(NT, True)
        if DEFER:
            barrier([xt[0][1], xt[1][1]], "x2st")
            run_until(cur, "SATT")
            bg = [(route_gen(ti), None) for ti in range(NT)]
            n_pages = 4 * NPG
            per = max(1, -(-(NT * 24) // max(1, n_pages)))
            for v in cur:
                if v == "SATT_END":
                    break
                for _ in range(per):
                    step_bg(bg)
            while bg:
                step_bg(bg)
        run_until(cur, None)
    barrier(s_res, "smp")
    esS.close()
    if STAGE < 3:
        barrier(R.ALL, 'end')
        es1.close()
        return nc, es
    barrier(p1_res, "p1")
    es1.close()
    es2 = ExitStack()
    p2_res = []

    def sb2(name, shape, dt=F32):
        t = es2.enter_context(nc.sbuf_tensor(name, list(shape), dt))
        r = R(name)
        p2_res.append(r)
        return t, r

    wg = [sb2(f"wg{i}", [128, 8, DEXP], BF16) for i in range(2)]
    wu = [sb2(f"wu{i}", [128, 8, DEXP], BF16) for i in range(2)]
    wd = [sb2(f"wd{i}", [128, 2, D], BF16) for i in range(2)]
    Xs = [sb2(f"Xs{i}", [128, 3, D], BF16) for i in range(2)]
    XT = [sb2(f"XT{i}", [128, 8, SCH], BF16) for i in range(2)]
    sa, r_sa = sb2("sa", [128, 2, SCH], F32)
    hh = [sb2(f"hh{i}", [128, 2, SCH], BF16) for i in range(2)]
    Ys = [sb2(f"Ys{i}", [128, 3, D], F32) for i in range(2)]
    cc = 0
    for e in range(NEXP):
        wg_t, r_wg = wg[e % 2]
        wu_t, r_wu = wu[e % 2]
        wd_t, r_wd = wd[e % 2]
        S.dma(P, lambda: nc.gpsimd.dma_start(out=wg_t[:], in_=w_ge[e].rearrange("(k p) f -> p k f", p=128)), w=[r_wg])
        S.dma(P, lambda: nc.gpsimd.dma_start(out=wu_t[:], in_=w_ue[e].rearrange("(k p) f -> p k f", p=128)), w=[r_wu])
        S.dma(P, lambda: nc.gpsimd.dma_start(out=wd_t[:], in_=w_de[e].rearrange("(c p) n -> p c n", p=128)), w=[r_wd])
        for sc in range(CAP // SCH):
            row0 = e * CAP + sc * SCH
            Xs_t, r_Xs = Xs[cc % 2]
            XT_t, r_XT = XT[cc % 2]
            hh_t, r_hh = hh[cc % 2]
            Ys_t, r_Ys = Ys[cc % 2]
            cc += 1
            S.dma(SP, lambda: nc.sync.dma_start(out=Xs_t[:], in_=X_scr[row0:row0 + SCH, :].rearrange("(t p) d -> p t d", p=128)),
                  w=[r_Xs])
            for st in range(SCH // 128):
                if st % 2 == 0:
                    pt, r_pt = pT[0][0][:], pT[0][1]
                else:
                    pt, r_pt = pO[0][:].bitcast(BF16), pO[1]
                for k in range(8):
                    S.op(PE, lambda k=k, st=st, pt=pt: nc.tensor.transpose(pt[:, k * 128:(k + 1) * 128], Xs_t[:, st, k * 128:(k + 1) * 128],
                                                                           ident_b[:]),
                         r=[r_Xs, r_ident_b], w=[r_pt], sig=(k == 7))
                S.op(A if st % 2 == 0 else V,
                     (lambda st=st, pt=pt: nc.scalar.copy(out=XT_t[:, :, st * 128:(st + 1) * 128], in_=pt.rearrange("p (k t) -> p k t", k=8)))
                     if st % 2 == 0 else
                     (lambda st=st, pt=pt: nc.vector.tensor_copy(out=XT_t[:, :, st * 128:(st + 1) * 128], in_=pt.rearrange("p (k t) -> p k t", k=8))),
                     r=[r_pt], w=[r_XT])
            banks = {("a", 0): pG[0], ("a", 1): pG[1], ("u", 0): pS[0], ("u", 1): pS[1]}
            for tag, wt, r_w in (("a", wg_t, r_wg), ("u", wu_t, r_wu)):
                for fc in range(2):
                    pb, r_pb = banks[(tag, fc)]
                    for k in range(8):
                        S.op(PE, lambda k=k, fc=fc, pb=pb, wt=wt: nc.tensor.matmul(
                            pb[:, 0:SCH], wt[:, k, fc * 128:(fc + 1) * 128], XT_t[:, k, :], start=(k == 0), stop=(k == 7)),
                            r=[r_w, r_XT], w=[r_pb], sig=(k == 7))
            for fc in range(2):
                pa, r_pa = banks[("a", fc)]
                pu, r_pu = banks[("u", fc)]
                S.op(A, lambda fc=fc, pa=pa: nc.scalar.activation(out=sa[:, fc, :], in_=pa[:, 0:SCH], func=AF.Silu),
                     r=[r_pa], w=[r_sa])
                S.op(V, lambda fc=fc, pu=pu: nc.vector.tensor_tensor(out=hh_t[:, fc, :], in0=pu[:, 0:SCH], in1=sa[:, fc, :], op=ALU.mult),
                     r=[r_pu, r_sa], w=[r_hh])
            for st in range(SCH // 128):
                for n in range(2):
                    pd, r_pd = pX[n]
                    for fc in range(2):
                        S.op(PE, lambda fc=fc, n=n, st=st, pd=pd: nc.tensor.matmul(
                            pd[:], hh_t[:, fc, st * 128:(st + 1) * 128], wd_t[:, fc, n * 512:(n + 1) * 512],
                            start=(fc == 0), stop=(fc == 1)),
                            r=[r_hh, r_wd], w=[r_pd], sig=(fc == 1))
                    if n == 0:
                        S.op(A, lambda st=st, n=n, pd=pd: nc.scalar.copy(out=Ys_t[:, st, n * 512:(n + 1) * 512], in_=pd[:]),
                             r=[r_pd], w=[r_Ys])
                    else:
                        S.op(V, lambda st=st, n=n, pd=pd: nc.vector.tensor_copy(out=Ys_t[:, st, n * 512:(n + 1) * 512], in_=pd[:]),
                             r=[r_pd], w=[r_Ys])
            S.dma(SP, lambda: nc.sync.dma_start(out=Y_scr[row0:row0 + SCH, :].rearrange("(t p) d -> p t d", p=128), in_=Ys_t[:]),
                  r=[r_Ys])

    if STAGE < 4:
        barrier(R.ALL, 'end')
        es2.close()
        return nc, es
    barrier(p2_res, "p2")
    es2.close()
    es3 = ExitStack()
    p3_res = []

    def sb3(name, shape, dt=F32):
        t = es3.enter_context(nc.sbuf_tensor(name, list(shape), dt))
        r = R(name)
        p3_res.append(r)
        return t, r

    x2r = [sb3(f"x2r{i}", [128, D]) for i in range(2)]
    y1r = [sb3(f"y1r{i}", [128, D]) for i in range(2)]
    y2r = [sb3(f"y2r{i}", [128, D]) for i in range(2)]
    yo = [sb3(f"yo{i}", [128, D]) for i in range(2)]
    junk3, r_junk3 = sb3("junk3", [128, D], BF16)
    st3, r_st3 = sb3("st3", [128, 2])
    eps3, r_eps3 = sb3("eps3", [128, 1])
    fing_bc, r_fing = sb3("fing_bc", [128, D])
    gt2_bc, r_gt2 = sb3("gt2_bc", [128, D])
    S.dma(SP, lambda: nc.sync.dma_start(out=fing_bc[:], in_=fin_g.partition_broadcast(128)), w=[r_fing])
    S.dma(SP, lambda: nc.sync.dma_start(out=gt2_bc[:], in_=gt2_scr.partition_broadcast(128)), r=[r_gt2scr], w=[r_gt2])
    S.op(V, lambda: nc.vector.memset(eps3[:], EPS), w=[r_eps3])

    def combine_tile(ti, M, gt2ap, r_g2, out_ap):
        x2_t, r_x2t = x2r[ti % 2]
        y1_t, r_y1 = y1r[ti % 2]
        y2_t, r_y2 = y2r[ti % 2]
        yo_t, r_yo = yo[ti % 2]
        rows = slice(ti * 128, ti * 128 + M)
        S.dma(SP, lambda: nc.sync.dma_start(out=x2_t[0:M, :], in_=x2_scr[rows, :]), w=[r_x2t])
        for c, (yt_, r_y) in enumerate(((y1_t, r_y1), (y2_t, r_y2))):
            S.dma(P, lambda c=c, yt_=yt_: nc.gpsimd.indirect_dma_start(
                out=yt_[:], out_offset=None, in_=Y_scr,
                in_offset=bass.IndirectOffsetOnAxis(ap=idx_all[:, ti, c:c + 1], axis=0)),
                r=[r_idx, r_ytrash], w=[r_y])
        S.op(V, lambda: nc.vector.tensor_scalar(out=y1_t[0:M, :], in0=y1_t[0:M, :], scalar1=wts_all[0:M, ti, 0:1], scalar2=None,
                                                op0=ALU.mult), r=[r_y1, r_wts], w=[r_y1])
        S.op(V, lambda: nc.vector.scalar_tensor_tensor(out=y1_t[0:M, :], in0=y2_t[0:M, :], scalar=wts_all[0:M, ti, 1:2],
                                                       in1=y1_t[0:M, :], op0=ALU.mult, op1=ALU.add),
             r=[r_y1, r_y2, r_wts], w=[r_y1])
        S.op(P, lambda: nc.gpsimd.tensor_tensor(out=y1_t[0:M, :], in0=y1_t[0:M, :], in1=gt2ap, op=ALU.mult),
             r=[r_y1, r_g2], w=[r_y1])
        S.op(V, lambda: nc.vector.tensor_tensor(out=x2_t[0:M, :], in0=x2_t[0:M, :], in1=y1_t[0:M, :], op=ALU.add),
             r=[r_y1, r_x2t], w=[r_x2t])
        S.op(A, lambda: nc.scalar.activation(out=junk3[0:M, :], in_=x2_t[0:M, :], func=AF.Square, accum_out=st3[0:M, 0:1]),
             r=[r_x2t], w=[r_junk3, r_st3])
        S.op(A, lambda: nc.scalar.activation(out=st3[0:M, 0:1], in_=st3[0:M, 0:1], func=AF.Sqrt, scale=1.0 / D, bias=eps3[0:M, 0:1]),
             r=[r_st3, r_eps3], w=[r_st3])
        S.op(V, lambda: nc.vector.reciprocal(out=st3[0:M, 0:1], in_=st3[0:M, 0:1]), r=[r_st3], w=[r_st3])
        S.op(V, lambda: nc.vector.scalar_tensor_tensor(out=yo_t[0:M, :], in0=x2_t[0:M, :], scalar=st3[0:M, 0:1],
                                                       in1=fing_bc[0:M, :], op0=ALU.mult, op1=ALU.mult),
             r=[r_x2t, r_st3, r_fing], w=[r_yo])
        S.dma(SP, lambda: nc.sync.dma_start(out=out_ap, in_=yo_t[0:M, :]), r=[r_yo])

    for ti in range(NT):
        combine_tile(ti, 128, gt2_bc[:], r_gt2, y_p[ti * 128:(ti + 1) * 128, :])
    if STAGE >= 5:
        gt2s, r_gt2s = sb3("gt2s", [16, D])
        S.dma(SP, lambda: nc.sync.dma_start(out=gt2s[:], in_=mods_scr[:, 5 * D:6 * D]), r=[r_modsscr], w=[r_gt2s])
        combine_tile(NT, 16, gt2s[:], r_gt2s, y_s)

    barrier(R.ALL, "end")
    es3.close()
    return nc, es


_CACHE = {}


def _consts(T, PAST):
    f32 = np.float32
    c = {}
    c["c_ident"] = np.eye(128, dtype=f32)
    k = np.arange(128)
    c["c_mask"] = (k[:, None] <= k[None, :]).astype(f32)
    c["c_ut"] = (k[:, None] < k[None, :]).astype(f32)

    def rope_tab(pos):
        cols = []
        for d in (64, 128):
            half = d // 2
            inv = np.power(f32(10000.0), (-(f32(2.0) / f32(d)) * np.arange(half, dtype=f32)).astype(f32)).astype(f32)
            ang = (pos.astype(f32)[:, None] * inv[None, :]).astype(f32).astype(np.float64)
            cs, sn = np.cos(ang), np.sin(ang)
            cols += [cs, cs, -sn, sn]
        return np.concatenate(cols, axis=1).astype(f32)
    c["c_rope_p"] = rope_tab(np.arange(T))
    c["c_rope_s"] = rope_tab(PAST + (np.arange(16) % 4))
    gam = 1.0 - 2.0 ** (-5.0 - np.arange(4))
    sc = 128.0 ** -0.5
    j = np.arange(128)[:, None, None]
    i = np.arange(128)[None, None, :]
    g = gam[None, :, None]
    dect = np.where(i >= j, sc * g ** np.maximum(i - j, 0), 0.0)
    c["c_dect"] = dect.reshape(128, 512).astype(f32)
    qd = (g ** (i + 1.0)) * np.ones((128, 1, 1))
    c["c_qdec"] = qd.reshape(128, 512).astype(f32)
    c["c_kdec"] = (sc * gam[None, :] ** (127.0 - np.arange(128)[:, None])).astype(f32)
    ec = np.zeros((128, 18), f32)
    ec[:, :16] = np.arange(16)[None, :] * CAP
    ec[:, 16] = TRASH + np.arange(128)
    ec[:, 17] = TRASH + 128 + np.arange(128)
    c["c_ec"] = ec
    r16 = np.arange(16)
    ds = np.zeros((128, 4, 128))
    for h in range(4):
        for jj in range(16):
            for ii in range(16):
                if jj // 4 == ii // 4 and ii % 4 >= jj % 4:
                    ds[jj, h, ii] = sc * gam[h] ** (ii % 4 - jj % 4)
    c["c_dect_s"] = ds.reshape(128, 512).astype(f32)
    qs = np.zeros((128, 4, 4, 128))
    ks = np.zeros((128, 4, 4))
    mn = np.zeros((128, 4, 32))
    for b in range(4):
        for h in range(4):
            for t in range(4):
                qs[:, b, h, 4 * b + t] = gam[h] ** (t + 1.0)
                ks[4 * b + t, b, h] = sc * gam[h] ** (3.0 - t)
        for kk in range(16):
            for col in range(32):
                if kk // 4 == b and kk % 4 <= col % 4:
                    mn[kk, b, col] = 1.0
    c["c_qdec_s"] = qs.reshape(128, 2048).astype(f32)
    c["c_kdec_s"] = ks.reshape(128, 16).astype(f32)
    c["c_masknew"] = mn.reshape(128, 128).astype(f32)
    sl = np.zeros((32, 4, 4, 16))
    for row in range(32):
        for b in range(4):
            sl[row, b, row // 8, 4 * b + row % 4] = 1.0
    c["c_sel"] = sl.reshape(32, 256).astype(f32)
    sm = np.zeros((32, 2))
    sm[:, 1] = (np.arange(32) // 4) % 2
    sm[:, 0] = 1.0 - sm[:, 1]
    c["c_smask"] = sm.astype(f32)
    c["c_pcol"] = np.arange(128, dtype=f32)[:, None]
    return c


def kernel(**inp):
    f32 = np.float32
    xpr = np.asarray(inp["x_prompt"], f32)
    B, T, _ = xpr.shape
    xsm = np.asarray(inp["x_sample"], f32)
    DB, TS, _ = xsm.shape
    pt = np.asarray(inp["page_table"])
    NPG = pt.shape[1]
    ck = np.asarray(inp["cache_k"], f32)
    NPHYS = ck.shape[1]
    ncores = B
    nb = DB // ncores
    assert nb * TS == 16
    key = (T, NPG, NPHYS)
    nc, es = build(T, NPG, NPHYS)
    cst = _consts(T, NPG * 128)
    w_er = np.asarray(inp["w_expert_router"], f32)[0]
    common = {
        "w_ada": np.asarray(inp["w_ada"], f32)[0], "b_ada": np.asarray(inp["b_ada"], f32)[0][None, :],
        "g_mix": np.asarray(inp["norm_mix_g"], f32), "g_ffn": np.asarray(inp["norm_ffn_g"], f32),
        "w_in": np.asarray(inp["w_in"], f32)[0],
        "lam_p": np.concatenate([np.asarray(inp[n], f32)[0] for n in ("lambda_q1", "lambda_k1", "lambda_q2", "lambda_k2")])[None, :],
        "beta": np.asarray(inp["beta_mix"], f32), "w_out": np.asarray(inp["w_out"], f32)[0],
        "w_rt": np.ascontiguousarray(np.concatenate([np.asarray(inp["w_group"], f32)[0],
                                                     w_er.transpose(1, 0, 2).reshape(D, 16)], axis=1)),
        "b_rt": np.concatenate([np.asarray(inp["b_group"], f32)[0], np.asarray(inp["b_expert_router"], f32)[0].reshape(16)])[None, :],
        "w_ge": np.asarray(inp["w_gate_e"], f32)[0], "w_ue": np.asarray(inp["w_up_e"], f32)[0],
        "w_de": np.asarray(inp["w_down_e"], f32)[0], "fin_g": np.asarray(inp["final_g"], f32)[None, :],
    }
    common.update(cst)
    cpr = np.asarray(inp["c_prompt"], f32)
    csm = np.asarray(inp["c_sample"], f32)
    in_maps = []
    ckv2 = np.concatenate([ck[0].reshape(NPHYS * 128, 512),
                           np.asarray(inp["cache_v"], f32)[0].reshape(NPHYS * 128, 512)], axis=1)
    sret = np.asarray(inp["state_ret"], f32)[0]
    for c in range(ncores):
        m = dict(common)
        m["ckv"] = ckv2
        m["ptab"] = np.ascontiguousarray(pt[c * nb:(c + 1) * nb].astype(np.int32).reshape(1, -1))
        m["st_ret"] = np.ascontiguousarray(sret[c * nb:(c + 1) * nb])
        m["xp"] = np.ascontiguousarray(xpr[c])
        m["xs"] = np.ascontiguousarray(xsm[c * nb:(c + 1) * nb].reshape(16, D))
        m["cp_rep"] = np.ascontiguousarray(np.repeat(cpr[c:c + 1], 128, axis=0))
        m["cs_rep"] = np.ascontiguousarray(np.repeat(csm[c * nb:(c + 1) * nb], TS, axis=0))
        in_maps.append(m)
    res = run_bass_kernel_spmd(nc, in_maps, core_ids=list(range(ncores)))
    try:
        es.close()
    except Exception:
        pass
    rr = res.results
    y_prompt = np.stack([r["y_p"] for r in rr]).astype(f32)
    y_sample = np.concatenate([r["y_s"].reshape(nb, TS, D) for r in rr]).astype(f32)
    k_prompt = np.stack([r["k_p"].reshape(T, 8, 64) for r in rr])[None].astype(f32)
    v_prompt = np.stack([r["v_p"].reshape(T, 4, 128) for r in rr])[None].astype(f32)
    ret_prompt = np.stack([r["ret_p"] for r in rr])[None].astype(f32)
    k_sample = np.concatenate([r["k_s"].reshape(nb, TS, 8, 64) for r in rr])[None].astype(f32)
    v_sample = np.concatenate([r["v_s"].reshape(nb, TS, 4, 128) for r in rr])[None].astype(f32)
    ret_sample = np.concatenate([r["ret_s"] for r in rr])[None].astype(f32)
    return (y_prompt, y_sample, k_prompt, v_prompt, ret_prompt, k_sample, v_sample, ret_sample)
```

```python
import math
from contextlib import ExitStack
import numpy as np
import concourse.bass as bass
import concourse.mybir as mybir
from concourse.bass_utils import run_bass_kernel_spmd

F32 = mybir.dt.float32
BF16 = mybir.dt.bfloat16
I32 = mybir.dt.int32
AF = mybir.ActivationFunctionType
ALU = mybir.AluOpType
AX = mybir.AxisListType

D = 1024
NCOL = 3584
NEXP = 16
DEXP = 256
EPS = 1e-6
GN_EPS = 1e-5
LAM_INIT = 0.2
CAP = 1152
SCH = 384
TRASH = NEXP * CAP


class R:
    __slots__ = ("name", "w", "rd", "excl")

    ALL = []

    def __init__(self, name):
        self.name = name
        self.w = None
        self.rd = {}
        self.excl = False
        R.ALL.append(self)


class Sched:
    def __init__(self, nc, es):
        self.nc, self.es = nc, es
        self.engs = {"pe": nc.tensor, "act": nc.scalar, "dve": nc.vector, "pool": nc.gpsimd, "sp": nc.sync}
        self.sems, self.cnt = {}, {}
        for e in ("pe", "act", "dve", "pool"):
            self.sems[e] = es.enter_context(nc.semaphore("s_" + e))
            self.cnt[e] = 0
        self.seen = {e: {} for e in self.engs}
        self.nops = {e: 0 for e in self.engs}

    def _deps(self, r, w):
        d = {}

        def add(k, v):
            if d.get(k, 0) < v:
                d[k] = v
        for x in r:
            if x.w is not None:
                add(*x.w)
            if x.excl:
                for k, v in x.rd.items():
                    add(k, v)
        for x in w:
            if x.w is not None:
                add(*x.w)
            for k, v in x.rd.items():
                add(k, v)
        return d

    def _wait(self, E, deps):
        for k, v in deps.items():
            if k == E and E == "pe":
                continue
            if self.seen[E].get(k, 0) >= v:
                continue
            self.engs[E].wait_ge(self.sems[k], v)
            self.seen[E][k] = v

    def _mark(self, tok, r, w):
        k, v = tok
        for x in r:
            if x.excl:
                x.w = tok
                x.rd = {}
            elif x.rd.get(k, 0) < v:
                x.rd[k] = v
        for x in w:
            x.w = tok
            x.rd = {}

    def op(self, E, fn, r=(), w=(), sig=True):
        self._wait(E, self._deps(r, w))
        ins = fn()
        self.nops[E] += 1
        if sig:
            self.cnt[E] += 1
            ins.then_inc(self.sems[E], 1)
            tok = (E, self.cnt[E])
        else:
            tok = (E, self.cnt[E] + 1)
        self._mark(tok, r, w)
        return ins

    def dma(self, Q, fn, r=(), w=(), key=None):
        self._wait(Q, self._deps(r, w))
        if key is None:
            key = ("L_" + w[0].name) if w else ("S_" + r[0].name)
        if key not in self.sems:
            self.sems[key] = self.es.enter_context(self.nc.semaphore(key))
            self.cnt[key] = 0
        ins = fn()
        self.nops[Q] += 1
        self.cnt[key] += 16
        ins.then_inc(self.sems[key], 16)
        tok = (key, self.cnt[key])
        self._mark(tok, r, w)
        return tok

    def finish(self, resources):
        d = {}
        for x in resources:
            if x.w is not None and d.get(x.w[0], 0) < x.w[1]:
                d[x.w[0]] = x.w[1]
            for k, v in x.rd.items():
                if d.get(k, 0) < v:
                    d[k] = v
        self._wait("sp", d)


def build(T, NPG, NPHYS, debug=False):
    import os
    STAGE = int(os.environ.get("KSTAGE", "9"))
    KCUT = int(os.environ.get("KCUT", "99"))
    R.ALL = []
    NT = T // 128
    nc = bass.Bass("TRN2", target_bir_lowering=False)
    es = ExitStack()
    S = Sched(nc, es)

    def din(name, shape, dt=F32):
        return nc.dram_tensor(name, list(shape), dt, kind="ExternalInput").ap()

    def dout(name, shape, dt=F32):
        return nc.dram_tensor(name, list(shape), dt, kind="ExternalOutput").ap()

    xp = din("xp", [T, D])
    xs = din("xs", [16, D])
    cp_rep = din("cp_rep", [128, D])
    cs_rep = din("cs_rep", [16, D])
    w_ada = din("w_ada", [D, 6 * D])
    b_ada = din("b_ada", [1, 6 * D])
    g_mix = din("g_mix", [1, D])
    g_ffn = din("g_ffn", [1, D])
    w_in = din("w_in", [D, NCOL])
    lam_p = din("lam_p", [1, 256])
    beta = din("beta", [1, D])
    w_out = din("w_out", [D, D])
    w_rt = din("w_rt", [D, 20])
    b_rt = din("b_rt", [1, 20])
    w_ge = din("w_ge", [NEXP, D, DEXP])
    w_ue = din("w_ue", [NEXP, D, DEXP])
    w_de = din("w_de", [NEXP, DEXP, D])
    fin_g = din("fin_g", [1, D])
    c_ident = din("c_ident", [128, 128])
    c_mask = din("c_mask", [128, 128])
    c_ut = din("c_ut", [128, 128])
    c_rope_p = din("c_rope_p", [T, 384])
    c_rope_s = din("c_rope_s", [16, 384])
    c_dect = din("c_dect", [128, 512])
    c_qdec = din("c_qdec", [128, 512])
    c_kdec = din("c_kdec", [128, 4])
    c_ec = din("c_ec", [128, 18])
    c_dect_s = din("c_dect_s", [128, 512])
    c_qdec_s = din("c_qdec_s", [128, 2048])
    c_kdec_s = din("c_kdec_s", [128, 16])
    c_masknew = din("c_masknew", [128, 128])
    c_sel = din("c_sel", [32, 256])
    c_smask = din("c_smask", [32, 2])
    c_pcol = din("c_pcol", [128, 1])
    ckv = din("ckv", [NPHYS * 128, 1024])
    ptab = din("ptab", [1, 4 * NPG], I32)
    st_ret = din("st_ret", [4, 4, 128, 128])

    y_p = dout("y_p", [T, D])
    y_s = dout("y_s", [16, D])
    k_p = dout("k_p", [T, 512])
    v_p = dout("v_p", [T, 512])
    ret_p = dout("ret_p", [4, 128, 128])
    k_s = dout("k_s", [16, 512])
    v_s = dout("v_s", [16, 512])
    ret_s = dout("ret_s", [4, 4, 128, 128])

    NTT = NT + 1
    x2_scr = nc.dram_tensor("x2_scr", [NTT * 128, D], F32, kind="Internal").ap()
    X_scr = nc.dram_tensor("X_scr", [TRASH + 256, D], BF16, kind="Internal").ap()
    Y_scr = nc.dram_tensor("Y_scr", [TRASH + 256, D], F32, kind="Internal").ap()
    mods_scr = nc.dram_tensor("mods_scr", [16, 6 * D], F32, kind="Internal").ap()
    gt2_scr = nc.dram_tensor("gt2_scr", [1, D], F32, kind="Internal").ap()

    def sb(name, shape, dt=F32):
        t = es.enter_context(nc.sbuf_tensor(name, list(shape), dt))
        return t, R(name)

    def ps(name, shape, dt=F32):
        t = es.enter_context(nc.psum_tensor(name, list(shape), dt))
        r = R(name)
        r.excl = True
        return t, r

    V, A, P, PE, SP = "dve", "act", "pool", "pe", "sp"

    def barrier(resources, tag):
        d = {}
        for x in resources:
            if x.w is not None and d.get(x.w[0], 0) < x.w[1]:
                d[x.w[0]] = x.w[1]
            for k, v in x.rd.items():
                if d.get(k, 0) < v:
                    d[k] = v
        for E in ("pe", "act", "dve", "pool", "sp"):
            S._wait(E, d)

    pG = [ps(f"pG{i}", [128, 512]) for i in range(2)]
    pT = [ps(f"pT{i}", [128, 1024], BF16) for i in range(1)]
    pS = [ps(f"pS{i}", [128, 512]) for i in range(2)]
    pO = ps("pO", [128, 512])
    pX = [ps(f"pX{i}", [128, 512]) for i in range(2)]
    pO_reg = [R(f"pO_r{i}") for i in range(3)]
    gctr = [0]

    def nextG():
        gctr[0] += 1
        return pG[gctr[0] % 2]

    ident_b, r_ident_b = sb("ident_b", [128, 128], BF16)
    ident_f, r_ident_f = sb("ident_f", [128, 128], F32)
    mask_b, r_mask = sb("mask_b", [128, 128], BF16)
    ut_b, r_ut = sb("ut_b", [128, 128], BF16)
    ones_b, r_ones = sb("ones_b", [128, 128], BF16)
    dect, r_dect = sb("dect", [128, 512], F32)
    qdec, r_qdec = sb("qdec", [128, 512], BF16)
    kdec, r_kdec = sb("kdec", [128, 4], F32)
    ecr, r_ecr = sb("ecr", [128, 18], F32)
    beta_fm, r_betafm = sb("beta_fm", [128, 8])
    brt_bc, r_brt = sb("brt_bc", [128, 20])
    wrt, r_wrt = sb("wrt", [128, 8, 20], F32)
    a2_bc, r_a2 = sb("a2_bc", [128, D])
    sh2_bc, r_sh2 = sb("sh2_bc", [128, D])
    a1_fm, r_a1fm = sb("a1_fm", [128, 8])
    sh1_fm, r_sh1fm = sb("sh1_fm", [128, 8])
    neglam, r_neglam = sb("neglam", [128, 1])
    idx_all, r_idx = sb("idx_all", [128, NTT, 2], I32)
    wts_all, r_wts = sb("wts_all", [128, NTT, 2], F32)
    base_bc, r_base = sb("base_bc", [128, 16], F32)
    win, r_win = sb("win", [128, 8, NCOL], BF16)
    wout, r_wout = sb("wout", [128, 8, D], BF16)

    def load_const(dst, rdst, src, eng=SP):
        if eng == SP:
            S.dma(SP, lambda: nc.sync.dma_start(out=dst, in_=src), w=[rdst])
        else:
            S.dma(P, lambda: nc.gpsimd.dma_start(out=dst, in_=src), w=[rdst])

    load_const(ident_f[:], r_ident_f, c_ident)
    load_const(ident_b[:], r_ident_b, c_ident, P)
    load_const(mask_b[:], r_mask, c_mask, P)
    load_const(ut_b[:], r_ut, c_ut, P)
    load_const(dect[:], r_dect, c_dect)
    load_const(qdec[:], r_qdec, c_qdec, P)
    load_const(kdec[:], r_kdec, c_kdec)
    load_const(ecr[:], r_ecr, c_ec)
    load_const(brt_bc[:], r_brt, b_rt.partition_broadcast(128))
    load_const(wrt[:], r_wrt, w_rt.rearrange("(k p) n -> p k n", p=128))
    S.op(V, lambda: nc.vector.memset(ones_b[:], 1.0), w=[r_ones])
    S.op(V, lambda: nc.vector.memset(base_bc[:], 0.0), w=[r_base])
    w_in_v = w_in.rearrange("(k p) n -> p k n", p=128)
    for k in range(8):
        S.dma(P, lambda k=k: nc.gpsimd.dma_start(out=win[:, k, :], in_=w_in_v[:, k, :]), w=[r_win], key="L_win")
    S.dma(P, lambda: nc.gpsimd.dma_start(out=wout[:], in_=w_out.rearrange("(k p) n -> p k n", p=128)), w=[r_wout])

    es_setup = ExitStack()

    def sbt(name, shape, dt=F32):
        t = es_setup.enter_context(nc.sbuf_tensor(name, list(shape), dt))
        return t, R(name)

    lam_t, r_lamt = sbt("lam_t", [128, 256])
    lam_j, r_lamj = sbt("lam_j", [128, 64])
    lam_s, r_lams = sbt("lam_s", [128, 2])
    S.dma(SP, lambda: nc.sync.dma_start(out=lam_t[:], in_=lam_p.partition_broadcast(128)), w=[r_lamt])
    for i in range(2):
        S.op(V, lambda i=i: nc.vector.scalar_tensor_tensor(
            out=lam_j[:], in0=lam_t[:, 128 * i:128 * i + 64], scalar=1.0, in1=lam_t[:, 128 * i + 64:128 * i + 128],
            op0=ALU.mult, op1=ALU.mult, accum_out=lam_s[:, i:i + 1]),
            r=[r_lamt], w=[r_lamj, r_lams])
    S.op(A, lambda: nc.scalar.activation(out=lam_s[:], in_=lam_s[:], func=AF.Exp), r=[r_lams], w=[r_lams])
    S.op(V, lambda: nc.vector.scalar_tensor_tensor(out=neglam[:], in0=lam_s[:, 1:2], scalar=-LAM_INIT,
                                                   in1=lam_s[:, 0:1], op0=ALU.add, op1=ALU.subtract),
         r=[r_lams], w=[r_neglam])

    if KCUT == 1:
        barrier(R.ALL, 'end')
        return nc, es
    modp, r_modp = sbt("modp", [128, 6 * D])
    mods, r_mods = sbt("mods", [16, 6 * D])
    beta_bc, r_beta = sbt("beta_bc", [128, D])
    S.dma(SP, lambda: nc.sync.dma_start(out=beta_bc[:], in_=beta.partition_broadcast(128)), w=[r_beta])
    c_f, r_cf = sbt("c_f", [128, D])
    c_b, r_cb = sbt("c_b", [128, D], BF16)
    cs_f, r_csf = sbt("cs_f", [16, D])
    cs_b, r_csb = sbt("cs_b", [16, D], BF16)
    cT, r_cT = sbt("cT", [128, 8, 128], BF16)
    csT, r_csT = sbt("csT", [128, 8, 16], BF16)
    bada, r_bada = sbt("bada", [128, 6 * D])
    S.dma(SP, lambda: nc.sync.dma_start(out=c_f[:], in_=cp_rep), w=[r_cf])
    S.dma(SP, lambda: nc.sync.dma_start(out=cs_f[:], in_=cs_rep), w=[r_csf])
    S.dma(SP, lambda: nc.sync.dma_start(out=bada[:], in_=b_ada.partition_broadcast(128)), w=[r_bada])
    S.op(A, lambda: nc.scalar.activation(out=c_b[:], in_=c_f[:], func=AF.Silu), r=[r_cf], w=[r_cb])
    S.op(A, lambda: nc.scalar.activation(out=cs_b[:], in_=cs_f[:], func=AF.Silu), r=[r_csf], w=[r_csb])
    pt, r_pt = pT[0]
    for k in range(8):
        S.op(PE, lambda k=k: nc.tensor.transpose(pt[:, k * 128:(k + 1) * 128], c_b[:, k * 128:(k + 1) * 128], ident_b[:]),
             r=[r_cb, r_ident_b], w=[r_pt], sig=(k == 7))
    S.op(A, lambda: nc.scalar.copy(out=cT[:].rearrange("p k t -> p (k t)"), in_=pt[:]), r=[r_pt], w=[r_cT])
    for k in range(8):
        S.op(PE, lambda k=k: nc.tensor.transpose(pt[:, k * 16:(k + 1) * 16], cs_b[:, k * 128:(k + 1) * 128], ident_b[0:16, 0:16]),
             r=[r_csb, r_ident_b], w=[r_pt], sig=(k == 7))
    S.op(A, lambda: nc.scalar.copy(out=csT[:].rearrange("p k t -> p (k t)"), in_=pt[:, 0:128]), r=[r_pt], w=[r_csT])
    wa = [sbt(f"wa{i}", [128, 8, 512], BF16) for i in range(2)]
    w_ada_v = w_ada.rearrange("(k p) n -> p k n", p=128)
    for g in range(12):
        wt, r_w = wa[g % 2]
        S.dma(P, lambda g=g, wt=wt: nc.gpsimd.dma_start(out=wt[:], in_=w_ada_v[:, :, g * 512:(g + 1) * 512]), w=[r_w])
        for (lT, r_l, M, dst, r_d) in ((cT, r_cT, 128, modp, r_modp), (csT, r_csT, 16, mods, r_mods)):
            pg, r_pg = nextG()
            for k in range(8):
                S.op(PE, lambda k=k, pg=pg, lT=lT, M=M, wt=wt: nc.tensor.matmul(
                    pg[0:M, :], lT[:, k, :], wt[:, k, :], start=(k == 0), stop=(k == 7)),
                    r=[r_l, r_w], w=[r_pg], sig=(k == 7))
            S.op(V, lambda pg=pg, M=M, dst=dst, g=g: nc.vector.tensor_tensor(
                out=dst[0:M, g * 512:(g + 1) * 512], in0=pg[0:M, :], in1=bada[0:M, g * 512:(g + 1) * 512], op=ALU.add),
                r=[r_pg, r_bada], w=[r_d])
    gm_bc, r_gm = sbt("gm_bc", [128, D])
    gf_bc, r_gf = sbt("gf_bc", [128, D])
    a1_tok, r_a1t = sbt("a1_tok", [128, D])
    S.dma(SP, lambda: nc.sync.dma_start(out=gm_bc[:], in_=g_mix.partition_broadcast(128)), w=[r_gm])
    S.dma(SP, lambda: nc.sync.dma_start(out=gf_bc[:], in_=g_ffn.partition_broadcast(128)), w=[r_gf])
    S.op(V, lambda: nc.vector.scalar_tensor_tensor(out=a1_tok[:], in0=modp[:, D:2 * D], scalar=1.0, in1=gm_bc[:],
                                                   op0=ALU.add, op1=ALU.mult), r=[r_modp, r_gm], w=[r_a1t])
    S.op(V, lambda: nc.vector.scalar_tensor_tensor(out=a2_bc[:], in0=modp[:, 4 * D:5 * D], scalar=1.0, in1=gf_bc[:],
                                                   op0=ALU.add, op1=ALU.mult), r=[r_modp, r_gf], w=[r_a2])
    S.op(V, lambda: nc.vector.tensor_copy(out=sh2_bc[:], in_=modp[:, 3 * D:4 * D]), r=[r_modp], w=[r_sh2])
    r_gt2scr = R("gt2scr")
    S.dma(SP, lambda: nc.sync.dma_start(out=gt2_scr, in_=modp[0:1, 5 * D:6 * D]), r=[r_modp], w=[r_gt2scr], key="S_modp")
    S.op(V, lambda: nc.vector.scalar_tensor_tensor(out=mods[:, D:2 * D], in0=mods[:, D:2 * D], scalar=1.0, in1=gm_bc[0:16, :],
                                                   op0=ALU.add, op1=ALU.mult), r=[r_mods, r_gm], w=[r_mods])
    S.op(V, lambda: nc.vector.scalar_tensor_tensor(out=mods[:, 4 * D:5 * D], in0=mods[:, 4 * D:5 * D], scalar=1.0, in1=gf_bc[0:16, :],
                                                   op0=ALU.add, op1=ALU.mult), r=[r_mods, r_gf], w=[r_mods])
    for (src, r_src, dstt, r_dst) in ((a1_tok, r_a1t, a1_fm, r_a1fm), (modp, r_modp, sh1_fm, r_sh1fm), (beta_bc, r_beta, beta_fm, r_betafm)):
        for half in range(2):
            pg, r_pg = nextG()
            for kk in range(4):
                k = half * 4 + kk
                S.op(PE, lambda k=k, kk=kk, pg=pg, src=src: nc.tensor.transpose(
                    pg[:, kk * 128:(kk + 1) * 128], src[:, k * 128:(k + 1) * 128], ident_f[:]),
                    r=[r_src, r_ident_f], w=[r_pg], sig=(kk == 3))
            S.op(V, lambda pg=pg, dstt=dstt, half=half: nc.vector.tensor_copy(
                out=dstt[:, half * 4:half * 4 + 4], in_=pg[:].rearrange("p (k t) -> p k t", k=4)[:, :, 0]),
                r=[r_pg], w=[r_dst])

    r_modsscr = R("modsscr")
    S.dma(SP, lambda: nc.sync.dma_start(out=mods_scr, in_=mods[:]), r=[r_mods], w=[r_modsscr], key="S_mods")
    for k in range(8):
        S.op(V, lambda k=k: nc.vector.scalar_tensor_tensor(out=wout[:, k, :], in0=wout[:, k, :], scalar=beta_fm[:, k:k + 1],
                                                           in1=modp[:, 2 * D:3 * D], op0=ALU.mult, op1=ALU.mult),
             r=[r_wout, r_betafm, r_modp], w=[r_wout])
    setup_res = [r_lamt, r_lamj, r_lams, r_modp, r_cf, r_cb, r_csf, r_csb, r_cT, r_csT, r_bada,
                 wa[0][1], wa[1][1], r_gm, r_gf, r_a1t, r_mods, r_beta]
    setup_guard = R("setup_guard")

    def barrier(resources, tag):
        d = {}
        for x in resources:
            if x.w is not None and d.get(x.w[0], 0) < x.w[1]:
                d[x.w[0]] = x.w[1]
            for k, v in x.rd.items():
                if d.get(k, 0) < v:
                    d[k] = v
        for E in ("pe", "act", "dve", "pool", "sp"):
            S._wait(E, d)

    if KCUT == 2:
        barrier(R.ALL, 'end')
        return nc, es
    barrier(setup_res, "setup")
    es_setup.close()

    es1 = ExitStack()

    def sb1(name, shape, dt=F32):
        t = es1.enter_context(nc.sbuf_tensor(name, list(shape), dt))
        return t, R(name)

    p1_res = []

    def sb1r(name, shape, dt=F32):
        t, r = sb1(name, shape, dt)
        p1_res.append(r)
        return t, r

    rst, r_rst = sb1r("rst", [128, 4, 128], F32)
    rbf, r_rbf = sb1r("rbf", [128, 4, 128], BF16)
    S.op(V, lambda: nc.vector.memset(rst[:].rearrange("p a b -> p (a b)"), 0.0), w=[r_rst])
    S.op(V, lambda: nc.vector.memset(rbf[:].rearrange("p a b -> p (a b)"), 0.0), w=[r_rbf])

    def ring(name, n, shape, dt=F32):
        out = []
        for i in range(n):
            out.append(sb1r(f"{name}{i}", shape, dt))
        return out

    xt = ring("xt", 2, [128, D])
    rt = ring("rt", 2, [128, 384])
    xn, r_xn = sb1r("xn", [128, D], BF16)
    junk, r_junk = xn, r_xn
    ymix, r_ymix = xn, r_xn
    hT, r_hT = sb1r("hT", [128, 8, 128], BF16)
    yT, r_yT = hT, r_hT
    st4, r_st4 = sb1r("st4", [128, 8])
    tt, r_tt = sb1r("tt", [128, D])
    t1, r_t1 = tt[:, 0:512], r_tt
    t2, r_t2 = tt[:, 512:1024], r_tt
    h2f, r_h2f = tt, r_tt
    rb_, r_rb = sb1r("rb_", [128, 512], BF16)
    qb, r_qb = rb_, r_rb
    kb, r_kb = rb_, r_rb
    qrb, r_qrb = rb_, r_rb
    QT = ring("QT", 2, [128, 4, 128], BF16)
    kf = ring("kf", 1, [128, 512])
    vf = ring("vf", 1, [128, 512])
    qrT_r = ring("qrT", 2, [128, 4, 128], BF16)
    qrTd_r = ring("qrTd", 2, [128, 4, 128], BF16)
    krb_r = ring("krb", 2, [128, 4, 128], BF16)
    krd_r = ring("krd", 2, [128, 4, 128], BF16)
    krT_r = ring("krT", 2, [128, 4, 128], BF16)
    vrb_r = ring("vrb", 2, [128, 4, 128], BF16)
    sg_r = ring("sg", 2, [128, 512], BF16)
    PTr = ring("PT", 2, [128, 512], BF16)
    odr = ring("od", 2, [128, 4, 128])
    rl, r_rl = sb1r("rl", [128, 2])
    sTm, r_sTm = sb1r("sTm", [128, 4, 128], BF16)
    h2b = ring("h2b", 2, [128, D], BF16)
    h2T, r_h2T = sb1r("h2T", [128, 4, 128], F32)
    rlg, r_rlg = sb1r("rlg", [128, 20])
    rs_, r_rs = sb1r("rs_", [128, 100])
    oh16, r_oh16 = sb1r("oh16", [128, 2, 16])
    mb, r_mb = sb1r("mb", [128, 16], BF16)
    slotf, r_slotf = sb1r("slotf", [128, 4])

    def rope(pg, r_pg, H, tab, r_tab, coff, out_ap, r_out):
        hf = 256 // H
        zv = pg[:].rearrange("p (h two f) -> p h two f", h=H, two=2)
        cosv = tab[:, coff:coff + 2 * hf].rearrange("p (two f) -> p two f", two=2).unsqueeze(1).broadcast_to([128, H, 2, hf])
        sinv = tab[:, coff + 2 * hf:coff + 4 * hf].rearrange("p (two f) -> p two f", two=2)
        t1v = t1.rearrange("p (h two f) -> p h two f", h=H, two=2)
        t2v = t2.rearrange("p (h two f) -> p h two f", h=H, two=2)
        S.op(V, lambda: nc.vector.tensor_tensor(out=t1v, in0=zv, in1=cosv, op=ALU.mult), r=[r_pg, r_tab], w=[r_t1])
        for half in range(2):
            S.op(V, lambda half=half: nc.vector.tensor_tensor(
                out=t2v[:, :, half, :], in0=zv[:, :, 1 - half, :],
                in1=sinv[:, half, :].unsqueeze(1).broadcast_to([128, H, hf]), op=ALU.mult),
                r=[r_pg, r_tab], w=[r_t2])
        S.op(V, lambda: nc.vector.tensor_tensor(out=out_ap, in0=t1, in1=t2, op=ALU.add), r=[r_t1], w=[r_out])

    def transpose4(src, r_src, dst, r_dst, copy_eng=A):
        pt, r_pt = pT[0]
        for c in range(4):
            S.op(PE, lambda c=c: nc.tensor.transpose(pt[:, c * 128:(c + 1) * 128], src[:, c * 128:(c + 1) * 128], ident_b[:]),
                 r=[r_src, r_ident_b], w=[r_pt], sig=(c == 3))
        if copy_eng == A:
            S.op(A, lambda: nc.scalar.copy(out=dst, in_=pt[:, 0:512]), r=[r_pt], w=[r_dst])
        else:
            S.op(V, lambda: nc.vector.tensor_copy(out=dst, in_=pt[:, 0:512]), r=[r_pt], w=[r_dst])

    def rms_rstd(src, r_src, dst_col, r_dst, n, eps, M=128):
        S.op(A, lambda: nc.scalar.activation(out=junk[0:M, 0:n], in_=src, func=AF.Square, accum_out=dst_col),
             r=[r_src], w=[r_junk, r_dst])
        S.op(A, lambda: nc.scalar.activation(out=dst_col, in_=dst_col, func=AF.Ln, scale=1.0 / n, bias=eps_t[0:M, 0:1]),
             r=[r_dst, r_eps], w=[r_dst])
        S.op(A, lambda: nc.scalar.activation(out=dst_col, in_=dst_col, func=AF.Exp, scale=-0.5), r=[r_dst], w=[r_dst])

    eps_t, r_eps = sb1r("eps_t", [128, 2])
    S.op(V, lambda: nc.vector.memset(eps_t[:, 0:1], EPS), w=[r_eps])
    S.op(V, lambda: nc.vector.memset(eps_t[:, 1:2], GN_EPS), w=[r_eps])

    esKV = ExitStack()
    KT = esKV.enter_context(nc.sbuf_tensor("KT", [128, 4, T], BF16))
    r_KT = [R(f"KT{i}") for i in range(NT)]
    Vt = esKV.enter_context(nc.sbuf_tensor("Vt", [128, NT, 4, 129], BF16))
    r_V = [R(f"V{i}") for i in range(NT)]
    S.op(P, lambda: nc.gpsimd.memset(Vt[:].rearrange("p a b c -> p (a b c)"), 1.0), w=r_V)
    out_res = []
    if KCUT == 3:
        barrier(R.ALL, 'end')
        return nc, es


    ztile, r_ztile = xt[1]
    S.op(V, lambda: nc.vector.memset(ztile[:], 0.0), w=[r_ztile])
    r_ytrash = R("ytrash")
    S.dma(SP, lambda: nc.sync.dma_start(out=Y_scr[TRASH:TRASH + 128, :], in_=ztile[:]), r=[r_ztile], w=[r_ytrash], key="S_ztile")
    S.dma(SP, lambda: nc.sync.dma_start(out=Y_scr[TRASH + 128:TRASH + 256, :], in_=ztile[:]), r=[r_ztile], w=[r_ytrash], key="S_ztile")
    zb, r_zb = h2b[0]
    S.op(V, lambda: nc.vector.memset(zb[:], 0.0), w=[r_zb])
    NXR = (TRASH + 256) // 128
    r_xscr = R("xscr")
    for a in range(0, NXR, 16):
        b = min(NXR, a + 16)
        S.dma(SP, lambda a=a, b=b: nc.sync.dma_start(
            out=X_scr[a * 128:b * 128, :].rearrange("(t p) d -> p t d", p=128),
            in_=zb[:].unsqueeze(1).broadcast_to([128, b - a, D])), r=[r_zb], w=[r_xscr], key="S_zero")
    S.op(V, lambda: nc.vector.tensor_copy(out=idx_all[:, NT, :], in_=ecr[:, 16:18]), r=[r_ecr], w=[r_idx])
    S.op(V, lambda: nc.vector.memset(wts_all[:, NT, :], 0.0), w=[r_wts])

    def route_tile(ti, x2_t, r_x2, a2ap, r_a2_, sh2ap, r_sh2_, M):
        h2b_t, r_h2b = h2b[ti % 2]
        rms_rstd(x2_t[0:M, :], r_x2, st4[0:M, 1:2], r_st4, D, EPS, M)
        S.op(V, lambda: nc.vector.scalar_tensor_tensor(out=h2f[0:M, :], in0=x2_t[0:M, :], scalar=st4[0:M, 1:2], in1=a2ap,
                                                       op0=ALU.mult, op1=ALU.mult), r=[r_x2, r_st4, r_a2_], w=[r_h2f])
        S.op(P, lambda: nc.gpsimd.tensor_tensor(out=h2f[0:M, :], in0=h2f[0:M, :], in1=sh2ap, op=ALU.add),
             r=[r_h2f, r_sh2_], w=[r_h2f])
        S.op(A, lambda: nc.scalar.copy(out=h2b_t[0:M, :], in_=h2f[0:M, :]), r=[r_h2f], w=[r_h2b])
        yield 1
        pgr, r_pgr = pX[0]
        for half in range(2):
            pg, r_pg = nextG()
            for kk in range(4):
                k = half * 4 + kk
                S.op(PE, lambda k=k, kk=kk, pg=pg: nc.tensor.transpose(pg[:, kk * 128:kk * 128 + M], h2f[0:M, k * 128:(k + 1) * 128],
                                                                       ident_f[0:M, 0:M]),
                     r=[r_h2f, r_ident_f], w=[r_pg], sig=(kk == 3))
            yield 1
            S.op(V, lambda pg=pg: nc.vector.tensor_copy(
                out=h2T[:, :, 0:M], in_=pg[:].rearrange("p (k t) -> p k t", k=4)[:, :, 0:M]),
                r=[r_pg], w=[r_h2T])
            yield 1
            for kk in range(4):
                k = half * 4 + kk
                S.op(PE, lambda k=k, kk=kk: nc.tensor.matmul(pgr[0:M, 0:20], h2T[:, kk, 0:M], wrt[:, k, :], start=(k == 0), stop=(k == 7)),
                     r=[r_h2T, r_wrt], w=[r_pgr], sig=(kk == 3))
        pg, r_pg = pgr, r_pgr
        yield 1
        q = rs_
        rr = [r_rs]

        def v(fn, r=(), w=()):
            S.op(V, fn, r=list(r) + rr, w=list(w) + rr)
        S.op(V, lambda pg=pg: nc.vector.tensor_tensor(out=rlg[0:M, :], in0=pg[0:M, 0:20], in1=brt_bc[0:M, :], op=ALU.add),
             r=[r_pg, r_brt], w=[r_rlg])
        lg = rlg[0:M, 0:4]
        v(lambda: nc.vector.tensor_reduce(out=q[0:M, 0:1], in_=lg, axis=AX.X, op=ALU.max), r=[r_rlg])
        v(lambda: nc.vector.tensor_scalar(out=q[0:M, 4:8], in0=lg, scalar1=q[0:M, 0:1], scalar2=None, op0=ALU.is_ge), r=[r_rlg])
        v(lambda: nc.vector.tensor_scalar(out=q[0:M, 1:2], in0=q[0:M, 0:1], scalar1=-1.0, scalar2=None, op0=ALU.mult))
        S.op(A, lambda: nc.scalar.activation(out=q[0:M, 8:12], in_=lg, func=AF.Exp, bias=q[0:M, 1:2], accum_out=q[0:M, 2:3]),
             r=[r_rlg, r_rs], w=[r_rs])
        v(lambda: nc.vector.reciprocal(out=q[0:M, 2:3], in_=q[0:M, 2:3]))
        v(lambda: nc.vector.tensor_tensor(out=q[0:M, 16:32].rearrange("p (g e) -> p g e", g=4),
                                          in0=rlg[0:M, 4:20].rearrange("p (g e) -> p g e", g=4),
                                          in1=q[0:M, 4:8].unsqueeze(2).broadcast_to([M, 4, 4]), op=ALU.mult), r=[r_rlg])
        v(lambda: nc.vector.tensor_reduce(out=q[0:M, 12:16], in_=q[0:M, 16:32].rearrange("p (g e) -> p e g", g=4), axis=AX.X, op=ALU.add))
        le = q[0:M, 12:16]
        v(lambda: nc.vector.tensor_reduce(out=q[0:M, 3:4], in_=le, axis=AX.X, op=ALU.max))
        v(lambda: nc.vector.tensor_scalar(out=q[0:M, 32:36], in0=le, scalar1=q[0:M, 3:4], scalar2=None, op0=ALU.is_ge))
        v(lambda: nc.vector.scalar_tensor_tensor(out=q[0:M, 36:40], in0=q[0:M, 32:36], scalar=-1e30, in1=le, op0=ALU.mult, op1=ALU.add))
        v(lambda: nc.vector.tensor_reduce(out=q[0:M, 40:41], in_=q[0:M, 36:40], axis=AX.X, op=ALU.max))
        v(lambda: nc.vector.tensor_scalar(out=q[0:M, 44:48], in0=q[0:M, 36:40], scalar1=q[0:M, 40:41], scalar2=None, op0=ALU.is_ge))
        v(lambda: nc.vector.tensor_scalar(out=q[0:M, 42:43], in0=q[0:M, 3:4], scalar1=-1.0, scalar2=None, op0=ALU.mult))
        S.op(A, lambda: nc.scalar.activation(out=q[0:M, 41:42], in_=q[0:M, 40:41], func=AF.Exp, bias=q[0:M, 42:43]),
             r=[r_rs], w=[r_rs])
        v(lambda: nc.vector.tensor_scalar(out=q[0:M, 48:49], in0=q[0:M, 41:42], scalar1=1.0, scalar2=None, op0=ALU.add))
        v(lambda: nc.vector.reciprocal(out=q[0:M, 48:49], in_=q[0:M, 48:49]))
        v(lambda: nc.vector.tensor_tensor(out=q[0:M, 49:50], in0=q[0:M, 41:42], in1=q[0:M, 48:49], op=ALU.mult))
        v(lambda: nc.vector.tensor_scalar(out=q[0:M, 48:50], in0=q[0:M, 48:50], scalar1=q[0:M, 2:3], scalar2=None, op0=ALU.mult))
        for c, off in ((0, 32), (1, 44)):
            S.op(V, lambda c=c, off=off: nc.vector.tensor_tensor(
                out=oh16[0:M, c, :].rearrange("p (g e) -> p g e", g=4),
                in0=q[0:M, 4:8].unsqueeze(2).broadcast_to([M, 4, 4]),
                in1=q[0:M, off:off + 4].unsqueeze(1).broadcast_to([M, 4, 4]), op=ALU.mult), r=[r_rs], w=[r_oh16])
        S.op(V, lambda: nc.vector.tensor_tensor(out=mb[0:M, :], in0=oh16[0:M, 0, :], in1=oh16[0:M, 1, :], op=ALU.add),
             r=[r_oh16], w=[r_mb])
        yield 1
        pc, r_pc = nextG()
        S.op(PE, lambda: nc.tensor.matmul(pc[0:M, 0:16], ut_b[0:M, 0:M], mb[0:M, :], start=True, stop=True),
             r=[r_ut, r_mb], w=[r_pc], sig=False)
        S.op(PE, lambda: nc.tensor.matmul(pc[:, 16:32], ones_b[0:M, :], mb[0:M, :], start=True, stop=True),
             r=[r_ones, r_mb], w=[r_pc])
        yield 1
        S.op(V, lambda: nc.vector.tensor_tensor(out=q[0:M, 50:66], in0=pc[0:M, 0:16], in1=base_bc[0:M, :], op=ALU.add),
             r=[r_pc, r_base, r_rs], w=[r_rs])
        v(lambda: nc.vector.tensor_tensor(out=q[0:M, 66:82], in0=q[0:M, 50:66], in1=ecr[0:M, 0:16], op=ALU.add), r=[r_ecr])
        S.op(V, lambda: nc.vector.tensor_tensor(out=base_bc[:], in0=base_bc[:], in1=pc[:, 16:32], op=ALU.add),
             r=[r_pc, r_base], w=[r_base])
        for c in range(2):
            S.op(V, lambda c=c: nc.vector.scalar_tensor_tensor(out=q[0:M, 82:98], in0=oh16[0:M, c, :], scalar=1.0, in1=q[0:M, 66:82],
                                                               op0=ALU.mult, op1=ALU.mult, accum_out=slotf[0:M, c:c + 1]),
                 r=[r_oh16, r_rs], w=[r_rs, r_slotf])
            S.op(V, lambda c=c: nc.vector.scalar_tensor_tensor(out=q[0:M, 82:98], in0=oh16[0:M, c, :], scalar=1.0, in1=q[0:M, 50:66],
                                                               op0=ALU.mult, op1=ALU.mult, accum_out=slotf[0:M, 2 + c:3 + c]),
                 r=[r_oh16, r_rs], w=[r_rs, r_slotf])
        sf = [r_slotf]
        S.op(V, lambda: nc.vector.tensor_scalar(out=slotf[0:M, 2:4], in0=slotf[0:M, 2:4], scalar1=float(CAP), scalar2=None, op0=ALU.is_lt),
             r=sf, w=sf)
        S.op(V, lambda: nc.vector.tensor_tensor(out=slotf[0:M, 0:2], in0=slotf[0:M, 0:2], in1=ecr[0:M, 16:18],
                                                op=ALU.subtract), r=sf + [r_ecr], w=sf)
        S.op(V, lambda: nc.vector.tensor_tensor(out=slotf[0:M, 0:2], in0=slotf[0:M, 0:2], in1=slotf[0:M, 2:4], op=ALU.mult), r=sf, w=sf)
        S.op(V, lambda: nc.vector.tensor_tensor(out=slotf[0:M, 0:2], in0=slotf[0:M, 0:2], in1=ecr[0:M, 16:18],
                                                op=ALU.add), r=sf + [r_ecr], w=sf)
        S.op(V, lambda: nc.vector.tensor_copy(out=idx_all[0:M, ti, :], in_=slotf[0:M, 0:2]), r=sf, w=[r_idx])
        S.op(V, lambda: nc.vector.tensor_tensor(out=wts_all[0:M, ti, :], in0=q[0:M, 48:50], in1=slotf[0:M, 2:4], op=ALU.mult),
             r=sf + [r_rs], w=[r_wts])
        for c in range(2):
            S.dma(P, lambda c=c: nc.gpsimd.indirect_dma_start(
                out=X_scr, out_offset=bass.IndirectOffsetOnAxis(ap=idx_all[:, ti, c:c + 1], axis=0),
                in_=h2b_t[:], in_offset=None), r=[r_h2b, r_idx], key=f"S_h2b{ti % 2}")

    smp = {}

    def tile_body(i, SMP=False):
        x_t, r_x = xt[i % 2]
        od, r_od = odr[i % 2]
        smp['od'], smp['r_od'] = od, r_od
        qrT, r_qrT = qrT_r[i % 2]
        qrTd, r_qrTd = qrTd_r[i % 2]
        krb, r_krb = krb_r[i % 2]
        krd, r_krd = krd_r[i % 2]
        krT, r_krT = krT_r[i % 2]
        vrb, r_vrb = vrb_r[i % 2]
        sg, r_sg = sg_r[i % 2]
        smp.update(qrT=qrT, r_qrT=r_qrT, krb=krb, r_krb=r_krb, vrb=vrb, r_vrb=r_vrb)
        r_t, r_r = rt[i % 2]
        tok = slice(i * 128, (i + 1) * 128)
        if SMP:
            S.op(V, lambda: nc.vector.memset(x_t[:], 0.0), w=[r_x])
            S.op(V, lambda: nc.vector.memset(r_t[:], 0.0), w=[r_r])
            S.dma(SP, lambda: nc.sync.dma_start(out=x_t[0:16, :], in_=xs), w=[r_x])
            S.dma(SP, lambda: nc.sync.dma_start(out=r_t[0:16, :], in_=c_rope_s), w=[r_r])
        else:
            S.dma(SP, lambda: nc.sync.dma_start(out=x_t[:], in_=xp[tok, :]), w=[r_x])
            S.dma(SP, lambda: nc.sync.dma_start(out=r_t[:], in_=c_rope_p[tok, :]), w=[r_r])
        rms_rstd(x_t[:], r_x, st4[:, 0:1], r_st4, D, EPS)
        if SMP:
            S.op(V, lambda: nc.vector.scalar_tensor_tensor(out=tt[:], in0=x_t[:], scalar=st4[:, 0:1], in1=smp["mA"][:, 0, :],
                                                           op0=ALU.mult, op1=ALU.mult), r=[r_x, r_st4, smp["r_mA"]], w=[r_tt])
            S.op(V, lambda: nc.vector.tensor_tensor(out=xn[:], in0=tt[:], in1=smp["mA"][:, 1, :], op=ALU.add),
                 r=[r_tt, smp["r_mA"]], w=[r_xn])
        else:
            S.op(V, lambda: nc.vector.tensor_scalar(out=xn[:], in0=x_t[:], scalar1=st4[:, 0:1], scalar2=None, op0=ALU.mult),
                 r=[r_x, r_st4], w=[r_xn])
        pt, r_pt = pT[0]
        for k in range(8):
            S.op(PE, lambda k=k: nc.tensor.transpose(pt[:, k * 128:(k + 1) * 128], xn[:, k * 128:(k + 1) * 128], ident_b[:]),
                 r=[r_xn, r_ident_b], w=[r_pt], sig=(k == 7))
        if SMP:
            S.op(A, lambda: nc.scalar.copy(out=hT[:].rearrange("p k t -> p (k t)"), in_=pt[:]), r=[r_pt], w=[r_hT])
        else:
            for k in range(8):
                S.op(A, lambda k=k: nc.scalar.activation(out=hT[:, k, :], in_=pt[:, k * 128:(k + 1) * 128], func=AF.Identity,
                                                         scale=a1_fm[:, k:k + 1], bias=sh1_fm[:, k:k + 1]),
                     r=[r_pt, r_a1fm, r_sh1fm], w=[r_hT])
        if KCUT == 4:
            return
        yield 1
        q_t, r_q = QT[i % 2]
        k_f, r_kf = kf[0]
        v_f, r_vf = vf[0]
        for g in range(int(os.environ.get('KG', '7'))):
            pg, r_pg = nextG()
            for k in range(8):
                S.op(PE, lambda k=k, g=g, pg=pg: nc.tensor.matmul(pg[:], hT[:, k, :], win[:, k, g * 512:(g + 1) * 512],
                                                                  start=(k == 0), stop=(k == 7)),
                     r=[r_hT, r_win], w=[r_pg], sig=(k == 7))
            yield 1
            if g == 0:
                rope(pg, r_pg, 8, r_t, r_r, 0, qb[:], r_qb)
                yield 1
                transpose4(qb, r_qb, q_t[:].rearrange("p c t -> p (c t)"), r_q)
            elif g == 1:
                rope(pg, r_pg, 8, r_t, r_r, 0, k_f[:], r_kf)
                if SMP:
                    S.dma(SP, lambda: nc.sync.dma_start(out=k_s, in_=k_f[0:16, :]), r=[r_kf])
                else:
                    S.dma(SP, lambda: nc.sync.dma_start(out=k_p[tok, :], in_=k_f[:]), r=[r_kf])
                S.op(A, lambda: nc.scalar.copy(out=kb[:], in_=k_f[:]), r=[r_kf], w=[r_kb])
                yield 1
                if SMP:
                    transpose4(kb, r_kb, smp["KTs"][:].rearrange("p c t -> p (c t)"), smp["r_KTs"])
                else:
                    transpose4(kb, r_kb, KT[:, :, tok], r_KT[i])
            elif g == 2:
                S.op(A, lambda pg=pg: nc.scalar.copy(out=v_f[:], in_=pg[:]), r=[r_pg], w=[r_vf])
                if SMP:
                    S.op(V, lambda pg=pg: nc.vector.tensor_copy(out=smp["Vs"][:], in_=pg[:]), r=[r_pg], w=[smp["r_Vs"]])
                    S.dma(SP, lambda: nc.sync.dma_start(out=v_s, in_=v_f[0:16, :]), r=[r_vf])
                else:
                    S.op(V, lambda pg=pg: nc.vector.tensor_copy(out=Vt[:, i, :, 0:128], in_=pg[:].rearrange("p (h e) -> p h e", h=4)),
                         r=[r_pg], w=[r_V[i]])
                    S.dma(SP, lambda: nc.sync.dma_start(out=v_p[tok, :], in_=v_f[:]), r=[r_vf])
            elif g == 3:
                rope(pg, r_pg, 4, r_t, r_r, 128, qrb[:], r_qrb)
                yield 1
                transpose4(qrb, r_qrb, qrT[:].rearrange("p c t -> p (c t)"), r_qrT)
                S.op(P, lambda: nc.gpsimd.tensor_tensor(out=qrTd[:].rearrange("p c t -> p (c t)"),
                                                        in0=qrT[:].rearrange("p c t -> p (c t)"), in1=qdec[:], op=ALU.mult),
                     r=[r_qrT, r_qdec], w=[r_qrTd])
            elif g == 4:
                rope(pg, r_pg, 4, r_t, r_r, 128, krb[:].rearrange("p h d -> p (h d)"), r_krb)
                yield 1
                transpose4(krb[:].rearrange("p h d -> p (h d)"), r_krb, krT[:].rearrange("p c t -> p (c t)"), r_krT)
                S.op(P, lambda: nc.gpsimd.tensor_tensor(out=krd[:], in0=krb[:],
                                                        in1=kdec[:].unsqueeze(2).broadcast_to([128, 4, 128]), op=ALU.mult),
                     r=[r_krb, r_kdec], w=[r_krd])
            elif g == 5:
                S.op(A, lambda pg=pg: nc.scalar.copy(out=vrb[:].rearrange("p h d -> p (h d)"), in_=pg[:]), r=[r_pg], w=[r_vrb])
            else:
                S.op(A, lambda pg=pg: nc.scalar.activation(out=t1, in_=pg[:], func=AF.Exp, scale=-1.0), r=[r_pg], w=[r_t1])
                S.op(V, lambda: nc.vector.tensor_scalar(out=t1, in0=t1, scalar1=1.0, scalar2=None, op0=ALU.add), r=[r_t1], w=[r_t1])
                S.op(V, lambda: nc.vector.reciprocal(out=t1, in_=t1), r=[r_t1], w=[r_t1])
                S.op(V, lambda pg=pg: nc.vector.tensor_tensor(out=sg[:], in0=pg[:], in1=t1, op=ALU.mult), r=[r_pg, r_t1], w=[r_sg])
        if KCUT == 5:
            return
            yield 1
        yield 'ATTN'
        units = []
        for j in range(8):
            blocks = list(range(i + 1))
            grp = [blocks[a:a + 4] for a in range(0, len(blocks), 4)]
            for gi, gb in enumerate(grp):
                units.append((j, gi, gb, gi == 0, gi == len(grp) - 1))

        def emit_qk(u, ui):
            j, gi, gb, first, last = u
            h, s = j // 2, j % 2
            psl = slice(64 * s, 64 * s + 64)
            pS_t, r_pS = pS[ui % 2]
            for bi, kbk in enumerate(gb):
                S.op(PE, lambda bi=bi, kbk=kbk: nc.tensor.matmul(
                    pS_t[:, bi * 128:(bi + 1) * 128], KT[psl, h, kbk * 128:(kbk + 1) * 128], q_t[psl, h, :],
                    start=True, stop=True),
                    r=[r_KT[kbk], r_q], w=[r_pS], sig=(bi == len(gb) - 1))

        def emit_av(u, ui):
            j, gi, gb, first, last = u
            h, s = j // 2, j % 2
            pS_t, r_pS = pS[ui % 2]
            P_t, r_P = PTr[ui % 2]
            n = len(gb) * 128
            S.op(A, lambda: nc.scalar.activation(out=P_t[:, 0:n], in_=pS_t[:, 0:n], func=AF.Exp, scale=0.125),
                 r=[r_pS], w=[r_P])
            if gb[-1] == i:
                S.op(V, lambda: nc.vector.tensor_tensor(out=P_t[:, n - 128:n], in0=P_t[:, n - 128:n], in1=mask_b[:], op=ALU.mult),
                     r=[r_P, r_mask], w=[r_P])
            reg = 0
            po, r_po = (pO if j % 2 == 0 else pX[1])
            for bi, kbk in enumerate(gb):
                S.op(PE, lambda bi=bi, kbk=kbk: nc.tensor.matmul(
                    po[:, reg * 129:reg * 129 + 129], P_t[:, bi * 128:(bi + 1) * 128], Vt[:, kbk, h, :],
                    start=(first and bi == 0), stop=(last and bi == len(gb) - 1)),
                    r=[r_P, r_V[kbk]], w=[r_po], sig=(bi == len(gb) - 1))
            if last:
                c0 = reg * 129
                S.op(V, lambda: nc.vector.reciprocal(out=rl[:, s:s + 1], in_=po[:, c0 + 128:c0 + 129]), r=[r_po], w=[r_rl])
                if s == 0:
                    S.op(V, lambda: nc.vector.tensor_scalar(out=od[:, h, :], in0=po[:, c0:c0 + 128], scalar1=rl[:, 0:1],
                                                            scalar2=None, op0=ALU.mult), r=[r_po, r_rl], w=[r_od])
                else:
                    S.op(V, lambda: nc.vector.tensor_scalar(out=rl[:, 1:2], in0=rl[:, 1:2], scalar1=neglam[:, 0:1],
                                                            scalar2=None, op0=ALU.mult), r=[r_rl, r_neglam], w=[r_rl])
                    S.op(V, lambda: nc.vector.scalar_tensor_tensor(out=od[:, h, :], in0=po[:, c0:c0 + 128], scalar=rl[:, 1:2],
                                                                   in1=od[:, h, :], op0=ALU.mult, op1=ALU.add),
                         r=[r_po, r_rl, r_od], w=[r_od])

        if SMP:
            units = []
            sample_attention(q_t, r_q)
        elif STAGE < 1:
            units = []
            S.op(V, lambda: nc.vector.memset(od[:].rearrange('p a b -> p (a b)'), 0.5), w=[r_od])
        else:
            emit_qk(units[0], 0)
        for ui, u in enumerate(units):
            if ui + 1 < len(units):
                emit_qk(units[ui + 1], ui + 1)
            emit_av(u, ui)
            yield 1

        yield 'TAIL'
        for h in range(4):
            S.op(A, lambda h=h: nc.scalar.activation(out=junk[:, 0:128], in_=od[:, h, :], func=AF.Square,
                                                     accum_out=st4[:, 4 + h:5 + h]), r=[r_od], w=[r_junk, r_st4])
        c08 = (1.0 - LAM_INIT) ** 2
        S.op(A, lambda: nc.scalar.activation(out=st4[:, 4:8], in_=st4[:, 4:8], func=AF.Ln, scale=1.0 / (128 * c08),
                                             bias=eps_t[:, 0:1]), r=[r_st4, r_eps], w=[r_st4])
        S.op(A, lambda: nc.scalar.activation(out=st4[:, 4:8], in_=st4[:, 4:8], func=AF.Exp, scale=-0.5), r=[r_st4], w=[r_st4])
        for h in range(4):
            S.op(V, lambda h=h: nc.vector.tensor_scalar(out=ymix[:, h * 128:(h + 1) * 128], in0=od[:, h, :],
                                                        scalar1=st4[:, 4 + h:5 + h], scalar2=None, op0=ALU.mult),
                 r=[r_od, r_st4], w=[r_ymix])

        if KCUT == 6:
            return
        yield 1
        pg, r_pg = nextG()
        for h in range(4):
            S.op(PE, lambda h=h, pg=pg: nc.tensor.matmul(pg[:, h * 128:(h + 1) * 128], krT[:, h, :], qrT[:, h, :],
                                                         start=True, stop=True),
                 r=[r_krT, r_qrT], w=[r_pg], sig=(h == 3))
        yield 1
        dect_u, r_dect_u = (smp["dect_s"], smp["r_dect_s"]) if SMP else (dect, r_dect)
        S.op(V, lambda pg=pg: nc.vector.tensor_tensor(out=sTm[:].rearrange("p h t -> p (h t)"), in0=pg[:], in1=dect_u[:], op=ALU.mult),
             r=[r_pg, r_dect_u], w=[r_sTm])
        yield 1
        po_, r_po_ = nextG()
        if SMP:
            sample_ret_cross(po_, r_po_)
        else:
            for h in range(4):
                S.op(PE, lambda h=h, po_=po_: nc.tensor.matmul(po_[:, h * 128:(h + 1) * 128], sTm[:, h, :], vrb[:, h, :],
                                                               start=True, stop=False),
                     r=[r_sTm, r_vrb], w=[r_po_], sig=False)
                S.op(PE, lambda h=h, po_=po_: nc.tensor.matmul(po_[:, h * 128:(h + 1) * 128], qrTd[:, h, :], rbf[:, h, :],
                                                               start=False, stop=True),
                     r=[r_qrTd, r_rbf], w=[r_po_], sig=(h == 3))
            pk, r_pk = pX[0]
            for h in range(4):
                S.op(PE, lambda h=h: nc.tensor.matmul(pk[:, h * 128:(h + 1) * 128], krd[:, h, :], vrb[:, h, :],
                                                      start=True, stop=True),
                     r=[r_krd, r_vrb], w=[r_pk], sig=(h == 3))
            for h in range(4):
                cd = (1.0 - 2.0 ** (-5.0 - h)) ** 128
                S.op(V, lambda h=h, cd=cd: nc.vector.scalar_tensor_tensor(out=rst[:, h, :], in0=rst[:, h, :], scalar=float(cd),
                                                                          in1=pk[:, h * 128:(h + 1) * 128], op0=ALU.mult, op1=ALU.add),
                     r=[r_rst, r_pk], w=[r_rst])
            S.op(A, lambda: nc.scalar.copy(out=rbf[:], in_=rst[:]), r=[r_rst], w=[r_rbf])
        yield 1
        S.op(V, lambda po_=po_: nc.vector.tensor_reduce(out=rs_[:, 0:4], in_=po_[:].rearrange("p (h e) -> p h e", h=4),
                                                        axis=AX.X, op=ALU.add), r=[r_po_], w=[r_rs])
        S.op(A, lambda po_=po_: nc.scalar.activation(out=t1, in_=po_[:], func=AF.Square), r=[r_po_], w=[r_t1])
        S.op(V, lambda: nc.vector.tensor_reduce(out=rs_[:, 4:8], in_=t1.rearrange("p (h e) -> p h e", h=4),
                                                axis=AX.X, op=ALU.add), r=[r_t1], w=[r_rs])
        S.op(V, lambda: nc.vector.tensor_scalar(out=rs_[:, 0:4], in0=rs_[:, 0:4], scalar1=1.0 / 128, scalar2=None, op0=ALU.mult),
             r=[r_rs], w=[r_rs])
        S.op(V, lambda: nc.vector.tensor_tensor(out=rs_[:, 8:12], in0=rs_[:, 0:4], in1=rs_[:, 0:4], op=ALU.mult), r=[r_rs], w=[r_rs])
        S.op(V, lambda: nc.vector.scalar_tensor_tensor(out=rs_[:, 4:8], in0=rs_[:, 4:8], scalar=1.0 / 128, in1=rs_[:, 8:12],
                                                       op0=ALU.mult, op1=ALU.subtract), r=[r_rs], w=[r_rs])
        S.op(A, lambda: nc.scalar.activation(out=rs_[:, 4:8], in_=rs_[:, 4:8], func=AF.Ln, bias=eps_t[:, 1:2]),
             r=[r_rs, r_eps], w=[r_rs])
        S.op(A, lambda: nc.scalar.activation(out=rs_[:, 4:8], in_=rs_[:, 4:8], func=AF.Exp, scale=-0.5), r=[r_rs], w=[r_rs])
        for h in range(4):
            S.op(V, lambda h=h, po_=po_: nc.vector.tensor_scalar(out=t2[:, h * 128:(h + 1) * 128], in0=po_[:, h * 128:(h + 1) * 128],
                                                                 scalar1=rs_[:, h:h + 1], scalar2=rs_[:, 4 + h:5 + h],
                                                                 op0=ALU.subtract, op1=ALU.mult),
                 r=[r_po_, r_rs], w=[r_t2])
        S.op(V, lambda: nc.vector.tensor_tensor(out=ymix[:, 512:1024], in0=t2, in1=sg[:], op=ALU.mult),
             r=[r_t2, r_sg], w=[r_ymix])

        if KCUT == 7:
            return
        yield 1
        pt, r_pt = pT[0]
        for k in range(8):
            S.op(PE, lambda k=k: nc.tensor.transpose(pt[:, k * 128:(k + 1) * 128], ymix[:, k * 128:(k + 1) * 128], ident_b[:]),
                 r=[r_ymix, r_ident_b], w=[r_pt], sig=(k == 7))
        S.op(A, lambda: nc.scalar.copy(out=yT[:].rearrange("p k t -> p (k t)"), in_=pt[:]), r=[r_pt], w=[r_yT])
        yield 1
        x2_t, r_x2 = x_t, r_x
        for n in range(2):
            pg, r_pg = nextG()
            for k in range(8):
                S.op(PE, lambda k=k, n=n, pg=pg: nc.tensor.matmul(pg[:], yT[:, k, :], wout[:, k, n * 512:(n + 1) * 512],
                                                                  start=(k == 0), stop=(k == 7)),
                     r=[r_yT, r_wout], w=[r_pg], sig=(k == 7))
            yield 1
            cs_ = slice(n * 512, (n + 1) * 512)
            if SMP:
                S.op(V, lambda pg=pg, cs_=cs_: nc.vector.tensor_tensor(out=tt[:, cs_], in0=pg[:], in1=smp["gt1s"][:, cs_], op=ALU.mult),
                     r=[r_pg, smp["r_gt1s"]], w=[r_tt])
                S.op(V, lambda cs_=cs_: nc.vector.tensor_tensor(out=x2_t[:, cs_], in0=tt[:, cs_], in1=x_t[:, cs_], op=ALU.add),
                     r=[r_tt, r_x], w=[r_x2])
            else:
                S.op(V, lambda pg=pg, cs_=cs_: nc.vector.tensor_tensor(out=x2_t[:, cs_], in0=pg[:], in1=x_t[:, cs_], op=ALU.add),
                     r=[r_pg, r_x], w=[r_x2])
        if SMP:
            S.dma(SP, lambda: nc.sync.dma_start(out=x2_scr[i * 128:i * 128 + 16, :], in_=x2_t[0:16, :]), r=[r_x2], key=f"S_xt{i % 2}")
        else:
            S.dma(SP, lambda: nc.sync.dma_start(out=x2_scr[tok, :], in_=x2_t[:]), r=[r_x2], key=f"S_xt{i % 2}")

        if STAGE >= 2:
            if SMP:
                smp["load_mA"]((4, 3))
                yield from route_tile(i, x2_t, r_x2, smp["mA"][0:16, 0, :], smp["r_mA"], smp["mA"][0:16, 1, :], smp["r_mA"], 16)
            else:
                yield from route_tile(i, x2_t, r_x2, a2_bc[:], r_a2, sh2_bc[:], r_sh2, 128)

    def run_until(gen, marker):
        for v in gen:
            if v == marker:
                return True
        return False

    def step_bg(bg):
        while bg:
            gen, marker = bg[0]
            try:
                v = next(gen)
            except StopIteration:
                bg.pop(0)
                continue
            if marker is not None and v == marker:
                bg.pop(0)
                continue
            return

    if KCUT < 99 or os.environ.get("KSEQ"):
        for i_ in range(NT):
            run_until(tile_body(i_), None)
    else:
        cur = tile_body(0)
        run_until(cur, "ATTN")
        prev_tail = None
        for i_ in range(NT):
            nxt = tile_body(i_ + 1) if i_ + 1 < NT else None
            bg = []
            if prev_tail is not None:
                bg.append((prev_tail, None))
            if nxt is not None:
                bg.append((nxt, "ATTN"))
            n_units = 8 * ((i_ + 4) // 4)
            kk = max(1, -(-44 // n_units))
            while True:
                v = next(cur)
                if v == "TAIL":
                    break
                for _ in range(kk):
                    step_bg(bg)
            while bg:
                step_bg(bg)
            prev_tail = cur
            cur = nxt
        run_until(prev_tail, None)

    if KCUT < 99:
        barrier(R.ALL, 'end')
        return nc, es
    S.dma(SP, lambda: nc.sync.dma_start(out=ret_p.rearrange("h d e -> d h e"), in_=rst[:]), r=[r_rst])

    barrier(r_KT + r_V, "kv")
    esKV.close()
    esS = ExitStack()
    s_res = []

    def sbs(name, shape, dt=F32):
        t = esS.enter_context(nc.sbuf_tensor(name, list(shape), dt))
        r = R(name)
        s_res.append(r)
        return t, r

    mA, r_mA = sbs("mA", [128, 2, D])
    gt1s, r_gt1s = sbs("gt1s", [128, D])
    KTs, r_KTs = sbs("KTs", [128, 4, 128], BF16)
    Vs, r_Vs = sbs("Vs", [128, 512], BF16)
    Qb, r_Qb = sbs("Qb", [128, 4, 4, 8], BF16)
    idxp, r_idxp = sbs("idxp", [128, 4 * NPG], I32)
    pcol, r_pcol = sbs("pcol", [128, 1])
    KVpg = [sbs(f"KVpg{i}", [128, 1024]) for i in range(3)]
    Kb = [sbs(f"Kb{i}", [128, 512], BF16) for i in range(3)]
    KTp = [sbs(f"KTp{i}", [128, 512], BF16) for i in range(3)]
    Vb = [sbs(f"Vb{i}", [128, 512], BF16) for i in range(3)]
    PTd = [sbs(f"PTd{i}", [128, 32], BF16) for i in range(3)]
    Osc, r_Osc = sbs("Osc", [32, 4, 512], BF16)
    sel, r_sel = sbs("sel", [32, 256], BF16)
    smk, r_smk = sbs("smk", [32, 2])
    rl32, r_rl32 = sbs("rl32", [32, 2])
    rstS, r_rstS = sbs("rstS", [128, 4, 4, 128])
    rbS, r_rbS = sbs("rbS", [128, 4, 4, 128], BF16)
    dect_s, r_dect_s = sbs("dect_s", [128, 512], BF16)
    qdec_s, r_qdec_s = sbs("qdec_s", [128, 4, 4, 128], BF16)
    kdec_s, r_kdec_s = sbs("kdec_s", [128, 16])
    qrTdm, r_qrTdm = sbs("qrTdm", [128, 4, 4, 128], BF16)
    krdS, r_krdS = sbs("krdS", [128, 4, 128], BF16)
    mnew, r_mnew = sbs("mnew", [128, 4, 32])
    smp.update(mA=mA, r_mA=r_mA, gt1s=gt1s, r_gt1s=r_gt1s, KTs=KTs, r_KTs=r_KTs, Vs=Vs, r_Vs=r_Vs,
               dect_s=dect_s, r_dect_s=r_dect_s)
    S.op(V, lambda: nc.vector.memset(mA[:].rearrange("p a d -> p (a d)"), 0.0), w=[r_mA])
    S.op(V, lambda: nc.vector.memset(gt1s[:], 0.0), w=[r_gt1s])
    def load_mA(chs):
        for j, ch in enumerate(chs):
            S.dma(SP, lambda j=j, ch=ch: nc.sync.dma_start(out=mA[0:16, j, :], in_=mods_scr[:, ch * D:(ch + 1) * D]),
                  r=[r_modsscr], w=[r_mA])
    load_mA((1, 0))
    smp["load_mA"] = load_mA
    S.dma(SP, lambda: nc.sync.dma_start(out=gt1s[0:16, :], in_=mods_scr[:, 2 * D:3 * D]), r=[r_modsscr], w=[r_gt1s])
    S.dma(P, lambda: nc.gpsimd.dma_start(out=dect_s[:], in_=c_dect_s), w=[r_dect_s])
    S.dma(P, lambda: nc.gpsimd.dma_start(out=qdec_s[:].rearrange("p a b c -> p (a b c)"), in_=c_qdec_s), w=[r_qdec_s])
    S.dma(SP, lambda: nc.sync.dma_start(out=kdec_s[:], in_=c_kdec_s), w=[r_kdec_s])
    S.dma(SP, lambda: nc.sync.dma_start(out=mnew[:].rearrange("p a b -> p (a b)"), in_=c_masknew), w=[r_mnew])
    S.dma(P, lambda: nc.gpsimd.dma_start(out=sel[:], in_=c_sel), w=[r_sel])
    S.dma(SP, lambda: nc.sync.dma_start(out=smk[:], in_=c_smask), w=[r_smk])
    S.dma(SP, lambda: nc.sync.dma_start(out=pcol[:], in_=c_pcol), w=[r_pcol])
    S.dma(SP, lambda: nc.sync.dma_start(out=idxp[:], in_=ptab.partition_broadcast(128)), w=[r_idxp])
    S.dma(SP, lambda: nc.sync.dma_start(out=rstS[:], in_=st_ret.rearrange("b h d e -> d b h e")), w=[r_rstS])
    S.op(A, lambda: nc.scalar.copy(out=rbS[:].rearrange("p a b c -> p (a b c)"), in_=rstS[:].rearrange("p a b c -> p (a b c)")),
         r=[r_rstS], w=[r_rbS])
    S.op(V, lambda: nc.vector.tensor_scalar(out=idxp[:], in0=idxp[:], scalar1=128.0, scalar2=pcol[:, 0:1], op0=ALU.mult, op1=ALU.add),
         r=[r_idxp, r_pcol], w=[r_idxp])
    S.dma(P, lambda: nc.gpsimd.dma_start(out=wout[:], in_=w_out.rearrange("(k p) n -> p k n", p=128)), w=[r_wout])
    for k in range(8):
        S.op(V, lambda k=k: nc.vector.tensor_scalar(out=wout[:, k, :], in0=wout[:, k, :], scalar1=beta_fm[:, k:k + 1], scalar2=None,
                                                    op0=ALU.mult), r=[r_wout, r_betafm], w=[r_wout])
    S.op(V, lambda: nc.vector.scalar_tensor_tensor(out=rl32[:, 1:2], in0=smk[:, 1:2], scalar=neglam[0:32, 0:1], in1=smk[:, 0:1],
                                                   op0=ALU.mult, op1=ALU.add), r=[r_smk, r_neglam], w=[r_rl32])

    def sample_attention(q_t, r_q):
        S.op(V, lambda: nc.vector.memset(Qb[:].rearrange("p a b c -> p (a b c)"), 0.0), w=[r_Qb])
        for b in range(4):
            for s_ in range(2):
                psl = slice(64 * s_, 64 * s_ + 64)
                S.op(V, lambda b=b, s_=s_, psl=psl: nc.vector.tensor_copy(out=Qb[psl, b, :, 4 * s_:4 * s_ + 4],
                                                                          in_=q_t[psl, :, 4 * b:4 * b + 4]),
                     r=[r_q], w=[r_Qb])
        po, r_po = pO
        psm, r_psm = pX[1]
        cc_ = 0
        for b in range(4):
            for pg_ in range(NPG):
                KVg, r_Kg = KVpg[cc_ % 3]
                r_Vg = r_Kg
                Kb_t, r_Kb = Kb[cc_ % 3]
                KT_t, r_KTp = KTp[cc_ % 3]
                Vb_t, r_Vb = Vb[cc_ % 3]
                P_t, r_P = PTd[cc_ % 3]
                pS_t, r_pS = pS[cc_ % 2]
                cc_ += 1
                col = b * NPG + pg_
                S.dma(P, lambda KVg=KVg, col=col: nc.gpsimd.indirect_dma_start(
                    out=KVg[:], out_offset=None, in_=ckv, in_offset=bass.IndirectOffsetOnAxis(ap=idxp[:, col:col + 1], axis=0)),
                    r=[r_idxp], w=[r_Kg])
                S.op(A, lambda KVg=KVg, Kb_t=Kb_t: nc.scalar.copy(out=Kb_t[:], in_=KVg[:, 0:512]), r=[r_Kg], w=[r_Kb])
                S.op(V, lambda KVg=KVg, Vb_t=Vb_t: nc.vector.tensor_copy(out=Vb_t[:], in_=KVg[:, 512:1024]), r=[r_Vg], w=[r_Vb])
                pt, r_pt = pT[0]
                for c in range(4):
                    S.op(PE, lambda c=c, Kb_t=Kb_t: nc.tensor.transpose(pt[:, c * 128:(c + 1) * 128], Kb_t[:, c * 128:(c + 1) * 128], ident_b[:]),
                         r=[r_Kb, r_ident_b], w=[r_pt], sig=(c == 3))
                S.op(V, lambda KT_t=KT_t: nc.vector.tensor_copy(out=KT_t[:], in_=pt[:, 0:512]), r=[r_pt], w=[r_KTp])
                for h in range(4):
                    S.op(PE, lambda h=h, KT_t=KT_t, pS_t=pS_t, b=b: nc.tensor.matmul(
                        pS_t[:, h * 8:(h + 1) * 8], KT_t[:, h * 128:(h + 1) * 128], Qb[:, b, h, :], start=True, stop=True),
                        r=[r_KTp, r_Qb], w=[r_pS], sig=(h == 3))
                S.op(A, lambda P_t=P_t, pS_t=pS_t: nc.scalar.activation(out=P_t[:], in_=pS_t[:, 0:32], func=AF.Exp, scale=0.125),
                     r=[r_pS], w=[r_P])
                S.op(PE, lambda P_t=P_t, Vb_t=Vb_t, pg_=pg_: nc.tensor.matmul(po[0:32, :], P_t[:], Vb_t[:], start=(pg_ == 0), stop=False),
                     r=[r_P, r_Vb], w=[r_po], sig=False)
                S.op(PE, lambda P_t=P_t, pg_=pg_: nc.tensor.matmul(psm[0:32, 0:1], P_t[:], ones_b[:, 0:1], start=(pg_ == 0), stop=False),
                     r=[r_P, r_ones], w=[r_psm])
            P_t, r_P = PTd[cc_ % 3]
            pS_t, r_pS = pS[cc_ % 2]
            cc_ += 1
            for h in range(4):
                S.op(PE, lambda h=h, pS_t=pS_t, b=b: nc.tensor.matmul(
                    pS_t[0:16, h * 8:(h + 1) * 8], KTs[:, h, 0:16], Qb[:, b, h, :], start=True, stop=True),
                    r=[r_KTs, r_Qb], w=[r_pS], sig=(h == 3))
            S.op(A, lambda P_t=P_t, pS_t=pS_t: nc.scalar.activation(out=P_t[0:16, :], in_=pS_t[0:16, 0:32], func=AF.Exp, scale=0.125),
                 r=[r_pS], w=[r_P])
            S.op(V, lambda P_t=P_t, b=b: nc.vector.tensor_tensor(out=P_t[0:16, :], in0=P_t[0:16, :], in1=mnew[0:16, b, :], op=ALU.mult),
                 r=[r_P, r_mnew], w=[r_P])
            S.op(PE, lambda P_t=P_t: nc.tensor.matmul(po[0:32, :], P_t[0:16, :], Vs[0:16, :], start=False, stop=True),
                 r=[r_P, r_Vs], w=[r_po], sig=False)
            S.op(PE, lambda P_t=P_t: nc.tensor.matmul(psm[0:32, 0:1], P_t[0:16, :], ones_b[0:16, 0:1], start=False, stop=True),
                 r=[r_P, r_ones], w=[r_psm])
            S.op(V, lambda: nc.vector.reciprocal(out=rl32[:, 0:1], in_=psm[0:32, 0:1]), r=[r_psm], w=[r_rl32])
            S.op(V, lambda: nc.vector.tensor_tensor(out=rl32[:, 0:1], in0=rl32[:, 0:1], in1=rl32[:, 1:2], op=ALU.mult),
                 r=[r_rl32], w=[r_rl32])
            S.op(V, lambda b=b: nc.vector.tensor_scalar(out=Osc[:, b, :], in0=po[0:32, :], scalar1=rl32[:, 0:1], scalar2=None, op0=ALU.mult),
                 r=[r_po, r_rl32], w=[r_Osc])
        pg2, r_pg2 = nextG()
        for h in range(4):
            for b in range(4):
                S.op(PE, lambda h=h, b=b: nc.tensor.matmul(
                    pg2[0:16, h * 128:(h + 1) * 128], sel[:, (b * 4 + h) * 16:(b * 4 + h + 1) * 16], Osc[:, b, h * 128:(h + 1) * 128],
                    start=(b == 0), stop=(b == 3)),
                    r=[r_sel, r_Osc], w=[r_pg2], sig=(b == 3 and h == 3))
        S.op(V, lambda: nc.vector.tensor_copy(out=smp["od"][0:16, :, :].rearrange("p h e -> p (h e)"), in_=pg2[0:16, :]), r=[r_pg2], w=[smp["r_od"]])

    def sample_ret_cross(po_, r_po_):
        qrT, r_qrT, krb, r_krb, vrb, r_vrb = smp['qrT'], smp['r_qrT'], smp['krb'], smp['r_krb'], smp['vrb'], smp['r_vrb']
        for b in range(4):
            S.op(V, lambda b=b: nc.vector.tensor_tensor(out=qrTdm[:, b, :, :], in0=qrT[:], in1=qdec_s[:, b, :, :], op=ALU.mult),
                 r=[r_qrT, r_qdec_s], w=[r_qrTdm])
        for h in range(4):
            S.op(PE, lambda h=h: nc.tensor.matmul(po_[:, h * 128:(h + 1) * 128], sTm[:, h, :], vrb[:, h, :], start=True, stop=False),
                 r=[r_sTm, r_vrb], w=[r_po_], sig=False)
            for b in range(4):
                S.op(PE, lambda h=h, b=b: nc.tensor.matmul(po_[:, h * 128:(h + 1) * 128], qrTdm[:, b, h, :], rbS[:, b, h, :],
                                                           start=False, stop=(b == 3)),
                     r=[r_qrTdm, r_rbS], w=[r_po_], sig=(h == 3 and b == 3))
        g4 = [(1.0 - 2.0 ** (-5.0 - h)) ** 4 for h in range(4)]
        for b in range(4):
            pk, r_pk = pX[0]
            S.op(V, lambda b=b: nc.vector.tensor_tensor(out=krdS[:], in0=krb[:],
                                                        in1=kdec_s[:, b * 4:(b + 1) * 4].unsqueeze(2).broadcast_to([128, 4, 128]), op=ALU.mult),
                 r=[r_krb, r_kdec_s], w=[r_krdS])
            for h in range(4):
                S.op(PE, lambda h=h, b=b: nc.tensor.matmul(pk[:, h * 128:(h + 1) * 128], krdS[:, h, :], vrb[:, h, :],
                                                           start=True, stop=True),
                     r=[r_krdS, r_vrb], w=[r_pk], sig=(h == 3))
            for h in range(4):
                S.op(V, lambda h=h, b=b: nc.vector.scalar_tensor_tensor(out=rstS[:, b, h, :], in0=rstS[:, b, h, :], scalar=float(g4[h]),
                                                                        in1=pk[:, h * 128:(h + 1) * 128], op0=ALU.mult, op1=ALU.add),
                     r=[r_rstS, r_pk, r_rbS], w=[r_rstS])
        S.dma(SP, lambda: nc.sync.dma_start(out=ret_s.rearrange("b h d e -> d b h e"), in_=rstS[:]), r=[r_rstS])

    if STAGE >= 5:
        run_until(tile_body(NT, True), None)
    barrier(s_res, "smp")
    esS.close()
    if STAGE < 3:
        barrier(R.ALL, 'end')
        es1.close()
        return nc, es
    barrier(p1_res, "p1")
    es1.close()
    es2 = ExitStack()
    p2_res = []

    def sb2(name, shape, dt=F32):
        t = es2.enter_context(nc.sbuf_tensor(name, list(shape), dt))
        r = R(name)
        p2_res.append(r)
        return t, r

    wg = [sb2(f"wg{i}", [128, 8, DEXP], BF16) for i in range(2)]
    wu = [sb2(f"wu{i}", [128, 8, DEXP], BF16) for i in range(2)]
    wd = [sb2(f"wd{i}", [128, 2, D], BF16) for i in range(2)]
    Xs = [sb2(f"Xs{i}", [128, 3, D], BF16) for i in range(2)]
    XT = [sb2(f"XT{i}", [128, 8, SCH], BF16) for i in range(2)]
    sa, r_sa = sb2("sa", [128, 2, SCH], F32)
    hh = [sb2(f"hh{i}", [128, 2, SCH], BF16) for i in range(2)]
    Ys = [sb2(f"Ys{i}", [128, 3, D], F32) for i in range(2)]
    cc = 0
    for e in range(NEXP):
        wg_t, r_wg = wg[e % 2]
        wu_t, r_wu = wu[e % 2]
        wd_t, r_wd = wd[e % 2]
        S.dma(P, lambda: nc.gpsimd.dma_start(out=wg_t[:], in_=w_ge[e].rearrange("(k p) f -> p k f", p=128)), w=[r_wg])
        S.dma(P, lambda: nc.gpsimd.dma_start(out=wu_t[:], in_=w_ue[e].rearrange("(k p) f -> p k f", p=128)), w=[r_wu])
        S.dma(P, lambda: nc.gpsimd.dma_start(out=wd_t[:], in_=w_de[e].rearrange("(c p) n -> p c n", p=128)), w=[r_wd])
        for sc in range(CAP // SCH):
            row0 = e * CAP + sc * SCH
            Xs_t, r_Xs = Xs[cc % 2]
            XT_t, r_XT = XT[cc % 2]
            hh_t, r_hh = hh[cc % 2]
            Ys_t, r_Ys = Ys[cc % 2]
            cc += 1
            S.dma(SP, lambda: nc.sync.dma_start(out=Xs_t[:], in_=X_scr[row0:row0 + SCH, :].rearrange("(t p) d -> p t d", p=128)),
                  w=[r_Xs])
            for st in range(SCH // 128):
                if st % 2 == 0:
                    pt, r_pt = pT[0][0][:], pT[0][1]
                else:
                    pt, r_pt = pO[0][:].bitcast(BF16), pO[1]
                for k in range(8):
                    S.op(PE, lambda k=k, st=st, pt=pt: nc.tensor.transpose(pt[:, k * 128:(k + 1) * 128], Xs_t[:, st, k * 128:(k + 1) * 128],
                                                                           ident_b[:]),
                         r=[r_Xs, r_ident_b], w=[r_pt], sig=(k == 7))
                S.op(A if st % 2 == 0 else V,
                     (lambda st=st, pt=pt: nc.scalar.copy(out=XT_t[:, :, st * 128:(st + 1) * 128], in_=pt.rearrange("p (k t) -> p k t", k=8)))
                     if st % 2 == 0 else
                     (lambda st=st, pt=pt: nc.vector.tensor_copy(out=XT_t[:, :, st * 128:(st + 1) * 128], in_=pt.rearrange("p (k t) -> p k t", k=8))),
                     r=[r_pt], w=[r_XT])
            banks = {("a", 0): pG[0], ("a", 1): pG[1], ("u", 0): pS[0], ("u", 1): pS[1]}
            for tag, wt, r_w in (("a", wg_t, r_wg), ("u", wu_t, r_wu)):
                for fc in range(2):
                    pb, r_pb = banks[(tag, fc)]
                    for k in range(8):
                        S.op(PE, lambda k=k, fc=fc, pb=pb, wt=wt: nc.tensor.matmul(
                            pb[:, 0:SCH], wt[:, k, fc * 128:(fc + 1) * 128], XT_t[:, k, :], start=(k == 0), stop=(k == 7)),
                            r=[r_w, r_XT], w=[r_pb], sig=(k == 7))
            for fc in range(2):
                pa, r_pa = banks[("a", fc)]
                pu, r_pu = banks[("u", fc)]
                S.op(A, lambda fc=fc, pa=pa: nc.scalar.activation(out=sa[:, fc, :], in_=pa[:, 0:SCH], func=AF.Silu),
                     r=[r_pa], w=[r_sa])
                S.op(V, lambda fc=fc, pu=pu: nc.vector.tensor_tensor(out=hh_t[:, fc, :], in0=pu[:, 0:SCH], in1=sa[:, fc, :], op=ALU.mult),
                     r=[r_pu, r_sa], w=[r_hh])
            for st in range(SCH // 128):
                for n in range(2):
                    pd, r_pd = pX[n]
                    for fc in range(2):
                        S.op(PE, lambda fc=fc, n=n, st=st, pd=pd: nc.tensor.matmul(
                            pd[:], hh_t[:, fc, st * 128:(st + 1) * 128], wd_t[:, fc, n * 512:(n + 1) * 512],
                            start=(fc == 0), stop=(fc == 1)),
                            r=[r_hh, r_wd], w=[r_pd], sig=(fc == 1))
                    if n == 0:
                        S.op(A, lambda st=st, n=n, pd=pd: nc.scalar.copy(out=Ys_t[:, st, n * 512:(n + 1) * 512], in_=pd[:]),
                             r=[r_pd], w=[r_Ys])
                    else:
                        S.op(V, lambda st=st, n=n, pd=pd: nc.vector.tensor_copy(out=Ys_t[:, st, n * 512:(n + 1) * 512], in_=pd[:]),
                             r=[r_pd], w=[r_Ys])
            S.dma(SP, lambda: nc.sync.dma_start(out=Y_scr[row0:row0 + SCH, :].rearrange("(t p) d -> p t d", p=128), in_=Ys_t[:]),
                  r=[r_Ys])

    if STAGE < 4:
        barrier(R.ALL, 'end')
        es2.close()
        return nc, es
    barrier(p2_res, "p2")
    es2.close()
    es3 = ExitStack()
    p3_res = []

    def sb3(name, shape, dt=F32):
        t = es3.enter_context(nc.sbuf_tensor(name, list(shape), dt))
        r = R(name)
        p3_res.append(r)
        return t, r

    x2r = [sb3(f"x2r{i}", [128, D]) for i in range(2)]
    y1r = [sb3(f"y1r{i}", [128, D]) for i in range(2)]
    y2r = [sb3(f"y2r{i}", [128, D]) for i in range(2)]
    yo = [sb3(f"yo{i}", [128, D]) for i in range(2)]
    junk3, r_junk3 = sb3("junk3", [128, D], BF16)
    st3, r_st3 = sb3("st3", [128, 2])
    eps3, r_eps3 = sb3("eps3", [128, 1])
    fing_bc, r_fing = sb3("fing_bc", [128, D])
    gt2_bc, r_gt2 = sb3("gt2_bc", [128, D])
    S.dma(SP, lambda: nc.sync.dma_start(out=fing_bc[:], in_=fin_g.partition_broadcast(128)), w=[r_fing])
    S.dma(SP, lambda: nc.sync.dma_start(out=gt2_bc[:], in_=gt2_scr.partition_broadcast(128)), r=[r_gt2scr], w=[r_gt2])
    S.op(V, lambda: nc.vector.memset(eps3[:], EPS), w=[r_eps3])

    def combine_tile(ti, M, gt2ap, r_g2, out_ap):
        x2_t, r_x2t = x2r[ti % 2]
        y1_t, r_y1 = y1r[ti % 2]
        y2_t, r_y2 = y2r[ti % 2]
        yo_t, r_yo = yo[ti % 2]
        rows = slice(ti * 128, ti * 128 + M)
        S.dma(SP, lambda: nc.sync.dma_start(out=x2_t[0:M, :], in_=x2_scr[rows, :]), w=[r_x2t])
        for c, (yt_, r_y) in enumerate(((y1_t, r_y1), (y2_t, r_y2))):
            S.dma(P, lambda c=c, yt_=yt_: nc.gpsimd.indirect_dma_start(
                out=yt_[:], out_offset=None, in_=Y_scr,
                in_offset=bass.IndirectOffsetOnAxis(ap=idx_all[:, ti, c:c + 1], axis=0)),
                r=[r_idx, r_ytrash], w=[r_y])
        S.op(V, lambda: nc.vector.tensor_scalar(out=y1_t[0:M, :], in0=y1_t[0:M, :], scalar1=wts_all[0:M, ti, 0:1], scalar2=None,
                                                op0=ALU.mult), r=[r_y1, r_wts], w=[r_y1])
        S.op(V, lambda: nc.vector.scalar_tensor_tensor(out=y1_t[0:M, :], in0=y2_t[0:M, :], scalar=wts_all[0:M, ti, 1:2],
                                                       in1=y1_t[0:M, :], op0=ALU.mult, op1=ALU.add),
             r=[r_y1, r_y2, r_wts], w=[r_y1])
        S.op(P, lambda: nc.gpsimd.tensor_tensor(out=y1_t[0:M, :], in0=y1_t[0:M, :], in1=gt2ap, op=ALU.mult),
             r=[r_y1, r_g2], w=[r_y1])
        S.op(V, lambda: nc.vector.tensor_tensor(out=x2_t[0:M, :], in0=x2_t[0:M, :], in1=y1_t[0:M, :], op=ALU.add),
             r=[r_y1, r_x2t], w=[r_x2t])
        S.op(A, lambda: nc.scalar.activation(out=junk3[0:M, :], in_=x2_t[0:M, :], func=AF.Square, accum_out=st3[0:M, 0:1]),
             r=[r_x2t], w=[r_junk3, r_st3])
        S.op(A, lambda: nc.scalar.activation(out=st3[0:M, 0:1], in_=st3[0:M, 0:1], func=AF.Sqrt, scale=1.0 / D, bias=eps3[0:M, 0:1]),
             r=[r_st3, r_eps3], w=[r_st3])
        S.op(V, lambda: nc.vector.reciprocal(out=st3[0:M, 0:1], in_=st3[0:M, 0:1]), r=[r_st3], w=[r_st3])
        S.op(V, lambda: nc.vector.scalar_tensor_tensor(out=yo_t[0:M, :], in0=x2_t[0:M, :], scalar=st3[0:M, 0:1],
                                                       in1=fing_bc[0:M, :], op0=ALU.mult, op1=ALU.mult),
             r=[r_x2t, r_st3, r_fing], w=[r_yo])
        S.dma(SP, lambda: nc.sync.dma_start(out=out_ap, in_=yo_t[0:M, :]), r=[r_yo])

    for ti in range(NT):
        combine_tile(ti, 128, gt2_bc[:], r_gt2, y_p[ti * 128:(ti + 1) * 128, :])
    if STAGE >= 5:
        gt2s, r_gt2s = sb3("gt2s", [16, D])
        S.dma(SP, lambda: nc.sync.dma_start(out=gt2s[:], in_=mods_scr[:, 5 * D:6 * D]), r=[r_modsscr], w=[r_gt2s])
        combine_tile(NT, 16, gt2s[:], r_gt2s, y_s)

    barrier(R.ALL, "end")
    es3.close()
    return nc, es


_CACHE = {}


def _consts(T, PAST):
    f32 = np.float32
    c = {}
    c["c_ident"] = np.eye(128, dtype=f32)
    k = np.arange(128)
    c["c_mask"] = (k[:, None] <= k[None, :]).astype(f32)
    c["c_ut"] = (k[:, None] < k[None, :]).astype(f32)

    def rope_tab(pos):
        cols = []
        for d in (64, 128):
            half = d // 2
            inv = np.power(f32(10000.0), (-(f32(2.0) / f32(d)) * np.arange(half, dtype=f32)).astype(f32)).astype(f32)
            ang = (pos.astype(f32)[:, None] * inv[None, :]).astype(f32).astype(np.float64)
            cs, sn = np.cos(ang), np.sin(ang)
            cols += [cs, cs, -sn, sn]
        return np.concatenate(cols, axis=1).astype(f32)
    c["c_rope_p"] = rope_tab(np.arange(T))
    c["c_rope_s"] = rope_tab(PAST + (np.arange(16) % 4))
    gam = 1.0 - 2.0 ** (-5.0 - np.arange(4))
    sc = 128.0 ** -0.5
    j = np.arange(128)[:, None, None]
    i = np.arange(128)[None, None, :]
    g = gam[None, :, None]
    dect = np.where(i >= j, sc * g ** np.maximum(i - j, 0), 0.0)
    c["c_dect"] = dect.reshape(128, 512).astype(f32)
    qd = (g ** (i + 1.0)) * np.ones((128, 1, 1))
    c["c_qdec"] = qd.reshape(128, 512).astype(f32)
    c["c_kdec"] = (sc * gam[None, :] ** (127.0 - np.arange(128)[:, None])).astype(f32)
    ec = np.zeros((128, 18), f32)
    ec[:, :16] = np.arange(16)[None, :] * CAP
    ec[:, 16] = TRASH + np.arange(128)
    ec[:, 17] = TRASH + 128 + np.arange(128)
    c["c_ec"] = ec
    r16 = np.arange(16)
    ds = np.zeros((128, 4, 128))
    for h in range(4):
        for jj in range(16):
            for ii in range(16):
                if jj // 4 == ii // 4 and ii % 4 >= jj % 4:
                    ds[jj, h, ii] = sc * gam[h] ** (ii % 4 - jj % 4)
    c["c_dect_s"] = ds.reshape(128, 512).astype(f32)
    qs = np.zeros((128, 4, 4, 128))
    ks = np.zeros((128, 4, 4))
    mn = np.zeros((128, 4, 32))
    for b in range(4):
        for h in range(4):
            for t in range(4):
                qs[:, b, h, 4 * b + t] = gam[h] ** (t + 1.0)
                ks[4 * b + t, b, h] = sc * gam[h] ** (3.0 - t)
        for kk in range(16):
            for col in range(32):
                if kk // 4 == b and kk % 4 <= col % 4:
                    mn[kk, b, col] = 1.0
    c["c_qdec_s"] = qs.reshape(128, 2048).astype(f32)
    c["c_kdec_s"] = ks.reshape(128, 16).astype(f32)
    c["c_masknew"] = mn.reshape(128, 128).astype(f32)
    sl = np.zeros((32, 4, 4, 16))
    for row in range(32):
        for b in range(4):
            sl[row, b, row // 8, 4 * b + row % 4] = 1.0
    c["c_sel"] = sl.reshape(32, 256).astype(f32)
    sm = np.zeros((32, 2))
    sm[:, 1] = (np.arange(32) // 4) % 2
    sm[:, 0] = 1.0 - sm[:, 1]
    c["c_smask"] = sm.astype(f32)
    c["c_pcol"] = np.arange(128, dtype=f32)[:, None]
    return c


def kernel(**inp):
    f32 = np.float32
    xpr = np.asarray(inp["x_prompt"], f32)
    B, T, _ = xpr.shape
    xsm = np.asarray(inp["x_sample"], f32)
    DB, TS, _ = xsm.shape
    pt = np.asarray(inp["page_table"])
    NPG = pt.shape[1]
    ck = np.asarray(inp["cache_k"], f32)
    NPHYS = ck.shape[1]
    ncores = B
    nb = DB // ncores
    assert nb * TS == 16
    key = (T, NPG, NPHYS)
    nc, es = build(T, NPG, NPHYS)
    cst = _consts(T, NPG * 128)
    w_er = np.asarray(inp["w_expert_router"], f32)[0]
    common = {
        "w_ada": np.asarray(inp["w_ada"], f32)[0], "b_ada": np.asarray(inp["b_ada"], f32)[0][None, :],
        "g_mix": np.asarray(inp["norm_mix_g"], f32), "g_ffn": np.asarray(inp["norm_ffn_g"], f32),
        "w_in": np.asarray(inp["w_in"], f32)[0],
        "lam_p": np.concatenate([np.asarray(inp[n], f32)[0] for n in ("lambda_q1", "lambda_k1", "lambda_q2", "lambda_k2")])[None, :],
        "beta": np.asarray(inp["beta_mix"], f32), "w_out": np.asarray(inp["w_out"], f32)[0],
        "w_rt": np.ascontiguousarray(np.concatenate([np.asarray(inp["w_group"], f32)[0],
                                                     w_er.transpose(1, 0, 2).reshape(D, 16)], axis=1)),
        "b_rt": np.concatenate([np.asarray(inp["b_group"], f32)[0], np.asarray(inp["b_expert_router"], f32)[0].reshape(16)])[None, :],
        "w_ge": np.asarray(inp["w_gate_e"], f32)[0], "w_ue": np.asarray(inp["w_up_e"], f32)[0],
        "w_de": np.asarray(inp["w_down_e"], f32)[0], "fin_g": np.asarray(inp["final_g"], f32)[None, :],
    }
    common.update(cst)
    cpr = np.asarray(inp["c_prompt"], f32)
    csm = np.asarray(inp["c_sample"], f32)
    in_maps = []
    ckv2 = np.concatenate([ck[0].reshape(NPHYS * 128, 512),
                           np.asarray(inp["cache_v"], f32)[0].reshape(NPHYS * 128, 512)], axis=1)
    sret = np.asarray(inp["state_ret"], f32)[0]
    for c in range(ncores):
        m = dict(common)
        m["ckv"] = ckv2
        m["ptab"] = np.ascontiguousarray(pt[c * nb:(c + 1) * nb].astype(np.int32).reshape(1, -1))
        m["st_ret"] = np.ascontiguousarray(sret[c * nb:(c + 1) * nb])
        m["xp"] = np.ascontiguousarray(xpr[c])
        m["xs"] = np.ascontiguousarray(xsm[c * nb:(c + 1) * nb].reshape(16, D))
        m["cp_rep"] = np.ascontiguousarray(np.repeat(cpr[c:c + 1], 128, axis=0))
        m["cs_rep"] = np.ascontiguousarray(np.repeat(csm[c * nb:(c + 1) * nb], TS, axis=0))
        in_maps.append(m)
    res = run_bass_kernel_spmd(nc, in_maps, core_ids=list(range(ncores)))
    try:
        es.close()
    except Exception:
        pass
    rr = res.results
    y_prompt = np.stack([r["y_p"] for r in rr]).astype(f32)
    y_sample = np.concatenate([r["y_s"].reshape(nb, TS, D) for r in rr]).astype(f32)
    k_prompt = np.stack([r["k_p"].reshape(T, 8, 64) for r in rr])[None].astype(f32)
    v_prompt = np.stack([r["v_p"].reshape(T, 4, 128) for r in rr])[None].astype(f32)
    ret_prompt = np.stack([r["ret_p"] for r in rr])[None].astype(f32)
    k_sample = np.concatenate([r["k_s"].reshape(nb, TS, 8, 64) for r in rr])[None].astype(f32)
    v_sample = np.concatenate([r["v_s"].reshape(nb, TS, 4, 128) for r in rr])[None].astype(f32)
    ret_sample = np.concatenate([r["ret_s"] for r in rr])[None].astype(f32)
    return (y_prompt, y_sample, k_prompt, v_prompt, ret_prompt, k_sample, v_sample, ret_sample)
```

```python
import math
from contextlib import ExitStack
import numpy as np
import concourse.bass as bass
import concourse.mybir as mybir
from concourse.bass_utils import run_bass_kernel_spmd

F32 = mybir.dt.float32
BF16 = mybir.dt.bfloat16
I32 = mybir.dt.int32
AF = mybir.ActivationFunctionType
ALU = mybir.AluOpType
AX = mybir.AxisListType

D = 1024
NCOL = 3584
NEXP = 16
DEXP = 256
EPS = 1e-6
GN_EPS = 1e-5
LAM_INIT = 0.2
CAP = 1152
SCH = 384
TRASH = NEXP * CAP


class R:
    __slots__ = ("name", "w", "rd", "excl")

    ALL = []

    def __init__(self, name):
        self.name = name
        self.w = None
        self.rd = {}
        self.excl = False
        R.ALL.append(self)


class Sched:
    def __init__(self, nc, es):
        self.nc, self.es = nc, es
        self.engs = {"pe": nc.tensor, "act": nc.scalar, "dve": nc.vector, "pool": nc.gpsimd, "sp": nc.sync}
        self.sems, self.cnt = {}, {}
        for e in ("pe", "act", "dve", "pool"):
            self.sems[e] = es.enter_context(nc.semaphore("s_" + e))
            self.cnt[e] = 0
        self.seen = {e: {} for e in self.engs}
        self.nops = {e: 0 for e in self.engs}

    def _deps(self, r, w):
        d = {}

        def add(k, v):
            if d.get(k, 0) < v:
                d[k] = v
        for x in r:
            if x.w is not None:
                add(*x.w)
            if x.excl:
                for k, v in x.rd.items():
                    add(k, v)
        for x in w:
            if x.w is not None:
                add(*x.w)
            for k, v in x.rd.items():
                add(k, v)
        return d

    def _wait(self, E, deps):
        for k, v in deps.items():
            if k == E and E == "pe":
                continue
            if self.seen[E].get(k, 0) >= v:
                continue
            self.engs[E].wait_ge(self.sems[k], v)
            self.seen[E][k] = v

    def _mark(self, tok, r, w):
        k, v = tok
        for x in r:
            if x.excl:
                x.w = tok
                x.rd = {}
            elif x.rd.get(k, 0) < v:
                x.rd[k] = v
        for x in w:
            x.w = tok
            x.rd = {}

    def op(self, E, fn, r=(), w=(), sig=True):
        self._wait(E, self._deps(r, w))
        ins = fn()
        self.nops[E] += 1
        if sig:
            self.cnt[E] += 1
            ins.then_inc(self.sems[E], 1)
            tok = (E, self.cnt[E])
        else:
            tok = (E, self.cnt[E] + 1)
        self._mark(tok, r, w)
        return ins

    def dma(self, Q, fn, r=(), w=(), key=None):
        self._wait(Q, self._deps(r, w))
        if key is None:
            key = ("L_" + w[0].name) if w else ("S_" + r[0].name)
        if key not in self.sems:
            self.sems[key] = self.es.enter_context(self.nc.semaphore(key))
            self.cnt[key] = 0
        ins = fn()
        self.nops[Q] += 1
        self.cnt[key] += 16
        ins.then_inc(self.sems[key], 16)
        tok = (key, self.cnt[key])
        self._mark(tok, r, w)
        return tok

    def finish(self, resources):
        d = {}
        for x in resources:
            if x.w is not None and d.get(x.w[0], 0) < x.w[1]:
                d[x.w[0]] = x.w[1]
            for k, v in x.rd.items():
                if d.get(k, 0) < v:
                    d[k] = v
        self._wait("sp", d)


def build(T, NPG, NPHYS, debug=False):
    import os
    STAGE = int(os.environ.get("KSTAGE", "9"))
    KCUT = int(os.environ.get("KCUT", "99"))
    R.ALL = []
    NT = T // 128
    nc = bass.Bass("TRN2", target_bir_lowering=False)
    es = ExitStack()
    S = Sched(nc, es)

    def din(name, shape, dt=F32):
        return nc.dram_tensor(name, list(shape), dt, kind="ExternalInput").ap()

    def dout(name, shape, dt=F32):
        return nc.dram_tensor(name, list(shape), dt, kind="ExternalOutput").ap()

    xp = din("xp", [T, D])
    xs = din("xs", [16, D])
    cp_rep = din("cp_rep", [128, D])
    cs_rep = din("cs_rep", [16, D])
    w_ada = din("w_ada", [D, 6 * D])
    b_ada = din("b_ada", [1, 6 * D])
    g_mix = din("g_mix", [1, D])
    g_ffn = din("g_ffn", [1, D])
    w_in = din("w_in", [D, NCOL])
    lam_p = din("lam_p", [1, 256])
    beta = din("beta", [1, D])
    w_out = din("w_out", [D, D])
    w_rt = din("w_rt", [D, 20])
    b_rt = din("b_rt", [1, 20])
    w_ge = din("w_ge", [NEXP, D, DEXP])
    w_ue = din("w_ue", [NEXP, D, DEXP])
    w_de = din("w_de", [NEXP, DEXP, D])
    fin_g = din("fin_g", [1, D])
    c_ident = din("c_ident", [128, 128])
    c_mask = din("c_mask", [128, 128])
    c_ut = din("c_ut", [128, 128])
    c_rope_p = din("c_rope_p", [T, 384])
    c_rope_s = din("c_rope_s", [16, 384])
    c_dect = din("c_dect", [128, 512])
    c_qdec = din("c_qdec", [128, 512])
    c_kdec = din("c_kdec", [128, 4])
    c_ec = din("c_ec", [128, 18])
    c_dect_s = din("c_dect_s", [128, 512])
    c_qdec_s = din("c_qdec_s", [128, 2048])
    c_kdec_s = din("c_kdec_s", [128, 16])
    c_masknew = din("c_masknew", [128, 128])
    c_sel = din("c_sel", [32, 256])
    c_smask = din("c_smask", [32, 2])
    c_pcol = din("c_pcol", [128, 1])
    ckv = din("ckv", [NPHYS * 128, 1024])
    ptab = din("ptab", [1, 4 * NPG], I32)
    st_ret = din("st_ret", [4, 4, 128, 128])

    y_p = dout("y_p", [T, D])
    y_s = dout("y_s", [16, D])
    k_p = dout("k_p", [T, 512])
    v_p = dout("v_p", [T, 512])
    ret_p = dout("ret_p", [4, 128, 128])
    k_s = dout("k_s", [16, 512])
    v_s = dout("v_s", [16, 512])
    ret_s = dout("ret_s", [4, 4, 128, 128])

    NTT = NT + 1
    x2_scr = nc.dram_tensor("x2_scr", [NTT * 128, D], F32, kind="Internal").ap()
    X_scr = nc.dram_tensor("X_scr", [TRASH + 256, D], BF16, kind="Internal").ap()
    Y_scr = nc.dram_tensor("Y_scr", [TRASH + 256, D], F32, kind="Internal").ap()
    mods_scr = nc.dram_tensor("mods_scr", [16, 6 * D], F32, kind="Internal").ap()
    gt2_scr = nc.dram_tensor("gt2_scr", [1, D], F32, kind="Internal").ap()

    def sb(name, shape, dt=F32):
        t = es.enter_context(nc.sbuf_tensor(name, list(shape), dt))
        return t, R(name)

    def ps(name, shape, dt=F32):
        t = es.enter_context(nc.psum_tensor(name, list(shape), dt))
        r = R(name)
        r.excl = True
        return t, r

    V, A, P, PE, SP = "dve", "act", "pool", "pe", "sp"

    def barrier(resources, tag):
        d = {}
        for x in resources:
            if x.w is not None and d.get(x.w[0], 0) < x.w[1]:
                d[x.w[0]] = x.w[1]
            for k, v in x.rd.items():
                if d.get(k, 0) < v:
                    d[k] = v
        for E in ("pe", "act", "dve", "pool", "sp"):
            S._wait(E, d)

    pG = [ps(f"pG{i}", [128, 512]) for i in range(2)]
    pT = [ps(f"pT{i}", [128, 1024], BF16) for i in range(1)]
    pS = [ps(f"pS{i}", [128, 512]) for i in range(2)]
    pO = ps("pO", [128, 512])
    pX = [ps(f"pX{i}", [128, 512]) for i in range(2)]
    pO_reg = [R(f"pO_r{i}") for i in range(3)]
    gctr = [0]

    def nextG():
        gctr[0] += 1
        return pG[gctr[0] % 2]

    ident_b, r_ident_b = sb("ident_b", [128, 128], BF16)
    ident_f, r_ident_f = sb("ident_f", [128, 128], F32)
    mask_b, r_mask = sb("mask_b", [128, 128], BF16)
    ut_b, r_ut = sb("ut_b", [128, 128], BF16)
    ones_b, r_ones = sb("ones_b", [128, 128], BF16)
    dect, r_dect = sb("dect", [128, 512], F32)
    qdec, r_qdec = sb("qdec", [128, 512], BF16)
    kdec, r_kdec = sb("kdec", [128, 4], F32)
    ecr, r_ecr = sb("ecr", [128, 18], F32)
    beta_fm, r_betafm = sb("beta_fm", [128, 8])
    brt_bc, r_brt = sb("brt_bc", [128, 20])
    wrt, r_wrt = sb("wrt", [128, 8, 20], F32)
    a2_bc, r_a2 = sb("a2_bc", [128, D])
    sh2_bc, r_sh2 = sb("sh2_bc", [128, D])
    a1_fm, r_a1fm = sb("a1_fm", [128, 8])
    sh1_fm, r_sh1fm = sb("sh1_fm", [128, 8])
    neglam, r_neglam = sb("neglam", [128, 1])
    idx_all, r_idx = sb("idx_all", [128, NTT, 2], I32)
    wts_all, r_wts = sb("wts_all", [128, NTT, 2], F32)
    base_bc, r_base = sb("base_bc", [128, 16], F32)
    win, r_win = sb("win", [128, 8, NCOL], BF16)
    wout, r_wout = sb("wout", [128, 8, D], BF16)

    def load_const(dst, rdst, src, eng=SP):
        if eng == SP:
            S.dma(SP, lambda: nc.sync.dma_start(out=dst, in_=src), w=[rdst])
        else:
            S.dma(P, lambda: nc.gpsimd.dma_start(out=dst, in_=src), w=[rdst])

    load_const(ident_f[:], r_ident_f, c_ident)
    load_const(ident_b[:], r_ident_b, c_ident, P)
    load_const(mask_b[:], r_mask, c_mask, P)
    load_const(ut_b[:], r_ut, c_ut, P)
    load_const(dect[:], r_dect, c_dect)
    load_const(qdec[:], r_qdec, c_qdec, P)
    load_const(kdec[:], r_kdec, c_kdec)
    load_const(ecr[:], r_ecr, c_ec)
    load_const(brt_bc[:], r_brt, b_rt.partition_broadcast(128))
    load_const(wrt[:], r_wrt, w_rt.rearrange("(k p) n -> p k n", p=128))
    S.op(V, lambda: nc.vector.memset(ones_b[:], 1.0), w=[r_ones])
    S.op(V, lambda: nc.vector.memset(base_bc[:], 0.0), w=[r_base])
    w_in_v = w_in.rearrange("(k p) n -> p k n", p=128)
    for k in range(8):
        S.dma(P, lambda k=k: nc.gpsimd.dma_start(out=win[:, k, :], in_=w_in_v[:, k, :]), w=[r_win], key="L_win")
    S.dma(P, lambda: nc.gpsimd.dma_start(out=wout[:], in_=w_out.rearrange("(k p) n -> p k n", p=128)), w=[r_wout])

    es_setup = ExitStack()

    def sbt(name, shape, dt=F32):
        t = es_setup.enter_context(nc.sbuf_tensor(name, list(shape), dt))
        return t, R(name)

    lam_t, r_lamt = sbt("lam_t", [128, 256])
    lam_j, r_lamj = sbt("lam_j", [128, 64])
    lam_s, r_lams = sbt("lam_s", [128, 2])
    S.dma(SP, lambda: nc.sync.dma_start(out=lam_t[:], in_=lam_p.partition_broadcast(128)), w=[r_lamt])
    for i in range(2):
        S.op(V, lambda i=i: nc.vector.scalar_tensor_tensor(
            out=lam_j[:], in0=lam_t[:, 128 * i:128 * i + 64], scalar=1.0, in1=lam_t[:, 128 * i + 64:128 * i + 128],
            op0=ALU.mult, op1=ALU.mult, accum_out=lam_s[:, i:i + 1]),
            r=[r_lamt], w=[r_lamj, r_lams])
    S.op(A, lambda: nc.scalar.activation(out=lam_s[:], in_=lam_s[:], func=AF.Exp), r=[r_lams], w=[r_lams])
    S.op(V, lambda: nc.vector.scalar_tensor_tensor(out=neglam[:], in0=lam_s[:, 1:2], scalar=-LAM_INIT,
                                                   in1=lam_s[:, 0:1], op0=ALU.add, op1=ALU.subtract),
         r=[r_lams], w=[r_neglam])

    if KCUT == 1:
        barrier(R.ALL, 'end')
        return nc, es
    modp, r_modp = sbt("modp", [128, 6 * D])
    mods, r_mods = sbt("mods", [16, 6 * D])
    beta_bc, r_beta = sbt("beta_bc", [128, D])
    S.dma(SP, lambda: nc.sync.dma_start(out=beta_bc[:], in_=beta.partition_broadcast(128)), w=[r_beta])
    c_f, r_cf = sbt("c_f", [128, D])
    c_b, r_cb = sbt("c_b", [128, D], BF16)
    cs_f, r_csf = sbt("cs_f", [16, D])
    cs_b, r_csb = sbt("cs_b", [16, D], BF16)
    cT, r_cT = sbt("cT", [128, 8, 128], BF16)
    csT, r_csT = sbt("csT", [128, 8, 16], BF16)
    bada, r_bada = sbt("bada", [128, 6 * D])
    S.dma(SP, lambda: nc.sync.dma_start(out=c_f[:], in_=cp_rep), w=[r_cf])
    S.dma(SP, lambda: nc.sync.dma_start(out=cs_f[:], in_=cs_rep), w=[r_csf])
    S.dma(SP, lambda: nc.sync.dma_start(out=bada[:], in_=b_ada.partition_broadcast(128)), w=[r_bada])
    S.op(A, lambda: nc.scalar.activation(out=c_b[:], in_=c_f[:], func=AF.Silu), r=[r_cf], w=[r_cb])
    S.op(A, lambda: nc.scalar.activation(out=cs_b[:], in_=cs_f[:], func=AF.Silu), r=[r_csf], w=[r_csb])
    pt, r_pt = pT[0]
    for k in range(8):
        S.op(PE, lambda k=k: nc.tensor.transpose(pt[:, k * 128:(k + 1) * 128], c_b[:, k * 128:(k + 1) * 128], ident_b[:]),
             r=[r_cb, r_ident_b], w=[r_pt], sig=(k == 7))
    S.op(A, lambda: nc.scalar.copy(out=cT[:].rearrange("p k t -> p (k t)"), in_=pt[:]), r=[r_pt], w=[r_cT])
    for k in range(8):
        S.op(PE, lambda k=k: nc.tensor.transpose(pt[:, k * 16:(k + 1) * 16], cs_b[:, k * 128:(k + 1) * 128], ident_b[0:16, 0:16]),
             r=[r_csb, r_ident_b], w=[r_pt], sig=(k == 7))
    S.op(A, lambda: nc.scalar.copy(out=csT[:].rearrange("p k t -> p (k t)"), in_=pt[:, 0:128]), r=[r_pt], w=[r_csT])
    wa = [sbt(f"wa{i}", [128, 8, 512], BF16) for i in range(2)]
    w_ada_v = w_ada.rearrange("(k p) n -> p k n", p=128)
    for g in range(12):
        wt, r_w = wa[g % 2]
        S.dma(P, lambda g=g, wt=wt: nc.gpsimd.dma_start(out=wt[:], in_=w_ada_v[:, :, g * 512:(g + 1) * 512]), w=[r_w])
        for (lT, r_l, M, dst, r_d) in ((cT, r_cT, 128, modp, r_modp), (csT, r_csT, 16, mods, r_mods)):
            pg, r_pg = nextG()
            for k in range(8):
                S.op(PE, lambda k=k, pg=pg, lT=lT, M=M, wt=wt: nc.tensor.matmul(
                    pg[0:M, :], lT[:, k, :], wt[:, k, :], start=(k == 0), stop=(k == 7)),
                    r=[r_l, r_w], w=[r_pg], sig=(k == 7))
            S.op(V, lambda pg=pg, M=M, dst=dst, g=g: nc.vector.tensor_tensor(
                out=dst[0:M, g * 512:(g + 1) * 512], in0=pg[0:M, :], in1=bada[0:M, g * 512:(g + 1) * 512], op=ALU.add),
                r=[r_pg, r_bada], w=[r_d])
    gm_bc, r_gm = sbt("gm_bc", [128, D])
    gf_bc, r_gf = sbt("gf_bc", [128, D])
    a1_tok, r_a1t = sbt("a1_tok", [128, D])
    S.dma(SP, lambda: nc.sync.dma_start(out=gm_bc[:], in_=g_mix.partition_broadcast(128)), w=[r_gm])
    S.dma(SP, lambda: nc.sync.dma_start(out=gf_bc[:], in_=g_ffn.partition_broadcast(128)), w=[r_gf])
    S.op(V, lambda: nc.vector.scalar_tensor_tensor(out=a1_tok[:], in0=modp[:, D:2 * D], scalar=1.0, in1=gm_bc[:],
                                                   op0=ALU.add, op1=ALU.mult), r=[r_modp, r_gm], w=[r_a1t])
    S.op(V, lambda: nc.vector.scalar_tensor_tensor(out=a2_bc[:], in0=modp[:, 4 * D:5 * D], scalar=1.0, in1=gf_bc[:],
                                                   op0=ALU.add, op1=ALU.mult), r=[r_modp, r_gf], w=[r_a2])
    S.op(V, lambda: nc.vector.tensor_copy(out=sh2_bc[:], in_=modp[:, 3 * D:4 * D]), r=[r_modp], w=[r_sh2])
    r_gt2scr = R("gt2scr")
    S.dma(SP, lambda: nc.sync.dma_start(out=gt2_scr, in_=modp[0:1, 5 * D:6 * D]), r=[r_modp], w=[r_gt2scr], key="S_modp")
    S.op(V, lambda: nc.vector.scalar_tensor_tensor(out=mods[:, D:2 * D], in0=mods[:, D:2 * D], scalar=1.0, in1=gm_bc[0:16, :],
                                                   op0=ALU.add, op1=ALU.mult), r=[r_mods, r_gm], w=[r_mods])
    S.op(V, lambda: nc.vector.scalar_tensor_tensor(out=mods[:, 4 * D:5 * D], in0=mods[:, 4 * D:5 * D], scalar=1.0, in1=gf_bc[0:16, :],
                                                   op0=ALU.add, op1=ALU.mult), r=[r_mods, r_gf], w=[r_mods])
    for (src, r_src, dstt, r_dst) in ((a1_tok, r_a1t, a1_fm, r_a1fm), (modp, r_modp, sh1_fm, r_sh1fm), (beta_bc, r_beta, beta_fm, r_betafm)):
        for half in range(2):
            pg, r_pg = nextG()
            for kk in range(4):
                k = half * 4 + kk
                S.op(PE, lambda k=k, kk=kk, pg=pg, src=src: nc.tensor.transpose(
                    pg[:, kk * 128:(kk + 1) * 128], src[:, k * 128:(k + 1) * 128], ident_f[:]),
                    r=[r_src, r_ident_f], w=[r_pg], sig=(kk == 3))
            S.op(V, lambda pg=pg, dstt=dstt, half=half: nc.vector.tensor_copy(
                out=dstt[:, half * 4:half * 4 + 4], in_=pg[:].rearrange("p (k t) -> p k t", k=4)[:, :, 0]),
                r=[r_pg], w=[r_dst])

    r_modsscr = R("modsscr")
    S.dma(SP, lambda: nc.sync.dma_start(out=mods_scr, in_=mods[:]), r=[r_mods], w=[r_modsscr], key="S_mods")
    for k in range(8):
        S.op(V, lambda k=k: nc.vector.scalar_tensor_tensor(out=wout[:, k, :], in0=wout[:, k, :], scalar=beta_fm[:, k:k + 1],
                                                           in1=modp[:, 2 * D:3 * D], op0=ALU.mult, op1=ALU.mult),
             r=[r_wout, r_betafm, r_modp], w=[r_wout])
    setup_res = [r_lamt, r_lamj, r_lams, r_modp, r_cf, r_cb, r_csf, r_csb, r_cT, r_csT, r_bada,
                 wa[0][1], wa[1][1], r_gm, r_gf, r_a1t, r_mods, r_beta]
    setup_guard = R("setup_guard")

    def barrier(resources, tag):
        d = {}
        for x in resources:
            if x.w is not None and d.get(x.w[0], 0) < x.w[1]:
                d[x.w[0]] = x.w[1]
            for k, v in x.rd.items():
                if d.get(k, 0) < v:
                    d[k] = v
        for E in ("pe", "act", "dve", "pool", "sp"):
            S._wait(E, d)

    if KCUT == 2:
        barrier(R.ALL, 'end')
        return nc, es
    barrier(setup_res, "setup")
    es_setup.close()

    es1 = ExitStack()

    def sb1(name, shape, dt=F32):
        t = es1.enter_context(nc.sbuf_tensor(name, list(shape), dt))
        return t, R(name)

    p1_res = []

    def sb1r(name, shape, dt=F32):
        t, r = sb1(name, shape, dt)
        p1_res.append(r)
        return t, r

    rst, r_rst = sb1r("rst", [128, 4, 128], F32)
    rbf, r_rbf = sb1r("rbf", [128, 4, 128], BF16)
    S.op(V, lambda: nc.vector.memset(rst[:].rearrange("p a b -> p (a b)"), 0.0), w=[r_rst])
    S.op(V, lambda: nc.vector.memset(rbf[:].rearrange("p a b -> p (a b)"), 0.0), w=[r_rbf])

    def ring(name, n, shape, dt=F32):
        out = []
        for i in range(n):
            out.append(sb1r(f"{name}{i}", shape, dt))
        return out

    xt = ring("xt", 2, [128, D])
    rt = ring("rt", 2, [128, 384])
    xn, r_xn = sb1r("xn", [128, D], BF16)
    junk, r_junk = xn, r_xn
    ymix, r_ymix = xn, r_xn
    hT, r_hT = sb1r("hT", [128, 8, 128], BF16)
    yT, r_yT = hT, r_hT
    st4, r_st4 = sb1r("st4", [128, 8])
    tt, r_tt = sb1r("tt", [128, D])
    t1, r_t1 = tt[:, 0:512], r_tt
    t2, r_t2 = tt[:, 512:1024], r_tt
    h2f, r_h2f = tt, r_tt
    rb_, r_rb = sb1r("rb_", [128, 512], BF16)
    qb, r_qb = rb_, r_rb
    kb, r_kb = rb_, r_rb
    qrb, r_qrb = rb_, r_rb
    QT = ring("QT", 2, [128, 4, 128], BF16)
    kf = ring("kf", 1, [128, 512])
    vf = ring("vf", 1, [128, 512])
    qrT_r = ring("qrT", 2, [128, 4, 128], BF16)
    qrTd_r = ring("qrTd", 2, [128, 4, 128], BF16)
    krb_r = ring("krb", 2, [128, 4, 128], BF16)
    krd_r = ring("krd", 2, [128, 4, 128], BF16)
    krT_r = ring("krT", 2, [128, 4, 128], BF16)
    vrb_r = ring("vrb", 2, [128, 4, 128], BF16)
    sg_r = ring("sg", 2, [128, 512], BF16)
    PTr = ring("PT", 2, [128, 512], BF16)
    odr = ring("od", 2, [128, 4, 128])
    rl, r_rl = sb1r("rl", [128, 2])
    sTm, r_sTm = sb1r("sTm", [128, 4, 128], BF16)
    h2b = ring("h2b", 2, [128, D], BF16)
    h2T, r_h2T = sb1r("h2T", [128, 4, 128], F32)
    rlg, r_rlg = sb1r("rlg", [128, 20])
    rs_, r_rs = sb1r("rs_", [128, 100])
    oh16, r_oh16 = sb1r("oh16", [128, 2, 16])
    mb, r_mb = sb1r("mb", [128, 16], BF16)
    slotf, r_slotf = sb1r("slotf", [128, 4])

    def rope(pg, r_pg, H, tab, r_tab, coff, out_ap, r_out):
        hf = 256 // H
        zv = pg[:].rearrange("p (h two f) -> p h two f", h=H, two=2)
        cosv = tab[:, coff:coff + 2 * hf].rearrange("p (two f) -> p two f", two=2).unsqueeze(1).broadcast_to([128, H, 2, hf])
        sinv = tab[:, coff + 2 * hf:coff + 4 * hf].rearrange("p (two f) -> p two f", two=2)
        t1v = t1.rearrange("p (h two f) -> p h two f", h=H, two=2)
        t2v = t2.rearrange("p (h two f) -> p h two f", h=H, two=2)
        S.op(V, lambda: nc.vector.tensor_tensor(out=t1v, in0=zv, in1=cosv, op=ALU.mult), r=[r_pg, r_tab], w=[r_t1])
        for half in range(2):
            S.op(V, lambda half=half: nc.vector.tensor_tensor(
                out=t2v[:, :, half, :], in0=zv[:, :, 1 - half, :],
                in1=sinv[:, half, :].unsqueeze(1).broadcast_to([128, H, hf]), op=ALU.mult),
                r=[r_pg, r_tab], w=[r_t2])
        S.op(V, lambda: nc.vector.tensor_tensor(out=out_ap, in0=t1, in1=t2, op=ALU.add), r=[r_t1], w=[r_out])

    def transpose4(src, r_src, dst, r_dst, copy_eng=A):
        pt, r_pt = pT[0]
        for c in range(4):
            S.op(PE, lambda c=c: nc.tensor.transpose(pt[:, c * 128:(c + 1) * 128], src[:, c * 128:(c + 1) * 128], ident_b[:]),
                 r=[r_src, r_ident_b], w=[r_pt], sig=(c == 3))
        if copy_eng == A:
            S.op(A, lambda: nc.scalar.copy(out=dst, in_=pt[:, 0:512]), r=[r_pt], w=[r_dst])
        else:
            S.op(V, lambda: nc.vector.tensor_copy(out=dst, in_=pt[:, 0:512]), r=[r_pt], w=[r_dst])

    def rms_rstd(src, r_src, dst_col, r_dst, n, eps, M=128):
        S.op(A, lambda: nc.scalar.activation(out=junk[0:M, 0:n], in_=src, func=AF.Square, accum_out=dst_col),
             r=[r_src], w=[r_junk, r_dst])
        S.op(A, lambda: nc.scalar.activation(out=dst_col, in_=dst_col, func=AF.Ln, scale=1.0 / n, bias=eps_t[0:M, 0:1]),
             r=[r_dst, r_eps], w=[r_dst])
        S.op(A, lambda: nc.scalar.activation(out=dst_col, in_=dst_col, func=AF.Exp, scale=-0.5), r=[r_dst], w=[r_dst])

    eps_t, r_eps = sb1r("eps_t", [128, 2])
    S.op(V, lambda: nc.vector.memset(eps_t[:, 0:1], EPS), w=[r_eps])
    S.op(V, lambda: nc.vector.memset(eps_t[:, 1:2], GN_EPS), w=[r_eps])

    esKV = ExitStack()
    KT = esKV.enter_context(nc.sbuf_tensor("KT", [128, 4, T], BF16))
    r_KT = [R(f"KT{i}") for i in range(NT)]
    Vt = esKV.enter_context(nc.sbuf_tensor("Vt", [128, NT, 4, 129], BF16))
    r_V = [R(f"V{i}") for i in range(NT)]
    S.op(P, lambda: nc.gpsimd.memset(Vt[:].rearrange("p a b c -> p (a b c)"), 1.0), w=r_V)
    out_res = []
    if KCUT == 3:
        barrier(R.ALL, 'end')
        return nc, es


    ztile, r_ztile = xt[1]
    S.op(V, lambda: nc.vector.memset(ztile[:], 0.0), w=[r_ztile])
    r_ytrash = R("ytrash")
    S.dma(SP, lambda: nc.sync.dma_start(out=Y_scr[TRASH:TRASH + 128, :], in_=ztile[:]), r=[r_ztile], w=[r_ytrash], key="S_ztile")
    S.dma(SP, lambda: nc.sync.dma_start(out=Y_scr[TRASH + 128:TRASH + 256, :], in_=ztile[:]), r=[r_ztile], w=[r_ytrash], key="S_ztile")
    zb, r_zb = h2b[0]
    S.op(V, lambda: nc.vector.memset(zb[:], 0.0), w=[r_zb])
    NXR = (TRASH + 256) // 128
    r_xscr = R("xscr")
    for a in range(0, NXR, 16):
        b = min(NXR, a + 16)
        S.dma(SP, lambda a=a, b=b: nc.sync.dma_start(
            out=X_scr[a * 128:b * 128, :].rearrange("(t p) d -> p t d", p=128),
            in_=zb[:].unsqueeze(1).broadcast_to([128, b - a, D])), r=[r_zb], w=[r_xscr], key="S_zero")
    S.op(V, lambda: nc.vector.tensor_copy(out=idx_all[:, NT, :], in_=ecr[:, 16:18]), r=[r_ecr], w=[r_idx])
    S.op(V, lambda: nc.vector.memset(wts_all[:, NT, :], 0.0), w=[r_wts])

    def route_tile(ti, x2_t, r_x2, a2ap, r_a2_, sh2ap, r_sh2_, M):
        h2b_t, r_h2b = h2b[ti % 2]
        rms_rstd(x2_t[0:M, :], r_x2, st4[0:M, 1:2], r_st4, D, EPS, M)
        S.op(V, lambda: nc.vector.scalar_tensor_tensor(out=h2f[0:M, :], in0=x2_t[0:M, :], scalar=st4[0:M, 1:2], in1=a2ap,
                                                       op0=ALU.mult, op1=ALU.mult), r=[r_x2, r_st4, r_a2_], w=[r_h2f])
        S.op(P, lambda: nc.gpsimd.tensor_tensor(out=h2f[0:M, :], in0=h2f[0:M, :], in1=sh2ap, op=ALU.add),
             r=[r_h2f, r_sh2_], w=[r_h2f])
        S.op(A, lambda: nc.scalar.copy(out=h2b_t[0:M, :], in_=h2f[0:M, :]), r=[r_h2f], w=[r_h2b])
        yield 1
        pgr, r_pgr = pX[0]
        for half in range(2):
            pg, r_pg = nextG()
            for kk in range(4):
                k = half * 4 + kk
                S.op(PE, lambda k=k, kk=kk, pg=pg: nc.tensor.transpose(pg[:, kk * 128:kk * 128 + M], h2f[0:M, k * 128:(k + 1) * 128],
                                                                       ident_f[0:M, 0:M]),
                     r=[r_h2f, r_ident_f], w=[r_pg], sig=(kk == 3))
            yield 1
            S.op(V, lambda pg=pg: nc.vector.tensor_copy(
                out=h2T[:, :, 0:M], in_=pg[:].rearrange("p (k t) -> p k t", k=4)[:, :, 0:M]),
                r=[r_pg], w=[r_h2T])
            yield 1
            for kk in range(4):
                k = half * 4 + kk
                S.op(PE, lambda k=k, kk=kk: nc.tensor.matmul(pgr[0:M, 0:20], h2T[:, kk, 0:M], wrt[:, k, :], start=(k == 0), stop=(k == 7)),
                     r=[r_h2T, r_wrt], w=[r_pgr], sig=(kk == 3))
        pg, r_pg = pgr, r_pgr
        yield 1
        q = rs_
        rr = [r_rs]

        def v(fn, r=(), w=()):
            S.op(V, fn, r=list(r) + rr, w=list(w) + rr)
        S.op(V, lambda pg=pg: nc.vector.tensor_tensor(out=rlg[0:M, :], in0=pg[0:M, 0:20], in1=brt_bc[0:M, :], op=ALU.add),
             r=[r_pg, r_brt], w=[r_rlg])
        lg = rlg[0:M, 0:4]
        v(lambda: nc.vector.tensor_reduce(out=q[0:M, 0:1], in_=lg, axis=AX.X, op=ALU.max), r=[r_rlg])
        v(lambda: nc.vector.tensor_scalar(out=q[0:M, 4:8], in0=lg, scalar1=q[0:M, 0:1], scalar2=None, op0=ALU.is_ge), r=[r_rlg])
        v(lambda: nc.vector.tensor_scalar(out=q[0:M, 1:2], in0=q[0:M, 0:1], scalar1=-1.0, scalar2=None, op0=ALU.mult))
        S.op(A, lambda: nc.scalar.activation(out=q[0:M, 8:12], in_=lg, func=AF.Exp, bias=q[0:M, 1:2], accum_out=q[0:M, 2:3]),
             r=[r_rlg, r_rs], w=[r_rs])
        v(lambda: nc.vector.reciprocal(out=q[0:M, 2:3], in_=q[0:M, 2:3]))
        v(lambda: nc.vector.tensor_tensor(out=q[0:M, 16:32].rearrange("p (g e) -> p g e", g=4),
                                          in0=rlg[0:M, 4:20].rearrange("p (g e) -> p g e", g=4),
                                          in1=q[0:M, 4:8].unsqueeze(2).broadcast_to([M, 4, 4]), op=ALU.mult), r=[r_rlg])
        v(lambda: nc.vector.tensor_reduce(out=q[0:M, 12:16], in_=q[0:M, 16:32].rearrange("p (g e) -> p e g", g=4), axis=AX.X, op=ALU.add))
        le = q[0:M, 12:16]
        v(lambda: nc.vector.tensor_reduce(out=q[0:M, 3:4], in_=le, axis=AX.X, op=ALU.max))
        v(lambda: nc.vector.tensor_scalar(out=q[0:M, 32:36], in0=le, scalar1=q[0:M, 3:4], scalar2=None, op0=ALU.is_ge))
        v(lambda: nc.vector.scalar_tensor_tensor(out=q[0:M, 36:40], in0=q[0:M, 32:36], scalar=-1e30, in1=le, op0=ALU.mult, op1=ALU.add))
        v(lambda: nc.vector.tensor_reduce(out=q[0:M, 40:41], in_=q[0:M, 36:40], axis=AX.X, op=ALU.max))
        v(lambda: nc.vector.tensor_scalar(out=q[0:M, 44:48], in0=q[0:M, 36:40], scalar1=q[0:M, 40:41], scalar2=None, op0=ALU.is_ge))
        v(lambda: nc.vector.tensor_scalar(out=q[0:M, 42:43], in0=q[0:M, 3:4], scalar1=-1.0, scalar2=None, op0=ALU.mult))
        S.op(A, lambda: nc.scalar.activation(out=q[0:M, 41:42], in_=q[0:M, 40:41], func=AF.Exp, bias=q[0:M, 42:43]),
             r=[r_rs], w=[r_rs])
        v(lambda: nc.vector.tensor_scalar(out=q[0:M, 48:49], in0=q[0:M, 41:42], scalar1=1.0, scalar2=None, op0=ALU.add))
        v(lambda: nc.vector.reciprocal(out=q[0:M, 48:49], in_=q[0:M, 48:49]))
        v(lambda: nc.vector.tensor_tensor(out=q[0:M, 49:50], in0=q[0:M, 41:42], in1=q[0:M, 48:49], op=ALU.mult))
        v(lambda: nc.vector.tensor_scalar(out=q[0:M, 48:50], in0=q[0:M, 48:50], scalar1=q[0:M, 2:3], scalar2=None, op0=ALU.mult))
        for c, off in ((0, 32), (1, 44)):
            S.op(V, lambda c=c, off=off: nc.vector.tensor_tensor(
                out=oh16[0:M, c, :].rearrange("p (g e) -> p g e", g=4),
                in0=q[0:M, 4:8].unsqueeze(2).broadcast_to([M, 4, 4]),
                in1=q[0:M, off:off + 4].unsqueeze(1).broadcast_to([M, 4, 4]), op=ALU.mult), r=[r_rs], w=[r_oh16])
        S.op(V, lambda: nc.vector.tensor_tensor(out=mb[0:M, :], in0=oh16[0:M, 0, :], in1=oh16[0:M, 1, :], op=ALU.add),
             r=[r_oh16], w=[r_mb])
        yield 1
        pc, r_pc = nextG()
        S.op(PE, lambda: nc.tensor.matmul(pc[0:M, 0:16], ut_b[0:M, 0:M], mb[0:M, :], start=True, stop=True),
             r=[r_ut, r_mb], w=[r_pc], sig=False)
        S.op(PE, lambda: nc.tensor.matmul(pc[:, 16:32], ones_b[0:M, :], mb[0:M, :], start=True, stop=True),
             r=[r_ones, r_mb], w=[r_pc])
        yield 1
        S.op(V, lambda: nc.vector.tensor_tensor(out=q[0:M, 50:66], in0=pc[0:M, 0:16], in1=base_bc[0:M, :], op=ALU.add),
             r=[r_pc, r_base, r_rs], w=[r_rs])
        v(lambda: nc.vector.tensor_tensor(out=q[0:M, 66:82], in0=q[0:M, 50:66], in1=ecr[0:M, 0:16], op=ALU.add), r=[r_ecr])
        S.op(V, lambda: nc.vector.tensor_tensor(out=base_bc[:], in0=base_bc[:], in1=pc[:, 16:32], op=ALU.add),
             r=[r_pc, r_base], w=[r_base])
        for c in range(2):
            S.op(V, lambda c=c: nc.vector.scalar_tensor_tensor(out=q[0:M, 82:98], in0=oh16[0:M, c, :], scalar=1.0, in1=q[0:M, 66:82],
                                                               op0=ALU.mult, op1=ALU.mult, accum_out=slotf[0:M, c:c + 1]),
                 r=[r_oh16, r_rs], w=[r_rs, r_slotf])
            S.op(V, lambda c=c: nc.vector.scalar_tensor_tensor(out=q[0:M, 82:98], in0=oh16[0:M, c, :], scalar=1.0, in1=q[0:M, 50:66],
                                                               op0=ALU.mult, op1=ALU.mult, accum_out=slotf[0:M, 2 + c:3 + c]),
                 r=[r_oh16, r_rs], w=[r_rs, r_slotf])
        sf = [r_slotf]
        S.op(V, lambda: nc.vector.tensor_scalar(out=slotf[0:M, 2:4], in0=slotf[0:M, 2:4], scalar1=float(CAP), scalar2=None, op0=ALU.is_lt),
             r=sf, w=sf)
        S.op(V, lambda: nc.vector.tensor_tensor(out=slotf[0:M, 0:2], in0=slotf[0:M, 0:2], in1=ecr[0:M, 16:18],
                                                op=ALU.subtract), r=sf + [r_ecr], w=sf)
        S.op(V, lambda: nc.vector.tensor_tensor(out=slotf[0:M, 0:2], in0=slotf[0:M, 0:2], in1=slotf[0:M, 2:4], op=ALU.mult), r=sf, w=sf)
        S.op(V, lambda: nc.vector.tensor_tensor(out=slotf[0:M, 0:2], in0=slotf[0:M, 0:2], in1=ecr[0:M, 16:18],
                                                op=ALU.add), r=sf + [r_ecr], w=sf)
        S.op(V, lambda: nc.vector.tensor_copy(out=idx_all[0:M, ti, :], in_=slotf[0:M, 0:2]), r=sf, w=[r_idx])
        S.op(V, lambda: nc.vector.tensor_tensor(out=wts_all[0:M, ti, :], in0=q[0:M, 48:50], in1=slotf[0:M, 2:4], op=ALU.mult),
             r=sf + [r_rs], w=[r_wts])
        for c in range(2):
            S.dma(P, lambda c=c: nc.gpsimd.indirect_dma_start(
                out=X_scr, out_offset=bass.IndirectOffsetOnAxis(ap=idx_all[:, ti, c:c + 1], axis=0),
                in_=h2b_t[:], in_offset=None), r=[r_h2b, r_idx], key=f"S_h2b{ti % 2}")

    smp = {}

    def tile_body(i, SMP=False):
        x_t, r_x = xt[i % 2]
        od, r_od = odr[i % 2]
        smp['od'], smp['r_od'] = od, r_od
        qrT, r_qrT = qrT_r[i % 2]
        qrTd, r_qrTd = qrTd_r[i % 2]
        krb, r_krb = krb_r[i % 2]
        krd, r_krd = krd_r[i % 2]
        krT, r_krT = krT_r[i % 2]
        vrb, r_vrb = vrb_r[i % 2]
        sg, r_sg = sg_r[i % 2]
        smp.update(qrT=qrT, r_qrT=r_qrT, krb=krb, r_krb=r_krb, vrb=vrb, r_vrb=r_vrb)
        r_t, r_r = rt[i % 2]
        tok = slice(i * 128, (i + 1) * 128)
        if SMP:
            S.op(V, lambda: nc.vector.memset(x_t[:], 0.0), w=[r_x])
            S.op(V, lambda: nc.vector.memset(r_t[:], 0.0), w=[r_r])
            S.dma(SP, lambda: nc.sync.dma_start(out=x_t[0:16, :], in_=xs), w=[r_x])
            S.dma(SP, lambda: nc.sync.dma_start(out=r_t[0:16, :], in_=c_rope_s), w=[r_r])
        else:
            S.dma(SP, lambda: nc.sync.dma_start(out=x_t[:], in_=xp[tok, :]), w=[r_x])
            S.dma(SP, lambda: nc.sync.dma_start(out=r_t[:], in_=c_rope_p[tok, :]), w=[r_r])
        rms_rstd(x_t[:], r_x, st4[:, 0:1], r_st4, D, EPS)
        if SMP:
            S.op(V, lambda: nc.vector.scalar_tensor_tensor(out=tt[:], in0=x_t[:], scalar=st4[:, 0:1], in1=smp["mA"][:, 0, :],
                                                           op0=ALU.mult, op1=ALU.mult), r=[r_x, r_st4, smp["r_mA"]], w=[r_tt])
            S.op(V, lambda: nc.vector.tensor_tensor(out=xn[:], in0=tt[:], in1=smp["mA"][:, 1, :], op=ALU.add),
                 r=[r_tt, smp["r_mA"]], w=[r_xn])
        else:
            S.op(V, lambda: nc.vector.tensor_scalar(out=xn[:], in0=x_t[:], scalar1=st4[:, 0:1], scalar2=None, op0=ALU.mult),
                 r=[r_x, r_st4], w=[r_xn])
        pt, r_pt = pT[0]
        for k in range(8):
            S.op(PE, lambda k=k: nc.tensor.transpose(pt[:, k * 128:(k + 1) * 128], xn[:, k * 128:(k + 1) * 128], ident_b[:]),
                 r=[r_xn, r_ident_b], w=[r_pt], sig=(k == 7))
        if SMP:
            S.op(A, lambda: nc.scalar.copy(out=hT[:].rearrange("p k t -> p (k t)"), in_=pt[:]), r=[r_pt], w=[r_hT])
        else:
            for k in range(8):
                S.op(A, lambda k=k: nc.scalar.activation(out=hT[:, k, :], in_=pt[:, k * 128:(k + 1) * 128], func=AF.Identity,
                                                         scale=a1_fm[:, k:k + 1], bias=sh1_fm[:, k:k + 1]),
                     r=[r_pt, r_a1fm, r_sh1fm], w=[r_hT])
        if KCUT == 4:
            return
        yield 1
        q_t, r_q = QT[i % 2]
        k_f, r_kf = kf[0]
        v_f, r_vf = vf[0]
        for g in range(int(os.environ.get('KG', '7'))):
            pg, r_pg = nextG()
            for k in range(8):
                S.op(PE, lambda k=k, g=g, pg=pg: nc.tensor.matmul(pg[:], hT[:, k, :], win[:, k, g * 512:(g + 1) * 512],
                                                                  start=(k == 0), stop=(k == 7)),
                     r=[r_hT, r_win], w=[r_pg], sig=(k == 7))
            yield 1
            if g == 0:
                rope(pg, r_pg, 8, r_t, r_r, 0, qb[:], r_qb)
                yield 1
                transpose4(qb, r_qb, q_t[:].rearrange("p c t -> p (c t)"), r_q)
            elif g == 1:
                rope(pg, r_pg, 8, r_t, r_r, 0, k_f[:], r_kf)
                if SMP:
                    S.dma(SP, lambda: nc.sync.dma_start(out=k_s, in_=k_f[0:16, :]), r=[r_kf])
                else:
                    S.dma(SP, lambda: nc.sync.dma_start(out=k_p[tok, :], in_=k_f[:]), r=[r_kf])
                S.op(A, lambda: nc.scalar.copy(out=kb[:], in_=k_f[:]), r=[r_kf], w=[r_kb])
                yield 1
                if SMP:
                    transpose4(kb, r_kb, smp["KTs"][:].rearrange("p c t -> p (c t)"), smp["r_KTs"])
                else:
                    transpose4(kb, r_kb, KT[:, :, tok], r_KT[i])
            elif g == 2:
                S.op(A, lambda pg=pg: nc.scalar.copy(out=v_f[:], in_=pg[:]), r=[r_pg], w=[r_vf])
                if SMP:
                    S.op(V, lambda pg=pg: nc.vector.tensor_copy(out=smp["Vs"][:], in_=pg[:]), r=[r_pg], w=[smp["r_Vs"]])
                    S.dma(SP, lambda: nc.sync.dma_start(out=v_s, in_=v_f[0:16, :]), r=[r_vf])
                else:
                    S.op(V, lambda pg=pg: nc.vector.tensor_copy(out=Vt[:, i, :, 0:128], in_=pg[:].rearrange("p (h e) -> p h e", h=4)),
                         r=[r_pg], w=[r_V[i]])
                    S.dma(SP, lambda: nc.sync.dma_start(out=v_p[tok, :], in_=v_f[:]), r=[r_vf])
            elif g == 3:
                rope(pg, r_pg, 4, r_t, r_r, 128, qrb[:], r_qrb)
                yield 1
                transpose4(qrb, r_qrb, qrT[:].rearrange("p c t -> p (c t)"), r_qrT)
                S.op(P, lambda: nc.gpsimd.tensor_tensor(out=qrTd[:].rearrange("p c t -> p (c t)"),
                                                        in0=qrT[:].rearrange("p c t -> p (c t)"), in1=qdec[:], op=ALU.mult),
                     r=[r_qrT, r_qdec], w=[r_qrTd])
            elif g == 4:
                rope(pg, r_pg, 4, r_t, r_r, 128, krb[:].rearrange("p h d -> p (h d)"), r_krb)
                yield 1
                transpose4(krb[:].rearrange("p h d -> p (h d)"), r_krb, krT[:].rearrange("p c t -> p (c t)"), r_krT)
                S.op(P, lambda: nc.gpsimd.tensor_tensor(out=krd[:], in0=krb[:],
                                                        in1=kdec[:].unsqueeze(2).broadcast_to([128, 4, 128]), op=ALU.mult),
                     r=[r_krb, r_kdec], w=[r_krd])
            elif g == 5:
                S.op(A, lambda pg=pg: nc.scalar.copy(out=vrb[:].rearrange("p h d -> p (h d)"), in_=pg[:]), r=[r_pg], w=[r_vrb])
            else:
                S.op(A, lambda pg=pg: nc.scalar.activation(out=t1, in_=pg[:], func=AF.Exp, scale=-1.0), r=[r_pg], w=[r_t1])
                S.op(V, lambda: nc.vector.tensor_scalar(out=t1, in0=t1, scalar1=1.0, scalar2=None, op0=ALU.add), r=[r_t1], w=[r_t1])
                S.op(V, lambda: nc.vector.reciprocal(out=t1, in_=t1), r=[r_t1], w=[r_t1])
                S.op(V, lambda pg=pg: nc.vector.tensor_tensor(out=sg[:], in0=pg[:], in1=t1, op=ALU.mult), r=[r_pg, r_t1], w=[r_sg])
        if KCUT == 5:
            return
            yield 1
        yield 'ATTN'
        units = []
        for j in range(8):
            blocks = list(range(i + 1))
            grp = [blocks[a:a + 4] for a in range(0, len(blocks), 4)]
            for gi, gb in enumerate(grp):
                units.append((j, gi, gb, gi == 0, gi == len(grp) - 1))

        def emit_qk(u, ui):
            j, gi, gb, first, last = u
            h, s = j // 2, j % 2
            psl = slice(64 * s, 64 * s + 64)
            pS_t, r_pS = pS[ui % 2]
            for bi, kbk in enumerate(gb):
                S.op(PE, lambda bi=bi, kbk=kbk: nc.tensor.matmul(
                    pS_t[:, bi * 128:(bi + 1) * 128], KT[psl, h, kbk * 128:(kbk + 1) * 128], q_t[psl, h, :],
                    start=True, stop=True),
                    r=[r_KT[kbk], r_q], w=[r_pS], sig=(bi == len(gb) - 1))

        def emit_av(u, ui):
            j, gi, gb, first, last = u
            h, s = j // 2, j % 2
            pS_t, r_pS = pS[ui % 2]
            P_t, r_P = PTr[ui % 2]
            n = len(gb) * 128
            S.op(A, lambda: nc.scalar.activation(out=P_t[:, 0:n], in_=pS_t[:, 0:n], func=AF.Exp, scale=0.125),
                 r=[r_pS], w=[r_P])
            if gb[-1] == i:
                S.op(V, lambda: nc.vector.tensor_tensor(out=P_t[:, n - 128:n], in0=P_t[:, n - 128:n], in1=mask_b[:], op=ALU.mult),
                     r=[r_P, r_mask], w=[r_P])
            reg = 0
            po, r_po = (pO if j % 2 == 0 else pX[1])
            for bi, kbk in enumerate(gb):
                S.op(PE, lambda bi=bi, kbk=kbk: nc.tensor.matmul(
                    po[:, reg * 129:reg * 129 + 129], P_t[:, bi * 128:(bi + 1) * 128], Vt[:, kbk, h, :],
                    start=(first and bi == 0), stop=(last and bi == len(gb) - 1)),
                    r=[r_P, r_V[kbk]], w=[r_po], sig=(bi == len(gb) - 1))
            if last:
                c0 = reg * 129
                S.op(V, lambda: nc.vector.reciprocal(out=rl[:, s:s + 1], in_=po[:, c0 + 128:c0 + 129]), r=[r_po], w=[r_rl])
                if s == 0:
                    S.op(V, lambda: nc.vector.tensor_scalar(out=od[:, h, :], in0=po[:, c0:c0 + 128], scalar1=rl[:, 0:1],
                                                            scalar2=None, op0=ALU.mult), r=[r_po, r_rl], w=[r_od])
                else:
                    S.op(V, lambda: nc.vector.tensor_scalar(out=rl[:, 1:2], in0=rl[:, 1:2], scalar1=neglam[:, 0:1],
                                                            scalar2=None, op0=ALU.mult), r=[r_rl, r_neglam], w=[r_rl])
                    S.op(V, lambda: nc.vector.scalar_tensor_tensor(out=od[:, h, :], in0=po[:, c0:c0 + 128], scalar=rl[:, 1:2],
                                                                   in1=od[:, h, :], op0=ALU.mult, op1=ALU.add),
                         r=[r_po, r_rl, r_od], w=[r_od])

        if SMP:
            units = []
            sample_attention(q_t, r_q)
        elif STAGE < 1:
            units = []
            S.op(V, lambda: nc.vector.memset(od[:].rearrange('p a b -> p (a b)'), 0.5), w=[r_od])
        else:
            emit_qk(units[0], 0)
        for ui, u in enumerate(units):
            if ui + 1 < len(units):
                emit_qk(units[ui + 1], ui + 1)
            emit_av(u, ui)
            yield 1

        yield 'TAIL'
        for h in range(4):
            S.op(A, lambda h=h: nc.scalar.activation(out=junk[:, 0:128], in_=od[:, h, :], func=AF.Square,
                                                     accum_out=st4[:, 4 + h:5 + h]), r=[r_od], w=[r_junk, r_st4])
        c08 = (1.0 - LAM_INIT) ** 2
        S.op(A, lambda: nc.scalar.activation(out=st4[:, 4:8], in_=st4[:, 4:8], func=AF.Ln, scale=1.0 / (128 * c08),
                                             bias=eps_t[:, 0:1]), r=[r_st4, r_eps], w=[r_st4])
        S.op(A, lambda: nc.scalar.activation(out=st4[:, 4:8], in_=st4[:, 4:8], func=AF.Exp, scale=-0.5), r=[r_st4], w=[r_st4])
        for h in range(4):
            S.op(V, lambda h=h: nc.vector.tensor_scalar(out=ymix[:, h * 128:(h + 1) * 128], in0=od[:, h, :],
                                                        scalar1=st4[:, 4 + h:5 + h], scalar2=None, op0=ALU.mult),
                 r=[r_od, r_st4], w=[r_ymix])

        if KCUT == 6:
            return
        yield 1
        pg, r_pg = nextG()
        for h in range(4):
            S.op(PE, lambda h=h, pg=pg: nc.tensor.matmul(pg[:, h * 128:(h + 1) * 128], krT[:, h, :], qrT[:, h, :],
                                                         start=True, stop=True),
                 r=[r_krT, r_qrT], w=[r_pg], sig=(h == 3))
        yield 1
        dect_u, r_dect_u = (smp["dect_s"], smp["r_dect_s"]) if SMP else (dect, r_dect)
        S.op(V, lambda pg=pg: nc.vector.tensor_tensor(out=sTm[:].rearrange("p h t -> p (h t)"), in0=pg[:], in1=dect_u[:], op=ALU.mult),
             r=[r_pg, r_dect_u], w=[r_sTm])
        yield 1
        po_, r_po_ = nextG()
        if SMP:
            sample_ret_cross(po_, r_po_)
        else:
            for h in range(4):
                S.op(PE, lambda h=h, po_=po_: nc.tensor.matmul(po_[:, h * 128:(h + 1) * 128], sTm[:, h, :], vrb[:, h, :],
                                                               start=True, stop=False),
                     r=[r_sTm, r_vrb], w=[r_po_], sig=False)
                S.op(PE, lambda h=h, po_=po_: nc.tensor.matmul(po_[:, h * 128:(h + 1) * 128], qrTd[:, h, :], rbf[:, h, :],
                                                               start=False, stop=True),
                     r=[r_qrTd, r_rbf], w=[r_po_], sig=(h == 3))
            pk, r_pk = pX[0]
            for h in range(4):
                S.op(PE, lambda h=h: nc.tensor.matmul(pk[:, h * 128:(h + 1) * 128], krd[:, h, :], vrb[:, h, :],
                                                      start=True, stop=True),
                     r=[r_krd, r_vrb], w=[r_pk], sig=(h == 3))
            for h in range(4):
                cd = (1.0 - 2.0 ** (-5.0 - h)) ** 128
                S.op(V, lambda h=h, cd=cd: nc.vector.scalar_tensor_tensor(out=rst[:, h, :], in0=rst[:, h, :], scalar=float(cd),
                                                                          in1=pk[:, h * 128:(h + 1) * 128], op0=ALU.mult, op1=ALU.add),
                     r=[r_rst, r_pk], w=[r_rst])
            S.op(A, lambda: nc.scalar.copy(out=rbf[:], in_=rst[:]), r=[r_rst], w=[r_rbf])
        yield 1
        S.op(V, lambda po_=po_: nc.vector.tensor_reduce(out=rs_[:, 0:4], in_=po_[:].rearrange("p (h e) -> p h e", h=4),
                                                        axis=AX.X, op=ALU.add), r=[r_po_], w=[r_rs])
        S.op(A, lambda po_=po_: nc.scalar.activation(out=t1, in_=po_[:], func=AF.Square), r=[r_po_], w=[r_t1])
        S.op(V, lambda: nc.vector.tensor_reduce(out=rs_[:, 4:8], in_=t1.rearrange("p (h e) -> p h e", h=4),
                                                axis=AX.X, op=ALU.add), r=[r_t1], w=[r_rs])
        S.op(V, lambda: nc.vector.tensor_scalar(out=rs_[:, 0:4], in0=rs_[:, 0:4], scalar1=1.0 / 128, scalar2=None, op0=ALU.mult),
             r=[r_rs], w=[r_rs])
        S.op(V, lambda: nc.vector.tensor_tensor(out=rs_[:, 8:12], in0=rs_[:, 0:4], in1=rs_[:, 0:4], op=ALU.mult), r=[r_rs], w=[r_rs])
        S.op(V, lambda: nc.vector.scalar_tensor_tensor(out=rs_[:, 4:8], in0=rs_[:, 4:8], scalar=1.0 / 128, in1=rs_[:, 8:12],
                                                       op0=ALU.mult, op1=ALU.subtract), r=[r_rs], w=[r_rs])
        S.op(A, lambda: nc.scalar.activation(out=rs_[:, 4:8], in_=rs_[:, 4:8], func=AF.Ln, bias=eps_t[:, 1:2]),
             r=[r_rs, r_eps], w=[r_rs])
        S.op(A, lambda: nc.scalar.activation(out=rs_[:, 4:8], in_=rs_[:, 4:8], func=AF.Exp, scale=-0.5), r=[r_rs], w=[r_rs])
        for h in range(4):
            S.op(V, lambda h=h, po_=po_: nc.vector.tensor_scalar(out=t2[:, h * 128:(h + 1) * 128], in0=po_[:, h * 128:(h + 1) * 128],
                                                                 scalar1=rs_[:, h:h + 1], scalar2=rs_[:, 4 + h:5 + h],
                                                                 op0=ALU.subtract, op1=ALU.mult),
                 r=[r_po_, r_rs], w=[r_t2])
        S.op(V, lambda: nc.vector.tensor_tensor(out=ymix[:, 512:1024], in0=t2, in1=sg[:], op=ALU.mult),
             r=[r_t2, r_sg], w=[r_ymix])

        if KCUT == 7:
            return
        yield 1
        pt, r_pt = pT[0]
        for k in range(8):
            S.op(PE, lambda k=k: nc.tensor.transpose(pt[:, k * 128:(k + 1) * 128], ymix[:, k * 128:(k + 1) * 128], ident_b[:]),
                 r=[r_ymix, r_ident_b], w=[r_pt], sig=(k == 7))
        S.op(A, lambda: nc.scalar.copy(out=yT[:].rearrange("p k t -> p (k t)"), in_=pt[:]), r=[r_pt], w=[r_yT])
        yield 1
        x2_t, r_x2 = x_t, r_x
        for n in range(2):
            pg, r_pg = nextG()
            for k in range(8):
                S.op(PE, lambda k=k, n=n, pg=pg: nc.tensor.matmul(pg[:], yT[:, k, :], wout[:, k, n * 512:(n + 1) * 512],
                                                                  start=(k == 0), stop=(k == 7)),
                     r=[r_yT, r_wout], w=[r_pg], sig=(k == 7))
            yield 1
            cs_ = slice(n * 512, (n + 1) * 512)
            if SMP:
                S.op(V, lambda pg=pg, cs_=cs_: nc.vector.tensor_tensor(out=tt[:, cs_], in0=pg[:], in1=smp["gt1s"][:, cs_], op=ALU.mult),
                     r=[r_pg, smp["r_gt1s"]], w=[r_tt])
                S.op(V, lambda cs_=cs_: nc.vector.tensor_tensor(out=x2_t[:, cs_], in0=tt[:, cs_], in1=x_t[:, cs_], op=ALU.add),
                     r=[r_tt, r_x], w=[r_x2])
            else:
                S.op(V, lambda pg=pg, cs_=cs_: nc.vector.tensor_tensor(out=x2_t[:, cs_], in0=pg[:], in1=x_t[:, cs_], op=ALU.add),
                     r=[r_pg, r_x], w=[r_x2])
        if SMP:
            S.dma(SP, lambda: nc.sync.dma_start(out=x2_scr[i * 128:i * 128 + 16, :], in_=x2_t[0:16, :]), r=[r_x2], key=f"S_xt{i % 2}")
        else:
            S.dma(SP, lambda: nc.sync.dma_start(out=x2_scr[tok, :], in_=x2_t[:]), r=[r_x2], key=f"S_xt{i % 2}")

        if STAGE >= 2:
            if SMP:
                smp["load_mA"]((4, 3))
                yield from route_tile(i, x2_t, r_x2, smp["mA"][0:16, 0, :], smp["r_mA"], smp["mA"][0:16, 1, :], smp["r_mA"], 16)
            else:
                yield from route_tile(i, x2_t, r_x2, a2_bc[:], r_a2, sh2_bc[:], r_sh2, 128)

    def run_until(gen, marker):
        for v in gen:
            if v == marker:
                return True
        return False

    def step_bg(bg):
        while bg:
            gen, marker = bg[0]
            try:
                v = next(gen)
            except StopIteration:
                bg.pop(0)
                continue
            if marker is not None and v == marker:
                bg.pop(0)
                continue
            return

    if KCUT < 99 or os.environ.get("KSEQ"):
        for i_ in range(NT):
            run_until(tile_body(i_), None)
    else:
        cur = tile_body(0)
        run_until(cur, "ATTN")
        prev_tail = None
        for i_ in range(NT):
            nxt = tile_body(i_ + 1) if i_ + 1 < NT else None
            bg = []
            if prev_tail is not None:
                bg.append((prev_tail, None))
            if nxt is not None:
                bg.append((nxt, "ATTN"))
            n_units = 8 * ((i_ + 4) // 4)
            kk = max(1, -(-44 // n_units))
            while True:
                v = next(cur)
                if v == "TAIL":
                    break
                for _ in range(kk):
                    step_bg(bg)
            while bg:
                step_bg(bg)
            prev_tail = cur
            cur = nxt
        run_until(prev_tail, None)

    if KCUT < 99:
        barrier(R.ALL, 'end')
        return nc, es
    S.dma(SP, lambda: nc.sync.dma_start(out=ret_p.rearrange("h d e -> d h e"), in_=rst[:]), r=[r_rst])

    barrier(r_KT + r_V, "kv")
    esKV.close()
    esS = ExitStack()
    s_res = []

    def sbs(name, shape, dt=F32):
        t = esS.enter_context(nc.sbuf_tensor(name, list(shape), dt))
        r = R(name)
        s_res.append(r)
        return t, r

    mA, r_mA = sbs("mA", [128, 2, D])
    gt1s, r_gt1s = sbs("gt1s", [128, D])
    KTs, r_KTs = sbs("KTs", [128, 4, 128], BF16)
    Vs, r_Vs = sbs("Vs", [128, 512], BF16)
    Qb, r_Qb = sbs("Qb", [128, 4, 4, 8], BF16)
    idxp, r_idxp = sbs("idxp", [128, 4 * NPG], I32)
    pcol, r_pcol = sbs("pcol", [128, 1])
    KVpg = [sbs(f"KVpg{i}", [128, 1024]) for i in range(3)]
    Kb = [sbs(f"Kb{i}", [128, 512], BF16) for i in range(3)]
    KTp = [sbs(f"KTp{i}", [128, 512], BF16) for i in range(3)]
    Vb = [sbs(f"Vb{i}", [128, 512], BF16) for i in range(3)]
    PTd = [sbs(f"PTd{i}", [128, 32], BF16) for i in range(3)]
    Osc, r_Osc = sbs("Osc", [32, 4, 512], BF16)
    sel, r_sel = sbs("sel", [32, 256], BF16)
    smk, r_smk = sbs("smk", [32, 2])
    rl32, r_rl32 = sbs("rl32", [32, 2])
    rstS, r_rstS = sbs("rstS", [128, 4, 4, 128])
    rbS, r_rbS = sbs("rbS", [128, 4, 4, 128], BF16)
    dect_s, r_dect_s = sbs("dect_s", [128, 512], BF16)
    qdec_s, r_qdec_s = sbs("qdec_s", [128, 4, 4, 128], BF16)
    kdec_s, r_kdec_s = sbs("kdec_s", [128, 16])
    qrTdm, r_qrTdm = sbs("qrTdm", [128, 4, 4, 128], BF16)
    krdS, r_krdS = sbs("krdS", [128, 4, 128], BF16)
    mnew, r_mnew = sbs("mnew", [128, 4, 32])
    smp.update(mA=mA, r_mA=r_mA, gt1s=gt1s, r_gt1s=r_gt1s, KTs=KTs, r_KTs=r_KTs, Vs=Vs, r_Vs=r_Vs,
               dect_s=dect_s, r_dect_s=r_dect_s)
    S.op(V, lambda: nc.vector.memset(mA[:].rearrange("p a d -> p (a d)"), 0.0), w=[r_mA])
    S.op(V, lambda: nc.vector.memset(gt1s[:], 0.0), w=[r_gt1s])
    def load_mA(chs):
        for j, ch in enumerate(chs):
            S.dma(SP, lambda j=j, ch=ch: nc.sync.dma_start(out=mA[0:16, j, :], in_=mods_scr[:, ch * D:(ch + 1) * D]),
                  r=[r_modsscr], w=[r_mA])
    load_mA((1, 0))
    smp["load_mA"] = load_mA
    S.dma(SP, lambda: nc.sync.dma_start(out=gt1s[0:16, :], in_=mods_scr[:, 2 * D:3 * D]), r=[r_modsscr], w=[r_gt1s])
    S.dma(P, lambda: nc.gpsimd.dma_start(out=dect_s[:], in_=c_dect_s), w=[r_dect_s])
    S.dma(P, lambda: nc.gpsimd.dma_start(out=qdec_s[:].rearrange("p a b c -> p (a b c)"), in_=c_qdec_s), w=[r_qdec_s])
    S.dma(SP, lambda: nc.sync.dma_start(out=kdec_s[:], in_=c_kdec_s), w=[r_kdec_s])
    S.dma(SP, lambda: nc.sync.dma_start(out=mnew[:].rearrange("p a b -> p (a b)"), in_=c_masknew), w=[r_mnew])
    S.dma(P, lambda: nc.gpsimd.dma_start(out=sel[:], in_=c_sel), w=[r_sel])
    S.dma(SP, lambda: nc.sync.dma_start(out=smk[:], in_=c_smask), w=[r_smk])
    S.dma(SP, lambda: nc.sync.dma_start(out=pcol[:], in_=c_pcol), w=[r_pcol])
    S.dma(SP, lambda: nc.sync.dma_start(out=idxp[:], in_=ptab.partition_broadcast(128)), w=[r_idxp])
    S.dma(SP, lambda: nc.sync.dma_start(out=rstS[:], in_=st_ret.rearrange("b h d e -> d b h e")), w=[r_rstS])
    S.op(A, lambda: nc.scalar.copy(out=rbS[:].rearrange("p a b c -> p (a b c)"), in_=rstS[:].rearrange("p a b c -> p (a b c)")),
         r=[r_rstS], w=[r_rbS])
    S.op(V, lambda: nc.vector.tensor_scalar(out=idxp[:], in0=idxp[:], scalar1=128.0, scalar2=pcol[:, 0:1], op0=ALU.mult, op1=ALU.add),
         r=[r_idxp, r_pcol], w=[r_idxp])
    S.dma(P, lambda: nc.gpsimd.dma_start(out=wout[:], in_=w_out.rearrange("(k p) n -> p k n", p=128)), w=[r_wout])
    for k in range(8):
        S.op(V, lambda k=k: nc.vector.tensor_scalar(out=wout[:, k, :], in0=wout[:, k, :], scalar1=beta_fm[:, k:k + 1], scalar2=None,
                                                    op0=ALU.mult), r=[r_wout, r_betafm], w=[r_wout])
    S.op(V, lambda: nc.vector.scalar_tensor_tensor(out=rl32[:, 1:2], in0=smk[:, 1:2], scalar=neglam[0:32, 0:1], in1=smk[:, 0:1],
                                                   op0=ALU.mult, op1=ALU.add), r=[r_smk, r_neglam], w=[r_rl32])

    def sample_attention(q_t, r_q):
        S.op(V, lambda: nc.vector.memset(Qb[:].rearrange("p a b c -> p (a b c)"), 0.0), w=[r_Qb])
        for b in range(4):
            for s_ in range(2):
                psl = slice(64 * s_, 64 * s_ + 64)
                S.op(V, lambda b=b, s_=s_, psl=psl: nc.vector.tensor_copy(out=Qb[psl, b, :, 4 * s_:4 * s_ + 4],
                                                                          in_=q_t[psl, :, 4 * b:4 * b + 4]),
                     r=[r_q], w=[r_Qb])
        po, r_po = pO
        psm, r_psm = pX[1]
        cc_ = 0
        for b in range(4):
            base = b * NPG

            def stA(p, b=b, base=base):
                g = base + p
                KVg, r_Kg = KVpg[g % 3]
                Kb_t, r_Kb = Kb[g % 3]
                KT_t, r_KTp = KTp[g % 3]
                Vb_t, r_Vb = Vb[g % 3]
                S.dma(P, lambda: nc.gpsimd.indirect_dma_start(
                    out=KVg[:], out_offset=None, in_=ckv, in_offset=bass.IndirectOffsetOnAxis(ap=idxp[:, g:g + 1], axis=0)),
                    r=[r_idxp], w=[r_Kg])
                S.op(A, lambda: nc.scalar.copy(out=Kb_t[:], in_=KVg[:, 0:512]), r=[r_Kg], w=[r_Kb])
                S.op(V, lambda: nc.vector.tensor_copy(out=Vb_t[:], in_=KVg[:, 512:1024]), r=[r_Kg], w=[r_Vb])
                pt, r_pt = pT[0]
                for c in range(4):
                    S.op(PE, lambda c=c: nc.tensor.transpose(pt[:, c * 128:(c + 1) * 128], Kb_t[:, c * 128:(c + 1) * 128], ident_b[:]),
                         r=[r_Kb, r_ident_b], w=[r_pt], sig=(c == 3))
                S.op(V, lambda: nc.vector.tensor_copy(out=KT_t[:], in_=pt[:, 0:512]), r=[r_pt], w=[r_KTp])

            def stB(p, b=b, base=base):
                g = base + p
                KT_t, r_KTp = KTp[g % 3]
                P_t, r_P = PTd[g % 3]
                pS_t, r_pS = pS[g % 2]
                for h in range(4):
                    S.op(PE, lambda h=h: nc.tensor.matmul(
                        pS_t[:, h * 8:(h + 1) * 8], KT_t[:, h * 128:(h + 1) * 128], Qb[:, b, h, :], start=True, stop=True),
                        r=[r_KTp, r_Qb], w=[r_pS], sig=(h == 3))
                S.op(A, lambda: nc.scalar.activation(out=P_t[:], in_=pS_t[:, 0:32], func=AF.Exp, scale=0.125),
                     r=[r_pS], w=[r_P])

            def stC(p, b=b, base=base):
                g = base + p
                Vb_t, r_Vb = Vb[g % 3]
                P_t, r_P = PTd[g % 3]
                S.op(PE, lambda: nc.tensor.matmul(po[0:32, :], P_t[:], Vb_t[:], start=(p == 0), stop=False),
                     r=[r_P, r_Vb], w=[r_po], sig=False)
                S.op(PE, lambda: nc.tensor.matmul(psm[0:32, 0:1], P_t[:], ones_b[:, 0:1], start=(p == 0), stop=False),
                     r=[r_P, r_ones], w=[r_psm])

            for s_ in range(NPG + 2):
                if s_ < NPG:
                    stA(s_)
                if 0 <= s_ - 1 < NPG:
                    stB(s_ - 1)
                if 0 <= s_ - 2 < NPG:
                    stC(s_ - 2)
            cc_ = base + NPG
            P_t, r_P = PTd[cc_ % 3]
            pS_t, r_pS = pS[cc_ % 2]
            cc_ += 1
            for h in range(4):
                S.op(PE, lambda h=h, pS_t=pS_t, b=b: nc.tensor.matmul(
                    pS_t[0:16, h * 8:(h + 1) * 8], KTs[:, h, 0:16], Qb[:, b, h, :], start=True, stop=True),
                    r=[r_KTs, r_Qb], w=[r_pS], sig=(h == 3))
            S.op(A, lambda P_t=P_t, pS_t=pS_t: nc.scalar.activation(out=P_t[0:16, :], in_=pS_t[0:16, 0:32], func=AF.Exp, scale=0.125),
                 r=[r_pS], w=[r_P])
            S.op(V, lambda P_t=P_t, b=b: nc.vector.tensor_tensor(out=P_t[0:16, :], in0=P_t[0:16, :], in1=mnew[0:16, b, :], op=ALU.mult),
                 r=[r_P, r_mnew], w=[r_P])
            S.op(PE, lambda P_t=P_t: nc.tensor.matmul(po[0:32, :], P_t[0:16, :], Vs[0:16, :], start=False, stop=True),
                 r=[r_P, r_Vs], w=[r_po], sig=False)
            S.op(PE, lambda P_t=P_t: nc.tensor.matmul(psm[0:32, 0:1], P_t[0:16, :], ones_b[0:16, 0:1], start=False, stop=True),
                 r=[r_P, r_ones], w=[r_psm])
            S.op(V, lambda: nc.vector.reciprocal(out=rl32[:, 0:1], in_=psm[0:32, 0:1]), r=[r_psm], w=[r_rl32])
            S.op(V, lambda: nc.vector.tensor_tensor(out=rl32[:, 0:1], in0=rl32[:, 0:1], in1=rl32[:, 1:2], op=ALU.mult),
                 r=[r_rl32], w=[r_rl32])
            S.op(V, lambda b=b: nc.vector.tensor_scalar(out=Osc[:, b, :], in0=po[0:32, :], scalar1=rl32[:, 0:1], scalar2=None, op0=ALU.mult),
                 r=[r_po, r_rl32], w=[r_Osc])
        pg2, r_pg2 = nextG()
        for h in range(4):
            for b in range(4):
                S.op(PE, lambda h=h, b=b: nc.tensor.matmul(
                    pg2[0:16, h * 128:(h + 1) * 128], sel[:, (b * 4 + h) * 16:(b * 4 + h + 1) * 16], Osc[:, b, h * 128:(h + 1) * 128],
                    start=(b == 0), stop=(b == 3)),
                    r=[r_sel, r_Osc], w=[r_pg2], sig=(b == 3 and h == 3))
        S.op(V, lambda: nc.vector.tensor_copy(out=smp["od"][0:16, :, :].rearrange("p h e -> p (h e)"), in_=pg2[0:16, :]), r=[r_pg2], w=[smp["r_od"]])

    def sample_ret_cross(po_, r_po_):
        qrT, r_qrT, krb, r_krb, vrb, r_vrb = smp['qrT'], smp['r_qrT'], smp['krb'], smp['r_krb'], smp['vrb'], smp['r_vrb']
        for b in range(4):
            S.op(V, lambda b=b: nc.vector.tensor_tensor(out=qrTdm[:, b, :, :], in0=qrT[:], in1=qdec_s[:, b, :, :], op=ALU.mult),
                 r=[r_qrT, r_qdec_s], w=[r_qrTdm])
        for h in range(4):
            S.op(PE, lambda h=h: nc.tensor.matmul(po_[:, h * 128:(h + 1) * 128], sTm[:, h, :], vrb[:, h, :], start=True, stop=False),
                 r=[r_sTm, r_vrb], w=[r_po_], sig=False)
            for b in range(4):
                S.op(PE, lambda h=h, b=b: nc.tensor.matmul(po_[:, h * 128:(h + 1) * 128], qrTdm[:, b, h, :], rbS[:, b, h, :],
                                                           start=False, stop=(b == 3)),
                     r=[r_qrTdm, r_rbS], w=[r_po_], sig=(h == 3 and b == 3))
        g4 = [(1.0 - 2.0 ** (-5.0 - h)) ** 4 for h in range(4)]
        for b in range(4):
            pk, r_pk = pX[0]
            S.op(V, lambda b=b: nc.vector.tensor_tensor(out=krdS[:], in0=krb[:],
                                                        in1=kdec_s[:, b * 4:(b + 1) * 4].unsqueeze(2).broadcast_to([128, 4, 128]), op=ALU.mult),
                 r=[r_krb, r_kdec_s], w=[r_krdS])
            for h in range(4):
                S.op(PE, lambda h=h, b=b: nc.tensor.matmul(pk[:, h * 128:(h + 1) * 128], krdS[:, h, :], vrb[:, h, :],
                                                           start=True, stop=True),
                     r=[r_krdS, r_vrb], w=[r_pk], sig=(h == 3))
            for h in range(4):
                S.op(V, lambda h=h, b=b: nc.vector.scalar_tensor_tensor(out=rstS[:, b, h, :], in0=rstS[:, b, h, :], scalar=float(g4[h]),
                                                                        in1=pk[:, h * 128:(h + 1) * 128], op0=ALU.mult, op1=ALU.add),
                     r=[r_rstS, r_pk, r_rbS], w=[r_rstS])
        S.dma(SP, lambda: nc.sync.dma_start(out=ret_s.rearrange("b h d e -> d b h e"), in_=rstS[:]), r=[r_rstS])

    if STAGE >= 5:
        run_until(tile_body(NT, True), None)
    barrier(s_res, "smp")
    esS.close()
    if STAGE < 3:
        barrier(R.ALL, 'end')
        es1.close()
        return nc, es
    barrier(p1_res, "p1")
    es1.close()
    es2 = ExitStack()
    p2_res = []

    def sb2(name, shape, dt=F32):
        t = es2.enter_context(nc.sbuf_tensor(name, list(shape), dt))
        r = R(name)
        p2_res.append(r)
        return t, r

    wg = [sb2(f"wg{i}", [128, 8, DEXP], BF16) for i in range(2)]
    wu = [sb2(f"wu{i}", [128, 8, DEXP], BF16) for i in range(2)]
    wd = [sb2(f"wd{i}", [128, 2, D], BF16) for i in range(2)]
    Xs = [sb2(f"Xs{i}", [128, 3, D], BF16) for i in range(2)]
    XT = [sb2(f"XT{i}", [128, 8, SCH], BF16) for i in range(2)]
    sa, r_sa = sb2("sa", [128, 2, SCH], F32)
    hh = [sb2(f"hh{i}", [128, 2, SCH], BF16) for i in range(2)]
    Ys = [sb2(f"Ys{i}", [128, 3, D], F32) for i in range(2)]
    cc = 0
    for e in range(NEXP):
        wg_t, r_wg = wg[e % 2]
        wu_t, r_wu = wu[e % 2]
        wd_t, r_wd = wd[e % 2]
        S.dma(P, lambda: nc.gpsimd.dma_start(out=wg_t[:], in_=w_ge[e].rearrange("(k p) f -> p k f", p=128)), w=[r_wg])
        S.dma(P, lambda: nc.gpsimd.dma_start(out=wu_t[:], in_=w_ue[e].rearrange("(k p) f -> p k f", p=128)), w=[r_wu])
        S.dma(P, lambda: nc.gpsimd.dma_start(out=wd_t[:], in_=w_de[e].rearrange("(c p) n -> p c n", p=128)), w=[r_wd])
        for sc in range(CAP // SCH):
            row0 = e * CAP + sc * SCH
            Xs_t, r_Xs = Xs[cc % 2]
            XT_t, r_XT = XT[cc % 2]
            hh_t, r_hh = hh[cc % 2]
            Ys_t, r_Ys = Ys[cc % 2]
            cc += 1
            S.dma(SP, lambda: nc.sync.dma_start(out=Xs_t[:], in_=X_scr[row0:row0 + SCH, :].rearrange("(t p) d -> p t d", p=128)),
                  w=[r_Xs])
            for st in range(SCH // 128):
                if st % 2 == 0:
                    pt, r_pt = pT[0][0][:], pT[0][1]
                else:
                    pt, r_pt = pO[0][:].bitcast(BF16), pO[1]
                for k in range(8):
                    S.op(PE, lambda k=k, st=st, pt=pt: nc.tensor.transpose(pt[:, k * 128:(k + 1) * 128], Xs_t[:, st, k * 128:(k + 1) * 128],
                                                                           ident_b[:]),
                         r=[r_Xs, r_ident_b], w=[r_pt], sig=(k == 7))
                S.op(A if st % 2 == 0 else V,
                     (lambda st=st, pt=pt: nc.scalar.copy(out=XT_t[:, :, st * 128:(st + 1) * 128], in_=pt.rearrange("p (k t) -> p k t", k=8)))
                     if st % 2 == 0 else
                     (lambda st=st, pt=pt: nc.vector.tensor_copy(out=XT_t[:, :, st * 128:(st + 1) * 128], in_=pt.rearrange("p (k t) -> p k t", k=8))),
                     r=[r_pt], w=[r_XT])
            banks = {("a", 0): pG[0], ("a", 1): pG[1], ("u", 0): pS[0], ("u", 1): pS[1]}
            for tag, wt, r_w in (("a", wg_t, r_wg), ("u", wu_t, r_wu)):
                for fc in range(2):
                    pb, r_pb = banks[(tag, fc)]
                    for k in range(8):
                        S.op(PE, lambda k=k, fc=fc, pb=pb, wt=wt: nc.tensor.matmul(
                            pb[:, 0:SCH], wt[:, k, fc * 128:(fc + 1) * 128], XT_t[:, k, :], start=(k == 0), stop=(k == 7)),
                            r=[r_w, r_XT], w=[r_pb], sig=(k == 7))
            for fc in range(2):
                pa, r_pa = banks[("a", fc)]
                pu, r_pu = banks[("u", fc)]
                S.op(A, lambda fc=fc, pa=pa: nc.scalar.activation(out=sa[:, fc, :], in_=pa[:, 0:SCH], func=AF.Silu),
                     r=[r_pa], w=[r_sa])
                S.op(V, lambda fc=fc, pu=pu: nc.vector.tensor_tensor(out=hh_t[:, fc, :], in0=pu[:, 0:SCH], in1=sa[:, fc, :], op=ALU.mult),
                     r=[r_pu, r_sa], w=[r_hh])
            for st in range(SCH // 128):
                for n in range(2):
                    pd, r_pd = pX[n]
                    for fc in range(2):
                        S.op(PE, lambda fc=fc, n=n, st=st, pd=pd: nc.tensor.matmul(
                            pd[:], hh_t[:, fc, st * 128:(st + 1) * 128], wd_t[:, fc, n * 512:(n + 1) * 512],
                            start=(fc == 0), stop=(fc == 1)),
                            r=[r_hh, r_wd], w=[r_pd], sig=(fc == 1))
                    if n == 0:
                        S.op(A, lambda st=st, n=n, pd=pd: nc.scalar.copy(out=Ys_t[:, st, n * 512:(n + 1) * 512], in_=pd[:]),
                             r=[r_pd], w=[r_Ys])
                    else:
                        S.op(V, lambda st=st, n=n, pd=pd: nc.vector.tensor_copy(out=Ys_t[:, st, n * 512:(n + 1) * 512], in_=pd[:]),
                             r=[r_pd], w=[r_Ys])
            S.dma(SP, lambda: nc.sync.dma_start(out=Y_scr[row0:row0 + SCH, :].rearrange("(t p) d -> p t d", p=128), in_=Ys_t[:]),
                  r=[r_Ys])

    if STAGE < 4:
        barrier(R.ALL, 'end')
        es2.close()
        return nc, es
    barrier(p2_res, "p2")
    es2.close()
    es3 = ExitStack()
    p3_res = []

    def sb3(name, shape, dt=F32):
        t = es3.enter_context(nc.sbuf_tensor(name, list(shape), dt))
        r = R(name)
        p3_res.append(r)
        return t, r

    x2r = [sb3(f"x2r{i}", [128, D]) for i in range(2)]
    y1r = [sb3(f"y1r{i}", [128, D]) for i in range(2)]
    y2r = [sb3(f"y2r{i}", [128, D]) for i in range(2)]
    yo = [sb3(f"yo{i}", [128, D]) for i in range(2)]
    junk3, r_junk3 = sb3("junk3", [128, D], BF16)
    st3, r_st3 = sb3("st3", [128, 2])
    eps3, r_eps3 = sb3("eps3", [128, 1])
    fing_bc, r_fing = sb3("fing_bc", [128, D])
    gt2_bc, r_gt2 = sb3("gt2_bc", [128, D])
    S.dma(SP, lambda: nc.sync.dma_start(out=fing_bc[:], in_=fin_g.partition_broadcast(128)), w=[r_fing])
    S.dma(SP, lambda: nc.sync.dma_start(out=gt2_bc[:], in_=gt2_scr.partition_broadcast(128)), r=[r_gt2scr], w=[r_gt2])
    S.op(V, lambda: nc.vector.memset(eps3[:], EPS), w=[r_eps3])

    def combine_tile(ti, M, gt2ap, r_g2, out_ap):
        x2_t, r_x2t = x2r[ti % 2]
        y1_t, r_y1 = y1r[ti % 2]
        y2_t, r_y2 = y2r[ti % 2]
        yo_t, r_yo = yo[ti % 2]
        rows = slice(ti * 128, ti * 128 + M)
        S.dma(SP, lambda: nc.sync.dma_start(out=x2_t[0:M, :], in_=x2_scr[rows, :]), w=[r_x2t])
        for c, (yt_, r_y) in enumerate(((y1_t, r_y1), (y2_t, r_y2))):
            S.dma(P, lambda c=c, yt_=yt_: nc.gpsimd.indirect_dma_start(
                out=yt_[:], out_offset=None, in_=Y_scr,
                in_offset=bass.IndirectOffsetOnAxis(ap=idx_all[:, ti, c:c + 1], axis=0)),
                r=[r_idx, r_ytrash], w=[r_y])
        S.op(V, lambda: nc.vector.tensor_scalar(out=y1_t[0:M, :], in0=y1_t[0:M, :], scalar1=wts_all[0:M, ti, 0:1], scalar2=None,
                                                op0=ALU.mult), r=[r_y1, r_wts], w=[r_y1])
        S.op(V, lambda: nc.vector.scalar_tensor_tensor(out=y1_t[0:M, :], in0=y2_t[0:M, :], scalar=wts_all[0:M, ti, 1:2],
                                                       in1=y1_t[0:M, :], op0=ALU.mult, op1=ALU.add),
             r=[r_y1, r_y2, r_wts], w=[r_y1])
        S.op(P, lambda: nc.gpsimd.tensor_tensor(out=y1_t[0:M, :], in0=y1_t[0:M, :], in1=gt2ap, op=ALU.mult),
             r=[r_y1, r_g2], w=[r_y1])
        S.op(V, lambda: nc.vector.tensor_tensor(out=x2_t[0:M, :], in0=x2_t[0:M, :], in1=y1_t[0:M, :], op=ALU.add),
             r=[r_y1, r_x2t], w=[r_x2t])
        S.op(A, lambda: nc.scalar.activation(out=junk3[0:M, :], in_=x2_t[0:M, :], func=AF.Square, accum_out=st3[0:M, 0:1]),
             r=[r_x2t], w=[r_junk3, r_st3])
        S.op(A, lambda: nc.scalar.activation(out=st3[0:M, 0:1], in_=st3[0:M, 0:1], func=AF.Sqrt, scale=1.0 / D, bias=eps3[0:M, 0:1]),
             r=[r_st3, r_eps3], w=[r_st3])
        S.op(V, lambda: nc.vector.reciprocal(out=st3[0:M, 0:1], in_=st3[0:M, 0:1]), r=[r_st3], w=[r_st3])
        S.op(V, lambda: nc.vector.scalar_tensor_tensor(out=yo_t[0:M, :], in0=x2_t[0:M, :], scalar=st3[0:M, 0:1],
                                                       in1=fing_bc[0:M, :], op0=ALU.mult, op1=ALU.mult),
             r=[r_x2t, r_st3, r_fing], w=[r_yo])
        S.dma(SP, lambda: nc.sync.dma_start(out=out_ap, in_=yo_t[0:M, :]), r=[r_yo])

    for ti in range(NT):
        combine_tile(ti, 128, gt2_bc[:], r_gt2, y_p[ti * 128:(ti + 1) * 128, :])
    if STAGE >= 5:
        gt2s, r_gt2s = sb3("gt2s", [16, D])
        S.dma(SP, lambda: nc.sync.dma_start(out=gt2s[:], in_=mods_scr[:, 5 * D:6 * D]), r=[r_modsscr], w=[r_gt2s])
        combine_tile(NT, 16, gt2s[:], r_gt2s, y_s)

    barrier(R.ALL, "end")
    es3.close()
    return nc, es


_CACHE = {}


def _consts(T, PAST):
    f32 = np.float32
    c = {}
    c["c_ident"] = np.eye(128, dtype=f32)
    k = np.arange(128)
    c["c_mask"] = (k[:, None] <= k[None, :]).astype(f32)
    c["c_ut"] = (k[:, None] < k[None, :]).astype(f32)

    def rope_tab(pos):
        cols = []
        for d in (64, 128):
            half = d // 2
            inv = np.power(f32(10000.0), (-(f32(2.0) / f32(d)) * np.arange(half, dtype=f32)).astype(f32)).astype(f32)
            ang = (pos.astype(f32)[:, None] * inv[None, :]).astype(f32).astype(np.float64)
            cs, sn = np.cos(ang), np.sin(ang)
            cols += [cs, cs, -sn, sn]
        return np.concatenate(cols, axis=1).astype(f32)
    c["c_rope_p"] = rope_tab(np.arange(T))
    c["c_rope_s"] = rope_tab(PAST + (np.arange(16) % 4))
    gam = 1.0 - 2.0 ** (-5.0 - np.arange(4))
    sc = 128.0 ** -0.5
    j = np.arange(128)[:, None, None]
    i = np.arange(128)[None, None, :]
    g = gam[None, :, None]
    dect = np.where(i >= j, sc * g ** np.maximum(i - j, 0), 0.0)
    c["c_dect"] = dect.reshape(128, 512).astype(f32)
    qd = (g ** (i + 1.0)) * np.ones((128, 1, 1))
    c["c_qdec"] = qd.reshape(128, 512).astype(f32)
    c["c_kdec"] = (sc * gam[None, :] ** (127.0 - np.arange(128)[:, None])).astype(f32)
    ec = np.zeros((128, 18), f32)
    ec[:, :16] = np.arange(16)[None, :] * CAP
    ec[:, 16] = TRASH + np.arange(128)
    ec[:, 17] = TRASH + 128 + np.arange(128)
    c["c_ec"] = ec
    r16 = np.arange(16)
    ds = np.zeros((128, 4, 128))
    for h in range(4):
        for jj in range(16):
            for ii in range(16):
                if jj // 4 == ii // 4 and ii % 4 >= jj % 4:
                    ds[jj, h, ii] = sc * gam[h] ** (ii % 4 - jj % 4)
    c["c_dect_s"] = ds.reshape(128, 512).astype(f32)
    qs = np.zeros((128, 4, 4, 128))
    ks = np.zeros((128, 4, 4))
    mn = np.zeros((128, 4, 32))
    for b in range(4):
        for h in range(4):
            for t in range(4):
                qs[:, b, h, 4 * b + t] = gam[h] ** (t + 1.0)
                ks[4 * b + t, b, h] = sc * gam[h] ** (3.0 - t)
        for kk in range(16):
            for col in range(32):
                if kk // 4 == b and kk % 4 <= col % 4:
                    mn[kk, b, col] = 1.0
    c["c_qdec_s"] = qs.reshape(128, 2048).astype(f32)
    c["c_kdec_s"] = ks.reshape(128, 16).astype(f32)
    c["c_masknew"] = mn.reshape(128, 128).astype(f32)
    sl = np.zeros((32, 4, 4, 16))
    for row in range(32):
        for b in range(4):
            sl[row, b, row // 8, 4 * b + row % 4] = 1.0
    c["c_sel"] = sl.reshape(32, 256).astype(f32)
    sm = np.zeros((32, 2))
    sm[:, 1] = (np.arange(32) // 4) % 2
    sm[:, 0] = 1.0 - sm[:, 1]
    c["c_smask"] = sm.astype(f32)
    c["c_pcol"] = np.arange(128, dtype=f32)[:, None]
    return c


def kernel(**inp):
    f32 = np.float32
    xpr = np.asarray(inp["x_prompt"], f32)
    B, T, _ = xpr.shape
    xsm = np.asarray(inp["x_sample"], f32)
    DB, TS, _ = xsm.shape
    pt = np.asarray(inp["page_table"])
    NPG = pt.shape[1]
    ck = np.asarray(inp["cache_k"], f32)
    NPHYS = ck.shape[1]
    ncores = B
    nb = DB // ncores
    assert nb * TS == 16
    key = (T, NPG, NPHYS)
    nc, es = build(T, NPG, NPHYS)
    cst = _consts(T, NPG * 128)
    w_er = np.asarray(inp["w_expert_router"], f32)[0]
    common = {
        "w_ada": np.asarray(inp["w_ada"], f32)[0], "b_ada": np.asarray(inp["b_ada"], f32)[0][None, :],
        "g_mix": np.asarray(inp["norm_mix_g"], f32), "g_ffn": np.asarray(inp["norm_ffn_g"], f32),
        "w_in": np.asarray(inp["w_in"], f32)[0],
        "lam_p": np.concatenate([np.asarray(inp[n], f32)[0] for n in ("lambda_q1", "lambda_k1", "lambda_q2", "lambda_k2")])[None, :],
        "beta": np.asarray(inp["beta_mix"], f32), "w_out": np.asarray(inp["w_out"], f32)[0],
        "w_rt": np.ascontiguousarray(np.concatenate([np.asarray(inp["w_group"], f32)[0],
                                                     w_er.transpose(1, 0, 2).reshape(D, 16)], axis=1)),
        "b_rt": np.concatenate([np.asarray(inp["b_group"], f32)[0], np.asarray(inp["b_expert_router"], f32)[0].reshape(16)])[None, :],
        "w_ge": np.asarray(inp["w_gate_e"], f32)[0], "w_ue": np.asarray(inp["w_up_e"], f32)[0],
        "w_de": np.asarray(inp["w_down_e"], f32)[0], "fin_g": np.asarray(inp["final_g"], f32)[None, :],
    }
    common.update(cst)
    cpr = np.asarray(inp["c_prompt"], f32)
    csm = np.asarray(inp["c_sample"], f32)
    in_maps = []
    ckv2 = np.concatenate([ck[0].reshape(NPHYS * 128, 512),
                           np.asarray(inp["cache_v"], f32)[0].reshape(NPHYS * 128, 512)], axis=1)
    sret = np.asarray(inp["state_ret"], f32)[0]
    for c in range(ncores):
        m = dict(common)
        m["ckv"] = ckv2
        m["ptab"] = np.ascontiguousarray(pt[c * nb:(c + 1) * nb].astype(np.int32).reshape(1, -1))
        m["st_ret"] = np.ascontiguousarray(sret[c * nb:(c + 1) * nb])
        m["xp"] = np.ascontiguousarray(xpr[c])
        m["xs"] = np.ascontiguousarray(xsm[c * nb:(c + 1) * nb].reshape(16, D))
        m["cp_rep"] = np.ascontiguousarray(np.repeat(cpr[c:c + 1], 128, axis=0))
        m["cs_rep"] = np.ascontiguousarray(np.repeat(csm[c * nb:(c + 1) * nb], TS, axis=0))
        in_maps.append(m)
    res = run_bass_kernel_spmd(nc, in_maps, core_ids=list(range(ncores)))
    try:
        es.close()
    except Exception:
        pass
    rr = res.results
    y_prompt = np.stack([r["y_p"] for r in rr]).astype(f32)
    y_sample = np.concatenate([r["y_s"].reshape(nb, TS, D) for r in rr]).astype(f32)
    k_prompt = np.stack([r["k_p"].reshape(T, 8, 64) for r in rr])[None].astype(f32)
    v_prompt = np.stack([r["v_p"].reshape(T, 4, 128) for r in rr])[None].astype(f32)
    ret_prompt = np.stack([r["ret_p"] for r in rr])[None].astype(f32)
    k_sample = np.concatenate([r["k_s"].reshape(nb, TS, 8, 64) for r in rr])[None].astype(f32)
    v_sample = np.concatenate([r["v_s"].reshape(nb, TS, 4, 128) for r in rr])[None].astype(f32)
    ret_sample = np.concatenate([r["ret_s"] for r in rr])[None].astype(f32)
    return (y_prompt, y_sample, k_prompt, v_prompt, ret_prompt, k_sample, v_sample, ret_sample)
```
